# Optimizing a Trainium2 kernel written in Bass

```python
import math
import jax, jax.numpy as jnp
from jax import lax
import numpy as np

D_MODEL = 1024
BATCH = 8
SEQ = 2048
DEPTH = 1
DEC_BATCH = 128
DEC_SEQ = 4
PAST_LEN = 2048
PAGE_SIZE = 128

HEAD_DIM = 64
N_HEADS_SB = 8
N_HEADS_MOBA = 8
SB_WIDTH = N_HEADS_SB * HEAD_DIM
MOBA_WIDTH = N_HEADS_MOBA * HEAD_DIM
IN_WIDTH = 3 * SB_WIDTH + 3 * MOBA_WIDTH + 2 * D_MODEL
ROPE_DIM = HEAD_DIM // 4
ROPE_THETA = 500000.0
SB_Q_BLOCK = 128
MOBA_BLOCK = 256
MOBA_TOPK = 3
MOBA_Q_CHUNK = 16
N_GROUPS = 4
EXPERTS_PER_GROUP = 4
N_EXPERTS = N_GROUPS * EXPERTS_PER_GROUP
TOP_K_IN_GROUP = 2
D_EXPERT = 256
NORM_EPS = 1e-6
POOL_NUM = 5
POOL_DEN = 4

kernel_name = "stickbreak_moba_hiermoe_decode_step"


def rms_norm(x, g):
    xf = x.astype(jnp.float32)
    r = lax.rsqrt(jnp.mean(xf * xf, axis=-1, keepdims=True) + NORM_EPS)
    return (xf * r).astype(x.dtype) * g


def partial_rope(x, pos):
    half = ROPE_DIM // 2
    inv = jnp.float32(ROPE_THETA) ** (-jnp.arange(half, dtype=jnp.float32) / half)
    ang = pos.astype(jnp.float32)[:, None] * inv[None, :]
    cos = jnp.cos(ang)[None, :, None, :]
    sin = jnp.sin(ang)[None, :, None, :]
    xr = x[..., :ROPE_DIM].astype(jnp.float32)
    x1, x2 = xr[..., :half], xr[..., half:]
    rot = jnp.concatenate([x1 * cos - x2 * sin, x1 * sin + x2 * cos], axis=-1).astype(x.dtype)
    return jnp.concatenate([rot, x[..., ROPE_DIM:]], axis=-1)


def mixer_inputs(x, pos, g, w_in):
    B, T, _ = x.shape
    h = jnp.einsum('btd,de->bte', rms_norm(x, g), w_in)
    widths = [SB_WIDTH] * 3 + [MOBA_WIDTH] * 3 + [D_MODEL, D_MODEL]
    cuts = [int(c) for c in np.cumsum(widths)[:-1]]
    q_sb, k_sb, v_sb, q_m, k_m, v_m, gate_sb, gate_m = jnp.split(h, cuts, axis=-1)
    q_sb = q_sb.reshape(B, T, N_HEADS_SB, HEAD_DIM)
    k_sb = k_sb.reshape(B, T, N_HEADS_SB, HEAD_DIM)
    v_sb = v_sb.reshape(B, T, N_HEADS_SB, HEAD_DIM)
    q_m = partial_rope(q_m.reshape(B, T, N_HEADS_MOBA, HEAD_DIM), pos)
    k_m = partial_rope(k_m.reshape(B, T, N_HEADS_MOBA, HEAD_DIM), pos)
    v_m = v_m.reshape(B, T, N_HEADS_MOBA, HEAD_DIM)
    return q_sb, k_sb, v_sb, q_m, k_m, v_m, gate_sb, gate_m


def stick_breaking(q, k, v, q_pos, k_pos):
    z = jnp.einsum('bqhd,bkhd->bhqk', q, k, preferred_element_type=jnp.float32) * (HEAD_DIM ** -0.5)
    mask = (k_pos[None, :] < q_pos[:, None])[None, None]
    log_keep = jnp.where(mask, jax.nn.log_sigmoid(-z), 0.0)
    suffix = lax.cumsum(log_keep, axis=3, reverse=True) - log_keep
    a = jnp.where(mask, jnp.exp(jax.nn.log_sigmoid(z) + suffix), 0.0)
    return jnp.einsum('bhqk,bkhd->bqhd', a.astype(v.dtype), v)


def sb_prompt(q, k, v):
    B, S, H, D = q.shape
    nb = S // SB_Q_BLOCK
    qb = q.reshape(B, nb, SB_Q_BLOCK, H, D).transpose(1, 0, 2, 3, 4)
    k_pos = jnp.arange(S)

    def blk(args):
        qi, i = args
        return stick_breaking(qi, k, v, i * SB_Q_BLOCK + jnp.arange(SB_Q_BLOCK), k_pos)

    out = lax.map(blk, (qb, jnp.arange(nb)))
    return out.transpose(1, 0, 2, 3, 4).reshape(B, S, H, D)


def moba_blocks(k, v):
    B, T, H, D = k.shape
    nb = -(-T // MOBA_BLOCK)
    pad = nb * MOBA_BLOCK - T
    kp = jnp.pad(k, ((0, 0), (0, pad), (0, 0), (0, 0)))
    vp = jnp.pad(v, ((0, 0), (0, pad), (0, 0), (0, 0)))
    kb = kp.reshape(B, nb, MOBA_BLOCK, H, D).transpose(0, 3, 1, 2, 4)
    vb = vp.reshape(B, nb, MOBA_BLOCK, H, D).transpose(0, 3, 1, 2, 4)
    kmean = jnp.mean(kb.astype(jnp.float32), axis=3)
    return kb, vb, kmean


def moba_attend(q, q_pos, kb, vb, kmean):
    B, C, H, D = q.shape
    nb = kb.shape[2]
    k_sel = min(MOBA_TOPK, nb)
    own = q_pos // MOBA_BLOCK
    s_blk = jnp.einsum('bchd,bhnd->bchn', q.astype(jnp.float32), kmean)
    past = jnp.arange(nb)[None, :] < own[:, None]
    s_blk = jnp.where(past[None, :, None, :], s_blk, -jnp.inf)
    top_s, top_i = lax.top_k(s_blk, k_sel)
    idx = jnp.concatenate([top_i, jnp.broadcast_to(own[None, :, None, None], (B, C, H, 1))], axis=-1)
    valid = jnp.concatenate([jnp.isfinite(top_s), jnp.ones((B, C, H, 1), dtype=bool)], axis=-1)
    bi = jnp.arange(B)[:, None, None, None]
    hi = jnp.arange(H)[None, None, :, None]
    kg = kb[bi, hi, idx]
    vg = vb[bi, hi, idx]
    key_pos = idx[..., None] * MOBA_BLOCK + jnp.arange(MOBA_BLOCK)
    mask = valid[..., None] & (key_pos <= q_pos[None, :, None, None, None])
    logits = jnp.einsum('bchd,bchkjd->bchkj', q, kg, preferred_element_type=jnp.float32) * (HEAD_DIM ** -0.5)
    logits = jnp.where(mask, logits, -jnp.inf)
    p = jax.nn.softmax(logits.reshape(B, C, H, -1), axis=-1).reshape(logits.shape)
    return jnp.einsum('bchkj,bchkjd->bchd', p.astype(vg.dtype), vg)


def moba_sweep(q, pos0, kb, vb, kmean, chunk):
    B, T, H, D = q.shape
    n = T // chunk
    qc = q.reshape(B, n, chunk, H, D).transpose(1, 0, 2, 3, 4)

    def f(args):
        qi, i = args
        return moba_attend(qi, pos0 + i * chunk + jnp.arange(chunk), kb, vb, kmean)

    out = lax.map(f, (qc, jnp.arange(n)))
    return out.transpose(1, 0, 2, 3, 4).reshape(B, T, H, D)


def merge_branches(x, o_sb, o_m, gate_sb, gate_m, w_proj_sb, w_proj_moba, w_out):
    B, T, _ = x.shape
    b_sb = jnp.einsum('bte,ed->btd', o_sb.reshape(B, T, SB_WIDTH), w_proj_sb)
    b_m = jnp.einsum('bte,ed->btd', o_m.reshape(B, T, MOBA_WIDTH), w_proj_moba)
    m = jax.nn.sigmoid(gate_sb) * b_sb + jax.nn.sigmoid(gate_m) * b_m
    return x + jnp.einsum('btd,de->bte', m, w_out)


def hier_moe(x, w_rg, b_rg, w_re, b_re, w_gate, w_up, w_down):
    B, T, _ = x.shape
    g_prob = jax.nn.softmax((jnp.einsum('btd,dg->btg', x, w_rg) + b_rg).astype(jnp.float32), axis=-1)
    g_p, g_i = lax.top_k(g_prob, 1)
    e_logit = (jnp.einsum('btd,de->bte', x, w_re) + b_re).astype(jnp.float32)
    e_logit = e_logit.reshape(B, T, N_GROUPS, EXPERTS_PER_GROUP)
    e_logit = jnp.take_along_axis(e_logit, g_i[..., None], axis=2)[:, :, 0]
    e_p, e_i = lax.top_k(jax.nn.softmax(e_logit, axis=-1), TOP_K_IN_GROUP)
    w_sel = g_p * e_p / jnp.sum(e_p, axis=-1, keepdims=True)
    ids = g_i * EXPERTS_PER_GROUP + e_i
    combine = jnp.einsum('btk,btke->bte', w_sel, jax.nn.one_hot(ids, N_EXPERTS, dtype=jnp.float32)).astype(x.dtype)
    a = jnp.einsum('btd,edf->btef', x, w_gate)
    u = jnp.einsum('btd,edf->btef', x, w_up)
    hdn = jax.nn.silu(a) * u * combine[..., None]
    return jnp.einsum('btef,efd->btd', hdn, w_down)


def setup_inputs(seed: int = 0) -> dict:
    key = jax.random.key(seed)
    ks = jax.random.split(key, 24)
    n_pages = PAST_LEN // PAGE_SIZE
    n_phys = (DEC_BATCH * n_pages * POOL_NUM) // POOL_DEN
    f32 = jnp.float32
    nrm = lambda k, shape, scale: jax.random.normal(k, shape, f32) * scale
    pool_shape = (DEPTH, n_phys, PAGE_SIZE, N_HEADS_SB, HEAD_DIM)
    pool_shape_m = (DEPTH, n_phys, PAGE_SIZE, N_HEADS_MOBA, HEAD_DIM)
    perm = jax.random.permutation(ks[6], n_phys)
    page_table = perm[:DEC_BATCH * n_pages].reshape(DEC_BATCH, n_pages).astype(jnp.int32)
    return {
        "x_prompt": nrm(ks[0], (BATCH, SEQ, D_MODEL), 1.0),
        "x_sample": nrm(ks[1], (DEC_BATCH, DEC_SEQ, D_MODEL), 1.0),
        "cache_sb_k": nrm(ks[2], pool_shape, 1.0),
        "cache_sb_v": nrm(ks[3], pool_shape, 1.0),
        "cache_moba_k": nrm(ks[4], pool_shape_m, 1.0),
        "cache_moba_v": nrm(ks[5], pool_shape_m, 1.0),
        "page_table": page_table,
        "g_mix": 1.0 + nrm(ks[7], (DEPTH, D_MODEL), 0.01),
        "w_in": nrm(ks[8], (DEPTH, D_MODEL, IN_WIDTH), D_MODEL ** -0.5),
        "w_proj_sb": nrm(ks[9], (DEPTH, SB_WIDTH, D_MODEL), SB_WIDTH ** -0.5),
        "w_proj_moba": nrm(ks[10], (DEPTH, MOBA_WIDTH, D_MODEL), MOBA_WIDTH ** -0.5),
        "w_out": nrm(ks[11], (DEPTH, D_MODEL, D_MODEL), D_MODEL ** -0.5),
        "g_ffn": 1.0 + nrm(ks[12], (DEPTH, D_MODEL), 0.01),
        "w_router_group": nrm(ks[13], (DEPTH, D_MODEL, N_GROUPS), D_MODEL ** -0.5),
        "b_router_group": nrm(ks[14], (DEPTH, N_GROUPS), 0.01),
        "w_router_expert": nrm(ks[15], (DEPTH, D_MODEL, N_EXPERTS), D_MODEL ** -0.5),
        "b_router_expert": nrm(ks[16], (DEPTH, N_EXPERTS), 0.01),
        "w_expert_gate": nrm(ks[17], (DEPTH, N_EXPERTS, D_MODEL, D_EXPERT), D_MODEL ** -0.5),
        "w_expert_up": nrm(ks[18], (DEPTH, N_EXPERTS, D_MODEL, D_EXPERT), D_MODEL ** -0.5),
        "w_expert_down": nrm(ks[19], (DEPTH, N_EXPERTS, D_EXPERT, D_MODEL), D_EXPERT ** -0.5),
        "g_final": 1.0 + nrm(ks[20], (D_MODEL,), 0.01),
    }


def reference(x_prompt, x_sample, cache_sb_k, cache_sb_v, cache_moba_k, cache_moba_v, page_table,
              g_mix, w_in, w_proj_sb, w_proj_moba, w_out, g_ffn, w_router_group, b_router_group,
              w_router_expert, b_router_expert, w_expert_gate, w_expert_up, w_expert_down, g_final):
    seq = x_prompt.shape[1]
    dec_b, dec_seq = x_sample.shape[0], x_sample.shape[1]
    past_len = page_table.shape[1] * PAGE_SIZE
    pos_p = jnp.arange(seq)
    pos_s = past_len + jnp.arange(dec_seq)
    k_pos_s = jnp.arange(past_len + dec_seq)
    hp, hs = x_prompt, x_sample
    p_sbk, p_sbv, p_mk, p_mv = [], [], [], []
    s_sbk, s_sbv, s_mk, s_mv = [], [], [], []
    for layer in range(DEPTH):
        moe_w = (w_router_group[layer], b_router_group[layer], w_router_expert[layer],
                 b_router_expert[layer], w_expert_gate[layer], w_expert_up[layer], w_expert_down[layer])
        q_sb, k_sb, v_sb, q_m, k_m, v_m, ga, gb = mixer_inputs(hp, pos_p, g_mix[layer], w_in[layer])
        o_sb = sb_prompt(q_sb, k_sb, v_sb)
        kb, vb, km = moba_blocks(k_m, v_m)
        o_m = moba_sweep(q_m, 0, kb, vb, km, MOBA_Q_CHUNK)
        hp = merge_branches(hp, o_sb, o_m, ga, gb, w_proj_sb[layer], w_proj_moba[layer], w_out[layer])
        hp = hp + hier_moe(rms_norm(hp, g_ffn[layer]), *moe_w)
        p_sbk.append(k_sb); p_sbv.append(v_sb); p_mk.append(k_m); p_mv.append(v_m)
        q_sb, k_sb, v_sb, q_m, k_m, v_m, ga, gb = mixer_inputs(hs, pos_s, g_mix[layer], w_in[layer])
        past_sbk = cache_sb_k[layer, page_table].reshape(dec_b, past_len, N_HEADS_SB, HEAD_DIM)
        past_sbv = cache_sb_v[layer, page_table].reshape(dec_b, past_len, N_HEADS_SB, HEAD_DIM)
        o_sb = stick_breaking(q_sb, jnp.concatenate([past_sbk, k_sb], axis=1),
                              jnp.concatenate([past_sbv, v_sb], axis=1), pos_s, k_pos_s)
        past_mk = cache_moba_k[layer, page_table].reshape(dec_b, past_len, N_HEADS_MOBA, HEAD_DIM)
        past_mv = cache_moba_v[layer, page_table].reshape(dec_b, past_len, N_HEADS_MOBA, HEAD_DIM)
        kb, vb, km = moba_blocks(jnp.concatenate([past_mk, k_m], axis=1), jnp.concatenate([past_mv, v_m], axis=1))
        o_m = moba_sweep(q_m, past_len, kb, vb, km, 1)
        hs = merge_branches(hs, o_sb, o_m, ga, gb, w_proj_sb[layer], w_proj_moba[layer], w_out[layer])
        hs = hs + hier_moe(rms_norm(hs, g_ffn[layer]), *moe_w)
        s_sbk.append(k_sb); s_sbv.append(v_sb); s_mk.append(k_m); s_mv.append(v_m)
    y_prompt = rms_norm(hp, g_final)
    y_sample = rms_norm(hs, g_final)
    return (y_prompt, y_sample,
            jnp.stack(p_sbk), jnp.stack(p_sbv), jnp.stack(p_mk), jnp.stack(p_mv),
            jnp.stack(s_sbk), jnp.stack(s_sbv), jnp.stack(s_mk), jnp.stack(s_mv))
```

```python
import numpy as np
import concourse.bass as bass
import concourse.mybir as mybir
from concourse.bass_utils import run_bass_kernel_spmd

F32 = mybir.dt.float32
BF16 = mybir.dt.bfloat16
I32 = mybir.dt.int32
U32 = mybir.dt.uint32
AF = mybir.ActivationFunctionType
ALU = mybir.AluOpType
AX = mybir.AxisListType

NCORES = 8
D = 1024
S = 2048
NT = 17
NTOK = NT * 128
NEG = -30000.0
import os
STAGE = int(os.environ.get("KSTAGE", "99"))
SIM = os.environ.get("KSIM", "0") == "1"
SMALL = SIM or os.environ.get("KSMALL", "0") == "1"
NSEQ = int(os.environ.get("KNSEQ", "1")) if SIM else 16


class Buf:
    __slots__ = ("name", "lw", "rd", "excl", "last", "last_w")

    def __init__(self, name, excl=False):
        self.name = name
        self.lw = None
        self.rd = []
        self.excl = excl
        self.last = None
        self.last_w = False


class Op:
    __slots__ = ("eng", "fn", "deps", "sig", "need", "isdma", "semidx")

    def __init__(self, eng, fn, isdma):
        self.eng = eng
        self.fn = fn
        self.deps = {}
        self.sig = None
        self.need = False
        self.isdma = isdma
        self.semidx = -1


class Sched:
    ENG = ["pe", "act", "dve", "pool", "sp"]
    NDMA = 24
    NSW = 8
    ROLL = 30000

    def __init__(self, nc):
        self.nc = nc
        self.ops = {e: [] for e in self.ENG}
        self.dma_last = [None] * self.NDMA
        self.dma_rr = 0
        self.dmas = []
        self.sw_last = [None] * self.NSW
        self.sw_rr = 0
        self.sw_fresh = SIM

    def _deps(self, o, r, w):
        for b in r:
            if b.excl:
                if b.last is not None and b.last is not o:
                    o.deps.setdefault(b.last, "raw" if b.last_w else "war")
                b.last, b.last_w = o, False
                continue
            if b.lw is not None and b.lw is not o:
                o.deps.setdefault(b.lw, "raw")
            b.rd.append(o)
        for b in w:
            if b.excl:
                if b.last is not None and b.last is not o:
                    o.deps[b.last] = "raw"
                b.last, b.last_w = o, True
                continue
            if b.lw is not None and b.lw is not o:
                o.deps[b.lw] = "raw"
            for x in b.rd:
                if x is not o:
                    o.deps.setdefault(x, "war")
            b.rd = []
            b.lw = o

    def op(self, eng, fn, r=(), w=()):
        o = Op(eng, fn, False)
        self._deps(o, r, w)
        self.ops[eng].append(o)
        return o

    def barrier(self):
        lasts = []
        for e in self.ENG:
            comp = [o for o in self.ops[e] if not o.isdma]
            if comp:
                lasts.append(comp[-1])
        lasts += [d for d in self.dma_last if d is not None] + [d for d in self.sw_last if d is not None]
        if self.sw_fresh:
            lasts += [d for d in self.dmas if d.semidx < 0]
        for e in self.ENG:
            o = Op(e, lambda eng: eng.nop(), False)
            for d in lasts:
                o.deps[d] = "raw"
            self.ops[e].append(o)

    def dma(self, q, fn, r=(), w=(), sw=False):
        o = Op(q, fn, True)
        if sw and self.sw_fresh:
            o.semidx = -1
        elif sw:
            i = self.sw_rr
            self.sw_rr = (i + 1) % self.NSW
            if self.sw_last[i] is not None:
                o.deps[self.sw_last[i]] = "raw"
            self.sw_last[i] = o
            o.semidx = self.NDMA + i
        else:
            i = self.dma_rr
            self.dma_rr = (i + 1) % self.NDMA
            if self.dma_last[i] is not None:
                o.deps[self.dma_last[i]] = "raw"
            self.dma_last[i] = o
            o.semidx = i
        self._deps(o, r, w)
        self.ops[q].append(o)
        self.dmas.append(o)
        return o

    def emit(self):
        nc = self.nc
        for e in self.ENG:
            for o in self.ops[e]:
                keep = {}
                for d, kind in o.deps.items():
                    if d.isdma:
                        keep[d] = kind
                    elif d.eng == o.eng and not o.isdma:
                        if o.eng == "pe":
                            continue
                        keep[d] = kind
                    else:
                        keep[d] = kind
                o.deps = keep
                for d in keep:
                    d.need = True
        nsem = self.NDMA + self.NSW
        dma_sems = [nc.alloc_semaphore(name=f"dq{i}") for i in range(nsem)]
        dma_cnt = [0] * nsem
        for o in self.dmas:
            if o.semidx < 0:
                dma_sems.append(nc.alloc_semaphore(name=f"dsw{len(dma_sems)}"))
                dma_cnt.append(0)
                o.semidx = len(dma_sems) - 1
            dma_cnt[o.semidx] += 16
            o.sig = (dma_sems[o.semidx], dma_cnt[o.semidx])
        for e in self.ENG:
            sem = None
            cnt = 0
            k = 0
            for o in self.ops[e]:
                if o.isdma or not o.need:
                    continue
                if sem is None or cnt >= self.ROLL:
                    sem = nc.alloc_semaphore(name=f"e_{e}_{k}")
                    k += 1
                    cnt = 0
                cnt += 1
                o.sig = (sem, cnt)
        handles = {"pe": "tensor", "act": "scalar", "dve": "vector", "pool": "gpsimd", "sp": "sync"}
        final = [(dma_sems[i], dma_cnt[i]) for i in range(len(dma_sems)) if dma_cnt[i] > 0]
        sched = self

        with nc.Block() as block:
            def run(e, eng):
                known = {}
                for o in sched.ops[e]:
                    waits = {}
                    for d in o.deps:
                        s, v = d.sig
                        if waits.get(s, 0) < v:
                            waits[s] = v
                    for s, v in waits.items():
                        if known.get(s, 0) >= v:
                            continue
                        eng.wait_ge(s, v)
                        known[s] = v
                    ins = o.fn(eng)
                    if o.sig is not None:
                        ins.then_inc(o.sig[0], 16 if o.isdma else 1)
                if e == "sp":
                    for s, v in final:
                        eng.wait_ge(s, v)

            @block.tensor
            def _(eng):
                run("pe", eng)

            @block.scalar
            def _(eng):
                run("act", eng)

            @block.vector
            def _(eng):
                run("dve", eng)

            @block.gpsimd
            def _(eng):
                run("pool", eng)

            @block.sync
            def _(eng):
                run("sp", eng)


C_ID, C_NTRI, C_MSB, C_MMB = 0, 128, 256, 384
C_COS, C_SIN = 512, 512 + NT * 8
C_W = 512 + 2 * NT * 8


def make_consts():
    c = np.zeros((128, C_W), np.float32)
    p = np.arange(128)
    c[:, C_ID:C_ID + 128] = np.eye(128, dtype=np.float32)
    c[:, C_NTRI:C_NTRI + 128] = -(p[:, None] >= p[None, :]).astype(np.float32)
    c[:, C_MSB:C_MSB + 128] = NEG * (p[:, None] >= p[None, :])
    c[:, C_MMB:C_MMB + 128] = NEG * (p[:, None] > p[None, :])
    half = 8
    inv = np.float32(500000.0) ** (-np.arange(half, dtype=np.float32) / half)
    pos = np.zeros((NT, 128), np.float32)
    for i in range(16):
        pos[i] = i * 128 + p
    pos[16, :64] = 2048 + (p[:64] % 4)
    ang = pos[:, :, None].astype(np.float32) * inv[None, None, :]
    cs = np.cos(ang).astype(np.float32).transpose(1, 0, 2)
    sn = np.sin(ang).astype(np.float32).transpose(1, 0, 2)
    c[:, C_COS:C_COS + NT * 8] = cs.reshape(128, NT * 8)
    c[:, C_SIN:C_SIN + NT * 8] = sn.reshape(128, NT * 8)
    return c


def build():
    nc = bass.Bass("TRN2", target_bir_lowering=False)
    K = Sched(nc)

    def dram(name, shape, dt, kind):
        return nc.dram_tensor(name, list(shape), dt, kind=kind).ap()

    xin = dram("xin", [NTOK, D], F32, "ExternalInput")
    cst_d = dram("cst", [128, C_W], F32, "ExternalInput")
    g_mix = dram("g_mix", [1, D], F32, "ExternalInput")
    w_in = dram("w_in", [D, 5120], F32, "ExternalInput")
    NROWS = (256 if SMALL else 2560) * 128
    cache = {nm: dram("cache_" + nm, [NROWS, 512], F32, "ExternalInput") for nm in ("sb_k", "sb_v", "mb_k", "mb_v")}
    ptab = dram("ptab", [1, 256], I32, "ExternalInput")
    m01_d = dram("m01", [2, 128, 512], F32, "ExternalInput")
    w_ps = dram("w_proj_sb", [512, D], F32, "ExternalInput")
    w_pm = dram("w_proj_moba", [512, D], F32, "ExternalInput")
    w_o = dram("w_out", [D, D], F32, "ExternalInput")
    g_ffn = dram("g_ffn", [1, D], F32, "ExternalInput")
    g_fin = dram("g_final", [1, D], F32, "ExternalInput")
    w_r = dram("w_router", [D, 20], F32, "ExternalInput")
    b_r = dram("b_router", [1, 20], F32, "ExternalInput")
    w_eg = dram("w_expert_gate", [16, D, 256], F32, "ExternalInput")
    w_eu = dram("w_expert_up", [16, D, 256], F32, "ExternalInput")
    w_ed = dram("w_expert_down", [16, 256, D], F32, "ExternalInput")
    kv_out = dram("kv_out", [4, NTOK, 512], F32, "ExternalOutput")
    y_out = dram("y_out", [NTOK, D], F32, "ExternalOutput")

    def sb(name, shape, dt):
        return nc.alloc_sbuf_tensor("sb_" + name, list(shape), dt).ap()

    ps = [nc.alloc_psum_tensor(f"ps{i}", [128, 512], F32).ap() for i in range(8)]
    psb = [Buf(f"ps{i}", excl=True) for i in range(8)]

    cst = sb("cst", [128, C_W], F32)
    b_cst = Buf("cst")
    K.dma("sp", lambda e: e.dma_start(out=cst[:], in_=cst_d[:]), w=[b_cst])
    cbf = sb("cbf", [128, 512], BF16)
    b_cbf = Buf("cbf")
    K.op("dve", lambda e: e.tensor_copy(out=cbf[:], in_=cst[:, 0:512]), r=[b_cst], w=[b_cbf])
    ident = cbf[:, C_ID:C_ID + 128]
    ntri = cbf[:, C_NTRI:C_NTRI + 128]
    msb = cbf[:, C_MSB:C_MSB + 128]
    mmb = cbf[:, C_MMB:C_MMB + 128]
    cosT = cst[:, C_COS:C_COS + NT * 8].rearrange("p (t f) -> p t f", f=8)
    sinT = cst[:, C_SIN:C_SIN + NT * 8].rearrange("p (t f) -> p t f", f=8)
    gbc = sb("gbc", [128, D], F32)
    b_gbc = Buf("gbc")
    K.dma("sp", lambda e: e.dma_start(out=gbc[:], in_=g_mix.partition_broadcast(128)), w=[b_gbc])

    xnT = sb("xnT", [128, 8, NTOK], BF16)
    b_xnT = [Buf(f"xnT{i}") for i in range(NT)]
    QsT = sb("QsT", [128, 4, NTOK], BF16)
    KsT = sb("KsT", [128, 4, NTOK], BF16)
    Vs = sb("Vs", [128, NT, 512], BF16)
    b_QsT = [Buf(f"QsT{i}") for i in range(NT)]
    b_KsT = [Buf(f"KsT{i}") for i in range(NT)]
    b_Vs = [Buf(f"Vs{i}") for i in range(NT)]

    xt = [sb(f"xt{j}", [128, D], F32) for j in range(2)]
    b_xt = [Buf(f"xt{j}") for j in range(2)]
    junk = sb("junk", [128, D], BF16)
    b_junk = Buf("junk")
    ss = sb("ss", [128, NT], F32)
    rs = sb("rs", [128, NT], F32)
    rstd = sb("rstd", [128, NT], F32)
    b_ss = [Buf(f"ss{i}") for i in range(NT)]
    b_rs = [Buf(f"rs{i}") for i in range(NT)]
    b_rstd = [Buf(f"rstd{i}") for i in range(NT)]
    xn = [sb(f"xn{j}", [128, D], BF16) for j in range(2)]
    b_xn = [Buf(f"xn{j}") for j in range(2)]

    for i in range(int(os.environ.get("KNT", NT))):
        j = i % 2
        K.dma("sp", lambda e, i=i, j=j: e.dma_start(out=xt[j][:], in_=xin[i * 128:(i + 1) * 128, :]), w=[b_xt[j]])
        K.op("act", lambda e, i=i, j=j: e.activation(out=junk[:], in_=xt[j][:], func=AF.Square,
                                                       accum_out=ss[:, i:i + 1]),
             r=[b_xt[j]], w=[b_junk, b_ss[i]])
        K.op("act", lambda e, i=i: e.activation(out=rs[:, i:i + 1], in_=ss[:, i:i + 1], func=AF.Sqrt,
                                                 bias=1e-6, scale=1.0 / D), r=[b_ss[i]], w=[b_rs[i]])
        K.op("dve", lambda e, i=i: e.reciprocal(out=rstd[:, i:i + 1], in_=rs[:, i:i + 1]), r=[b_rs[i]], w=[b_rstd[i]])
        K.op("dve", lambda e, i=i, j=j: e.scalar_tensor_tensor(out=xn[j][:], in0=xt[j][:], scalar=rstd[:, i:i + 1],
                                                                in1=gbc[:], op0=ALU.mult, op1=ALU.mult),
             r=[b_xt[j], b_rstd[i], b_gbc], w=[b_xn[j]])
        pb = i % 2
        pst = ps[pb].bitcast(BF16)
        for kc in range(8):
            K.op("pe", lambda e, kc=kc, j=j, pst=pst: e.transpose(out=pst[:, kc * 128:(kc + 1) * 128],
                                                                     in_=xn[j][:, kc * 128:(kc + 1) * 128],
                                                                     identity=ident),
                 r=[b_xn[j], b_cbf], w=[psb[pb]])
        K.op("act", lambda e, i=i, pst=pst: e.activation(out=xnT[:, :, i * 128:(i + 1) * 128],
                                                          in_=pst.rearrange("p (k t) -> p k t", k=8), func=AF.Copy),
             r=[psb[pb]], w=[b_xnT[i]])

    wb = [sb(f"wb{j}", [128, 8, 512], BF16) for j in range(2)]
    b_wb = [Buf(f"wb{j}") for j in range(2)]
    w_in_v = w_in.rearrange("(k p) n -> p k n", p=128)
    wcount = [0]

    wst = [sb(f"wst{j}", [128, 4, 512], F32) for j in range(2)]
    b_wst = [Buf(f"wst{j}") for j in range(2)]

    def load_w(c):
        j = wcount[0] % 2
        wcount[0] += 1
        for h in range(2):
            K.dma("sp", lambda e, c=c, h=h: e.dma_start(out=wst[h][:], in_=w_in_v[:, 4 * h:4 * h + 4, c * 512:(c + 1) * 512]),
                  w=[b_wst[h]])
            K.op("pool", lambda e, j=j, h=h: e.tensor_copy(out=wb[j][:, 4 * h:4 * h + 4, :], in_=wst[h][:]),
                 r=[b_wst[h]], w=[b_wb[j]])
        return j

    hf = [sb(f"hf{j}", [128, 512], F32) for j in range(2)]
    b_hf = [Buf(f"hf{j}") for j in range(2)]
    hb = [sb(f"hb{j}", [128, 512], BF16) for j in range(2)]
    b_hb = [Buf(f"hb{j}") for j in range(2)]
    cnt = {"proj": 0, "hf": 0, "hb": 0}

    def project(c, i, j):
        pb = 2 + cnt["proj"] % 2
        cnt["proj"] += 1
        for kc in range(8):
            K.op("pe", lambda e, kc=kc, i=i, j=j, pb=pb: e.matmul(ps[pb][:], lhsT=xnT[:, kc, i * 128:(i + 1) * 128],
                                                                    rhs=wb[j][:, kc, :], start=(kc == 0), stop=(kc == 7)),
                 r=[b_xnT[i], b_wb[j]], w=[psb[pb]])
        return pb

    def to_T(src_bf, b_src, dstT, b_dst, i, pbank, dst_ap=None):
        pst = ps[pbank].bitcast(BF16)
        if dst_ap is None:
            dst_ap = dstT[:, :, i * 128:(i + 1) * 128]
        for cc in range(4):
            K.op("pe", lambda e, cc=cc: e.transpose(out=pst[:, cc * 128:(cc + 1) * 128],
                                                     in_=src_bf[:, cc * 128:(cc + 1) * 128], identity=ident),
                 r=[b_src, b_cbf], w=[psb[pbank]])
        K.op("dve", lambda e: e.tensor_copy(out=dst_ap,
                                            in_=pst[:, 0:512].rearrange("p (k t) -> p k t", k=4)),
             r=[psb[pbank]], w=[b_dst])

    def kv_store(which, i, src, b_src):
        K.dma("sp", lambda e: e.dma_start(out=kv_out[which, i * 128:(i + 1) * 128, :], in_=src[:]), r=[b_src])

    samp = {}
    for br in ("sb", "mb"):
        samp[br] = dict(QT=sb(f"sQT_{br}", [128, 4, 128], BF16), KT=sb(f"sKT_{br}", [128, 4, 128], BF16),
                        V=sb(f"sV_{br}", [128, 512], BF16), b=Buf(f"samp_{br}"))
    sQTf = sb("sQTf", [128, 4, 128], F32)
    b_sQTf = Buf("sQTf")

    for (c, kind) in (((1, "k"), (2, "v"), (0, "q"))[:int(os.environ.get("KA1", 3))] if STAGE >= 1 else ()):
        j = load_w(c)
        for i in range(int(os.environ.get("KA1T", NT))):
            pb = project(c, i, j)
            if kind in ("k", "v"):
                a = cnt["hf"] % 2
                cnt["hf"] += 1
                K.op("act", lambda e, a=a, pb=pb: e.activation(out=hf[a][:], in_=ps[pb][:], func=AF.Copy),
                     r=[psb[pb]], w=[b_hf[a]])
                kv_store(0 if kind == "k" else 1, i, hf[a], b_hf[a])
            if kind == "k":
                b = cnt["hb"] % 2
                cnt["hb"] += 1
                K.op("dve", lambda e, b=b, pb=pb: e.tensor_copy(out=hb[b][:], in_=ps[pb][:]), r=[psb[pb]], w=[b_hb[b]])
                if i == 16:
                    to_T(hb[b], b_hb[b], None, samp["sb"]["b"], i, 4 + i % 2, dst_ap=samp["sb"]["KT"][:])
                else:
                    to_T(hb[b], b_hb[b], KsT, b_KsT[i], i, 4 + i % 2)
            elif kind == "v":
                if i == 16:
                    K.op("dve", lambda e, pb=pb: e.tensor_copy(out=samp["sb"]["V"][:], in_=ps[pb][:]), r=[psb[pb]], w=[samp["sb"]["b"]])
                else:
                    K.op("dve", lambda e, i=i, pb=pb: e.tensor_copy(out=Vs[:, i, :], in_=ps[pb][:]), r=[psb[pb]], w=[b_Vs[i]])
            else:
                b = cnt["hb"] % 2
                cnt["hb"] += 1
                K.op("act", lambda e, b=b, pb=pb: e.activation(out=hb[b][:], in_=ps[pb][:], func=AF.Copy, scale=0.125),
                     r=[psb[pb]], w=[b_hb[b]])
                if i == 16:
                    to_T(hb[b], b_hb[b], None, samp["sb"]["b"], i, 4 + i % 2, dst_ap=samp["sb"]["QT"][:])
                else:
                    to_T(hb[b], b_hb[b], QsT, b_QsT[i], i, 4 + i % 2)

    o_sT = sb("o_sT", [128, 4, NTOK], BF16)
    b_osT = Buf("o_sT")
    negones = sb("negones", [128, 128], BF16)
    ones_bf = sb("ones_bf", [128, 128], BF16)
    b_ones = Buf("ones")
    K.op("pool", lambda e: e.memset(negones[:], -1.0), w=[b_ones])
    K.op("pool", lambda e: e.memset(ones_bf[:], 1.0), w=[b_ones])
    e_sb = [xt[j][:, 0:512] for j in range(2)]
    sp_sb = [sb(f"sp_sb{j}", [128, 512], BF16) for j in range(2)]
    A_sb = [junk[:, 512 * j:512 * (j + 1)] for j in range(2)]
    srun = [sb(f"srun{j}", [128, 512], BF16) for j in range(3)]
    b_e = b_xt
    b_sp = [Buf(f"sp{j}") for j in range(2)]
    b_A = [Buf(f"A{j}") for j in range(2)]
    b_srun = [Buf(f"srun{j}") for j in range(3)]
    dbg_o = dram("dbg_o", [2, 128, 4, NTOK], BF16, "ExternalOutput")

    steps = []
    for h in range(8):
        for g in range(4):
            for j in range(4 * g + 3, -1, -1):
                steps.append((h, g, j))

    def zmm(P, pbank, h, g, j, q0, last):
        c, p0 = h // 2, (h % 2) * 64
        K.op("pe", lambda e: e.matmul(P[:, q0:512], lhsT=KsT[p0:p0 + 64, c, j * 128:(j + 1) * 128],
                                      rhs=QsT[p0:p0 + 64, c, g * 512 + q0:(g + 1) * 512], start=True,
                                      stop=(last and j < 4 * g)),
             r=[b_KsT[j]] + [b_QsT[4 * g + t] for t in range(4)], w=[psb[pbank]])
        if j >= 4 * g:
            K.op("pe", lambda e: e.matmul(P[:, q0:q0 + 128], lhsT=ident, rhs=msb, start=False, stop=last),
                 r=[b_cbf], w=[psb[pbank]])

    def sb_X(t):
        h, g, j = steps[t]
        a = t % 2
        q0 = max(j - 4 * g, 0) * 128
        first = (j == 4 * g + 3)
        zmm(ps[a], a, h, g, j, q0, True)
        K.op("act", lambda e: e.activation(out=e_sb[a][:, q0:], in_=ps[a][:, q0:], func=AF.Exp), r=[psb[a]], w=[b_e[a]])
        K.op("act", lambda e: e.activation(out=sp_sb[a][:, q0:], in_=e_sb[a][:, q0:], func=AF.Ln, bias=1.0),
             r=[b_e[a]], w=[b_sp[a]])
        cu, nx = t % 3, (t + 1) % 3
        if first:
            K.op("pool", lambda e: e.memset(srun[nx][:, 0:q0], 0.0), w=[b_srun[nx]])
            K.op("pool", lambda e: e.tensor_copy(out=srun[nx][:, q0:], in_=sp_sb[a][:, q0:]), r=[b_sp[a]], w=[b_srun[nx]])
        elif j > 0:
            if q0 > 0:
                K.op("pool", lambda e: e.tensor_copy(out=srun[nx][:, 0:q0], in_=srun[cu][:, 0:q0]), r=[b_srun[cu]], w=[b_srun[nx]])
            K.op("pool", lambda e: e.tensor_tensor(out=srun[nx][:, q0:], in0=srun[cu][:, q0:], in1=sp_sb[a][:, q0:], op=ALU.add),
                 r=[b_srun[cu], b_sp[a]], w=[b_srun[nx]])

    def sb_Y(t):
        h, g, j = steps[t]
        a = t % 2
        c, p0 = h // 2, (h % 2) * 64
        q0 = max(j - 4 * g, 0) * 128
        first = (j == 4 * g + 3)
        P2 = ps[2 + a]
        zmm(P2, 2 + a, h, g, j, q0, False)
        K.op("pe", lambda e: e.matmul(P2[:, q0:], lhsT=ntri, rhs=sp_sb[a][:, q0:], start=False, stop=first),
             r=[b_sp[a], b_cbf], w=[psb[2 + a]])
        if not first:
            K.op("pe", lambda e: e.matmul(P2[:, q0:], lhsT=negones[:], rhs=srun[t % 3][:, q0:], start=False, stop=True),
                 r=[b_srun[t % 3], b_ones], w=[psb[2 + a]])
        K.op("act", lambda e: e.activation(out=A_sb[a][:, q0:], in_=P2[:, q0:], func=AF.Exp), r=[psb[2 + a]], w=[b_A[a]])
        ob = 4 + (h * 4 + g) % 2
        K.op("pe", lambda e: e.matmul(ps[ob][:, q0:], lhsT=Vs[:, j, c * 128:(c + 1) * 128], rhs=A_sb[a][:, q0:],
                                      start=first, stop=(j == 0), skip_group_check=True),
             r=[b_A[a], b_Vs[j]], w=[psb[ob]])
        if j == 0:
            K.op("dve", lambda e: e.tensor_copy(out=o_sT[p0:p0 + 64, c, g * 512:(g + 1) * 512], in_=ps[ob][p0:p0 + 64, :]),
                 r=[psb[ob]], w=[b_osT])

    if STAGE >= 2:
        sb_X(0)
        for t in range(len(steps)):
            if t + 1 < len(steps):
                sb_X(t + 1)
            sb_Y(t)
        K.dma("sp", lambda e: e.dma_start(out=dbg_o[0, :, :, 0:S], in_=o_sT[:, :, 0:S]), r=[b_osT])

    def view(t, dt, off, shape):
        nd = len(t.shape)
        f = t[:]
        if nd > 2:
            names = " ".join(f"a{i}" for i in range(nd - 1))
            f = f.rearrange(f"p {names} -> p ({names})")
        isz = {F32: 4, I32: 4, BF16: 2}[dt]
        osz = {F32: 4, I32: 4, BF16: 2}[t.dtype]
        if isz != osz:
            f = f.bitcast(dt)
        n = 1
        for d_ in shape[1:]:
            n *= d_
        v = f[0:shape[0], off // isz:off // isz + n]
        if len(shape) > 2:
            names = " ".join(f"b{i}" for i in range(len(shape) - 1))
            kw = {f"b{i}": shape[i + 1] for i in range(len(shape) - 1)}
            v = v.rearrange(f"p ({names}) -> p {names}", **kw)
        return v

    offs = sb("offs", [128, 256], I32)
    ptb = xn[0][:, 0:512].bitcast(I32)
    iot = sb("iot", [128, 1], I32)
    b_offs = Buf("offs")
    K.dma("sp", lambda e: e.dma_start(out=ptb[:], in_=ptab.partition_broadcast(128)), r=[b_xn[0]], w=[b_offs, b_xn[0]])
    K.op("pool", lambda e: e.iota(iot[:], pattern=[[0, 1]], base=0, channel_multiplier=1), w=[b_offs])
    K.op("pool", lambda e: e.tensor_scalar(out=offs[:], in0=ptb[:], scalar1=128.0, scalar2=None, op0=ALU.mult), r=[b_offs], w=[b_offs])
    K.op("pool", lambda e: e.tensor_tensor(out=offs[:], in0=offs[:], in1=iot[:].broadcast_to([128, 256]), op=ALU.add), r=[b_offs], w=[b_offs])

    def sample_attn(br):
        K.barrier()
        is_sb = (br == "sb")
        sm = samp[br]
        poolK, poolV = cache[br + "_k"], cache[br + "_v"]
        o_T, b_oT = (o_sT, b_osT) if is_sb else (o_mT, b_omT)
        Kg = view(QsT, BF16, 0, [128, 16, 512])
        Vg = view(KsT, BF16, 0, [128, 16, 512])
        KTs = view(Vs, BF16, 0, [128, 4, 2048])
        Qbdf = view(wst[0], F32, 0, [128, 4, 16, 32])
        Qbd = view(wst[1], BF16, 0, [128, 4, 16, 32])
        IndN = view(wst[1], BF16, 4096, [128, 16, 128])
        e_s = view(wb[0], F32, 0, [128, 512])
        sp_s = view(wb[0], BF16, 2048, [128, 512])
        A_s = view(wb[0], BF16, 3072, [128, 512])
        en = view(wb[0], F32, 4096, [128, 512])
        spn = view(wb[0], BF16, 6144, [128, 512])
        An = view(wb[0], BF16, 7168, [128, 512])
        m01 = view(wb[1], F32, 0, [128, 512])
        kmTs = view(wb[1], F32, 2048, [128, 4, 8])
        sblk_s = view(wb[1], F32, 2304, [128, 8])
        mx_s = view(wb[1], F32, 2432, [128, 8])
        b01_s = view(wb[1], BF16, 2560, [128, 8])
        biasTs = view(wb[1], BF16, 2624, [128, 32])
        rl_s = view(wb[1], F32, 4096, [128, 512])
        tmpn = view(wb[1], F32, 6144, [128, 512])
        bq, bm, bn, bkg, bvg, bkt = Buf("s_q"), Buf("s_m"), Buf("s_n"), Buf("s_kg"), Buf("s_vg"), Buf("s_kt")
        be, bsp, bA, bkm, bsb_, bbt, brl = Buf("s_e"), Buf("s_sp"), Buf("s_A"), Buf("s_km"), Buf("s_sblk"), Buf("s_bt"), Buf("s_rl")
        K.dma("sp", lambda e: e.dma_start(out=m01, in_=m01_d[0 if is_sb else 1]), w=[bm])
        K.op("pool", lambda e: e.memset(Qbd, 0.0), w=[bq])
        if not is_sb:
            K.op("pool", lambda e: e.memset(Qbdf, 0.0), w=[bq])
            K.op("pool", lambda e: e.memset(IndN[0:8], 0.0), w=[bq])
            K.op("dve", lambda e: e.tensor_copy(out=IndN[0:8].rearrange("p (n u) m -> p n (u m)", u=2),
                                                in_=id30k[0:8, 0:8].unsqueeze(2).broadcast_to([8, 8, 256])), r=[b_selm], w=[bq])
        for h in range(8):
            c, p0 = h // 2, (h % 2) * 64
            K.op("dve", lambda e, c=c, p0=p0, h=h: e.tensor_copy(out=Qbd[p0:p0 + 64, c, :, h * 4:(h + 1) * 4],
                                                               in_=sm["QT"][p0:p0 + 64, c, 0:64].rearrange("p (s t) -> p s t", t=4)),
                 r=[sm["b"]], w=[bq])
            if not is_sb:
                K.op("dve", lambda e, c=c, p0=p0, h=h: e.tensor_copy(out=Qbdf[p0:p0 + 64, c, :, h * 4:(h + 1) * 4],
                                                                   in_=sQTf[p0:p0 + 64, c, 0:64].rearrange("p (s t) -> p s t", t=4)),
                     r=[b_sQTf], w=[bq])
        def znew(bank):
            for s_ in range(16):
                for c in range(4):
                    K.op("pe", lambda e, s_=s_, c=c: e.matmul(ps[bank][:, s_ * 32:(s_ + 1) * 32], lhsT=sm["KT"][:, c, :], rhs=Qbd[:, c, s_, :],
                                                            start=(s_ == 0 and c == 0), stop=False, skip_group_check=True),
                         r=[sm["b"], bq], w=[psb[bank]])
        znew(4)
        K.op("act", lambda e: e.activation(out=en, in_=ps[4][:], func=AF.Exp), r=[psb[4]], w=[bn])
        K.op("dve", lambda e: e.tensor_tensor(out=en, in0=en, in1=m01, op=ALU.mult), r=[bn, bm], w=[bn])
        if is_sb:
            K.op("act", lambda e: e.activation(out=spn, in_=en, func=AF.Ln, bias=1.0), r=[bn], w=[bn])
            znew(5)
            K.op("pe", lambda e: e.matmul(ps[5][:], lhsT=ntri, rhs=spn, start=False, stop=True, skip_group_check=True), r=[bn, b_cbf], w=[psb[5]])
            K.op("act", lambda e: e.activation(out=tmpn, in_=ps[5][:], func=AF.Exp), r=[psb[5]], w=[bn])
            K.op("dve", lambda e: e.tensor_tensor(out=An, in0=tmpn, in1=m01, op=ALU.mult), r=[bn, bm], w=[bn])
        else:
            K.op("dve", lambda e: e.tensor_copy(out=An, in_=en), r=[bn], w=[bn])
        first_o = [True]
        for s_ in range(NSEQ):
            for pg in range(16):
                col = s_ * 16 + pg
                K.dma("pool", lambda e, pg=pg, col=col: e.indirect_dma_start(
                    out=Kg[:, pg, :], out_offset=None, in_=poolK[:, :],
                    in_offset=bass.IndirectOffsetOnAxis(ap=offs[:, col:col + 1], axis=0)), r=[b_offs], w=[bkg], sw=True)
                K.dma("pool", lambda e, pg=pg, col=col: e.indirect_dma_start(
                    out=Vg[:, pg, :], out_offset=None, in_=poolV[:, :],
                    in_offset=bass.IndirectOffsetOnAxis(ap=offs[:, col:col + 1], axis=0)), r=[b_offs], w=[bvg], sw=True)
            for pg in range(16):
                bk = 6 + pg % 2
                pst = ps[bk].bitcast(BF16)
                for c in range(4):
                    K.op("pe", lambda e, pg=pg, c=c, pst=pst: e.transpose(out=pst[:, c * 128:(c + 1) * 128], in_=Kg[:, pg, c * 128:(c + 1) * 128], identity=ident),
                         r=[bkg, b_cbf], w=[psb[bk]])
                if pg % 2 == 0:
                    K.op("act", lambda e, pg=pg, pst=pst: e.activation(out=KTs[:, :, pg * 128:(pg + 1) * 128], in_=pst[:, 0:512].rearrange("p (c t) -> p c t", c=4), func=AF.Copy),
                         r=[psb[bk]], w=[bkt])
                else:
                    K.op("dve", lambda e, pg=pg, pst=pst: e.tensor_copy(out=KTs[:, :, pg * 128:(pg + 1) * 128], in_=pst[:, 0:512].rearrange("p (c t) -> p c t", c=4)),
                         r=[psb[bk]], w=[bkt])
            if not is_sb:
                for c in range(4):
                    for n in range(8):
                        for u in range(2):
                            K.op("pe", lambda e, c=c, n=n, u=u: e.matmul(ps[5][:, c * 8 + n:c * 8 + n + 1], lhsT=Kg[:, 2 * n + u, c * 128:(c + 1) * 128],
                                                                       rhs=ones256b[:, 0:1], start=(u == 0), stop=(u == 1)),
                                 r=[bkg, b_ones], w=[psb[5]])
                K.op("dve", lambda e: e.tensor_copy(out=kmTs, in_=ps[5][:, 0:32].rearrange("p (c n) -> p c n", c=4)), r=[psb[5]], w=[bkm])
                for c in range(4):
                    K.op("pe", lambda e, c=c, s_=s_: e.matmul(ps[5][0:32, 32:40], lhsT=Qbdf[:, c, s_, :], rhs=kmTs[:, c, :], start=(c == 0), stop=(c == 3)),
                         r=[bq, bkm], w=[psb[5]])
                K.op("dve", lambda e: e.tensor_copy(out=sblk_s[0:32], in_=ps[5][0:32, 32:40]), r=[psb[5]], w=[bsb_])
                K.op("dve", lambda e: e.max(out=mx_s[0:32], in_=sblk_s[0:32]), r=[bsb_], w=[bsb_])
                K.op("dve", lambda e: e.tensor_scalar(out=b01_s[0:32], in0=sblk_s[0:32], scalar1=mx_s[0:32, 2:3], scalar2=1.0,
                                                      op0=ALU.is_ge, op1=ALU.subtract), r=[bsb_], w=[bsb_])
                pst5 = ps[5].bitcast(BF16)
                K.op("pe", lambda e: e.transpose(out=pst5[0:8, 128:160], in_=b01_s[0:32], identity=ident[0:32, 0:32]), r=[bsb_, b_cbf], w=[psb[5]])
                K.op("dve", lambda e: e.tensor_copy(out=biasTs[0:8], in_=pst5[0:8, 128:160]), r=[psb[5]], w=[bbt])

            def zs(bank):
                for pg in range(16):
                    for c in range(4):
                        K.op("pe", lambda e, pg=pg, c=c, s_=s_: e.matmul(ps[bank][:, pg * 32:(pg + 1) * 32], lhsT=KTs[:, c, pg * 128:(pg + 1) * 128], rhs=Qbd[:, c, s_, :],
                                                                start=(pg == 0 and c == 0), stop=False, skip_group_check=True),
                             r=[bkt, bq], w=[psb[bank]])
            zs(0)
            if is_sb:
                K.op("act", lambda e: e.activation(out=e_s, in_=ps[0][:], func=AF.Exp), r=[psb[0]], w=[be])
                K.op("act", lambda e: e.activation(out=sp_s, in_=e_s, func=AF.Ln, bias=1.0), r=[be], w=[bsp])
                zs(1)
                K.op("pe", lambda e: e.matmul(ps[1][:], lhsT=ntri, rhs=sp_s, start=False, stop=False, skip_group_check=True), r=[bsp, b_cbf], w=[psb[1]])
                for pg in range(1, 16):
                    K.op("pe", lambda e, pg=pg: e.matmul(ps[1][:, 0:pg * 32].rearrange("p (a b) -> p a b", b=32), lhsT=negones[:],
                                                       rhs=sp_s[:, pg * 32:(pg + 1) * 32].unsqueeze(1).broadcast_to([128, pg, 32]),
                                                       start=False, stop=False, skip_group_check=True), r=[bsp, b_ones], w=[psb[1]])
                K.op("pe", lambda e, s_=s_: e.matmul(ps[1][:].rearrange("p (a b) -> p a b", b=32), lhsT=negones[:],
                                              rhs=spn[:, s_ * 32:(s_ + 1) * 32].unsqueeze(1).broadcast_to([128, 16, 32]),
                                              start=False, stop=True, skip_group_check=True), r=[bn, b_ones], w=[psb[1]])
                K.op("act", lambda e: e.activation(out=A_s, in_=ps[1][:], func=AF.Exp), r=[psb[1]], w=[bA])
            else:
                for pg in range(16):
                    K.op("pe", lambda e, pg=pg: e.matmul(ps[0][:, pg * 32:(pg + 1) * 32], lhsT=IndN[0:8, pg, :], rhs=biasTs[0:8, :],
                                                       start=False, stop=(pg == 15), skip_group_check=True), r=[bq, bbt], w=[psb[0]])
                K.op("act", lambda e: e.activation(out=A_s, in_=ps[0][:], func=AF.Exp), r=[psb[0]], w=[bA])
            for pg in range(17):
                for h in range(8):
                    c = h // 2
                    if pg < 16:
                        lh, rh, rr = Vg[:, pg, c * 128:(c + 1) * 128], A_s[:, pg * 32 + h * 4:pg * 32 + h * 4 + 4], [bvg, bA]
                    else:
                        lh, rh, rr = sm["V"][:, c * 128:(c + 1) * 128], An[:, s_ * 32 + h * 4:s_ * 32 + h * 4 + 4], [sm["b"], bn]
                    K.op("pe", lambda e, lh=lh, rh=rh, h=h, st=first_o[0], s_=s_: e.matmul(ps[2][:, s_ * 32 + h * 4:s_ * 32 + h * 4 + 4], lhsT=lh, rhs=rh,
                                                                                  start=st, stop=False, skip_group_check=True), r=rr, w=[psb[2]])
                    first_o[0] = False
                if not is_sb:
                    rh = A_s[:, pg * 32:(pg + 1) * 32] if pg < 16 else An[:, s_ * 32:(s_ + 1) * 32]
                    K.op("pe", lambda e, rh=rh, st=(s_ == 0 and pg == 0), s_=s_: e.matmul(ps[3][:, s_ * 32:(s_ + 1) * 32], lhsT=ones_bf[:], rhs=rh,
                                                                                 start=st, stop=False, skip_group_check=True),
                         r=[bA, bn, b_ones], w=[psb[3]])
        ncol = NSEQ * 32
        if not is_sb:
            K.op("dve", lambda e: e.reciprocal(out=rl_s[:, 0:ncol], in_=ps[3][:, 0:ncol]), r=[psb[3]], w=[brl])
        for h in range(8):
            c, p0 = h // 2, (h % 2) * 64
            dst = o_T[p0:p0 + 64, c, S:S + 4 * NSEQ].rearrange("p (s t) -> p s t", t=4)
            src = ps[2][p0:p0 + 64, 0:ncol].rearrange("p (s x) -> p s x", x=32)[:, :, h * 4:(h + 1) * 4]
            if is_sb:
                K.op("dve", lambda e, dst=dst, src=src: e.tensor_copy(out=dst, in_=src), r=[psb[2]], w=[b_oT])
            else:
                rsrc = rl_s[p0:p0 + 64, 0:ncol].rearrange("p (s x) -> p s x", x=32)[:, :, h * 4:(h + 1) * 4]
                K.op("dve", lambda e, dst=dst, src=src, rsrc=rsrc: e.tensor_tensor(out=dst, in0=src, in1=rsrc, op=ALU.mult), r=[psb[2], brl], w=[b_oT])
        K.op("pool", lambda e: e.memset(o_T[:, :, S + 4 * NSEQ:NTOK], 0.0), w=[b_oT])
        K.barrier()

    ones256b = sb("ones256b", [128, 2], BF16)
    K.op("pool", lambda e: e.memset(ones256b[:], 1.0 / 256.0), w=[b_ones])
    if STAGE >= 4 and os.environ.get("KBR", "sb") in ("sb", "both") or (STAGE >= 4 and not SIM):
        sample_attn("sb")

    QmT, KmT, Vm = QsT, KsT, Vs
    b_QmT, b_KmT, b_Vm = b_QsT, b_KsT, b_Vs
    kmf = [sb(f"kmf{j}", [128, 512], F32) for j in range(2)]
    b_kmf = [Buf(f"kmf{j}") for j in range(2)]
    rot = sb("rot", [128, 4, 64], F32)
    b_rot = Buf("rot")
    ones256 = sb("ones256", [128, 2], F32)
    identf = cst[:, C_ID:C_ID + 128]
    kmT = sb("kmT", [128, 4, 8], F32)
    b_kmT = Buf("kmT")
    biasT = sb("biasT", [128, S], BF16)
    b_biasT = [Buf(f"biasT{i}") for i in range(16)]
    b_selm = Buf("selm")
    qTf = sb("qTf", [128, 4, 128], F32)
    b_qTf = Buf("qTf")
    sblk = sb("sblk", [128, 64], F32)
    mx8 = sb("mx8", [128, 64], F32)
    bias01 = sb("bias01", [128, 64], BF16)
    b_sblk, b_mx8, b_bias01 = Buf("sblk"), Buf("mx8"), Buf("bias01")
    id30k = sb("id30k", [64, 64], BF16)

    def rotary(t, b_t, i):
        tv = t[:].rearrange("p (h d) -> p h d", h=8)
        x1, x2 = tv[:, :, 0:8], tv[:, :, 8:16]
        cs = cosT[:, i, :].unsqueeze(1).broadcast_to([128, 8, 8])
        sn = sinT[:, i, :].unsqueeze(1).broadcast_to([128, 8, 8])
        K.op("dve", lambda e: e.tensor_tensor(out=rot[:, 0, :].rearrange("p (h f) -> p h f", h=8), in0=x1, in1=cs, op=ALU.mult), r=[b_t, b_cst], w=[b_rot])
        K.op("dve", lambda e: e.tensor_tensor(out=rot[:, 1, :].rearrange("p (h f) -> p h f", h=8), in0=x2, in1=sn, op=ALU.mult), r=[b_t, b_cst], w=[b_rot])
        K.op("dve", lambda e: e.tensor_tensor(out=rot[:, 2, :].rearrange("p (h f) -> p h f", h=8), in0=x1, in1=sn, op=ALU.mult), r=[b_t, b_cst], w=[b_rot])
        K.op("dve", lambda e: e.tensor_tensor(out=rot[:, 3, :].rearrange("p (h f) -> p h f", h=8), in0=x2, in1=cs, op=ALU.mult), r=[b_t, b_cst], w=[b_rot])
        K.op("dve", lambda e: e.tensor_tensor(out=x1, in0=rot[:, 0, :].rearrange("p (h f) -> p h f", h=8),
                                              in1=rot[:, 1, :].rearrange("p (h f) -> p h f", h=8), op=ALU.subtract), r=[b_rot], w=[b_t])
        K.op("dve", lambda e: e.tensor_tensor(out=x2, in0=rot[:, 2, :].rearrange("p (h f) -> p h f", h=8),
                                              in1=rot[:, 3, :].rearrange("p (h f) -> p h f", h=8), op=ALU.add), r=[b_rot], w=[b_t])

    if STAGE >= 3:
        K.op("pool", lambda e: e.memset(ones256[:], 1.0 / 256.0), w=[b_ones])
        K.op("dve", lambda e: e.tensor_scalar(out=id30k[:], in0=cst[0:64, C_ID:C_ID + 64], scalar1=30000.0, scalar2=None, op0=ALU.mult),
             r=[b_cst], w=[b_selm])
        K.op("pool", lambda e: e.memset(biasT[0:64, 0:256], 0.0), w=[b_biasT[0], b_biasT[1]])
        K.op("pool", lambda e: e.memset(biasT[64:128, :], 0.0), w=[b_selm])
        for (c, kind) in ((4, "k"), (5, "v"), (3, "q")):
            j = load_w(c)
            for i in range(NT):
                pb = project(c, i, j)
                if kind == "v":
                    a = cnt["hf"] % 2
                    cnt["hf"] += 1
                    K.op("act", lambda e, a=a, pb=pb: e.activation(out=hf[a][:], in_=ps[pb][:], func=AF.Copy), r=[psb[pb]], w=[b_hf[a]])
                    kv_store(3, i, hf[a], b_hf[a])
                    if i == 16:
                        K.op("dve", lambda e, pb=pb: e.tensor_copy(out=samp["mb"]["V"][:], in_=ps[pb][:]), r=[psb[pb]], w=[samp["mb"]["b"]])
                    else:
                        K.op("dve", lambda e, i=i, pb=pb: e.tensor_copy(out=Vm[:, i, :], in_=ps[pb][:]), r=[psb[pb]], w=[b_Vm[i]])
                    continue
                a = i % 2
                K.op("act", lambda e, a=a, pb=pb, kind=kind: e.activation(out=kmf[a][:], in_=ps[pb][:], func=AF.Copy,
                                                                         scale=(1.0 if kind == "k" else 0.125)),
                     r=[psb[pb]], w=[b_kmf[a]])
                rotary(kmf[a], b_kmf[a], i)
                b = cnt["hb"] % 2
                cnt["hb"] += 1
                K.op("pool", lambda e, a=a, b=b: e.tensor_copy(out=hb[b][:], in_=kmf[a][:]), r=[b_kmf[a]], w=[b_hb[b]])
                if kind == "k":
                    kv_store(2, i, kmf[a], b_kmf[a])
                    if i == 16:
                        to_T(hb[b], b_hb[b], None, samp["mb"]["b"], i, 4 + i % 2, dst_ap=samp["mb"]["KT"][:])
                    else:
                        to_T(hb[b], b_hb[b], KmT, b_KmT[i], i, 4 + i % 2)
                    if i < 16 and i % 2 == 1:
                        n = i // 2
                        for ch in range(4):
                            for u in range(2):
                                src = kmf[1 - a] if u == 0 else kmf[a]
                                K.op("pe", lambda e, ch=ch, u=u, src=src, n=n: e.matmul(ps[6][:, ch * 8 + n:ch * 8 + n + 1],
                                                                                    lhsT=src[:, ch * 128:(ch + 1) * 128],
                                                                                    rhs=ones256[:, 0:1], start=(u == 0), stop=(u == 1)),
                                     r=[b_kmf[0], b_kmf[1], b_ones], w=[psb[6]])
                        if i == 15:
                            K.op("dve", lambda e: e.tensor_copy(out=kmT[:], in_=ps[6][:, 0:32].rearrange("p (c n) -> p c n", c=4)),
                                 r=[psb[6]], w=[b_kmT])
                else:
                    if i == 16:
                        to_T(hb[b], b_hb[b], None, samp["mb"]["b"], i, 4 + i % 2, dst_ap=samp["mb"]["QT"][:])
                        for cc in range(4):
                            K.op("pe", lambda e, cc=cc, a=a: e.transpose(out=ps[7][:, cc * 128:(cc + 1) * 128], in_=kmf[a][:, cc * 128:(cc + 1) * 128],
                                                                      identity=identf), r=[b_kmf[a], b_cst], w=[psb[7]])
                        K.op("act", lambda e: e.activation(out=sQTf[:], in_=ps[7][:].rearrange("p (c t) -> p c t", c=4), func=AF.Copy),
                             r=[psb[7]], w=[b_sQTf])
                    else:
                        to_T(hb[b], b_hb[b], QmT, b_QmT[i], i, 4 + i % 2)
                    own = i // 2
                    if i < 16 and own >= 1:
                        for cc in range(4):
                            K.op("pe", lambda e, cc=cc, a=a: e.transpose(out=ps[7][:, cc * 128:(cc + 1) * 128], in_=kmf[a][:, cc * 128:(cc + 1) * 128],
                                                                      identity=identf), r=[b_kmf[a], b_cst], w=[psb[7]])
                        K.op("act", lambda e: e.activation(out=qTf[:], in_=ps[7][:].rearrange("p (c t) -> p c t", c=4), func=AF.Copy),
                             r=[psb[7]], w=[b_qTf])
                        for h in range(8):
                            c2, p0 = h // 2, (h % 2) * 64
                            K.op("pe", lambda e, h=h, c2=c2, p0=p0: e.matmul(ps[6][:, 64 + h * 8:64 + h * 8 + 8], lhsT=qTf[p0:p0 + 64, c2, :],
                                                                         rhs=kmT[p0:p0 + 64, c2, :], start=True, stop=True),
                                 r=[b_qTf, b_kmT], w=[psb[6]])
                        K.op("dve", lambda e: e.tensor_copy(out=sblk[:], in_=ps[6][:, 64:128]), r=[psb[6]], w=[b_sblk])
                        if own < 8:
                            K.op("dve", lambda e, own=own: e.memset(sblk[:].rearrange("p (h n) -> p h n", h=8)[:, :, own:8], -1e30), w=[b_sblk])
                        for h in range(8):
                            K.op("dve", lambda e, h=h: e.max(out=mx8[:, h * 8:(h + 1) * 8], in_=sblk[:, h * 8:(h + 1) * 8]), r=[b_sblk], w=[b_mx8])
                        ti = min(own, 3) - 1
                        for h in range(8):
                            K.op("dve", lambda e, h=h, ti=ti: e.tensor_scalar(out=bias01[:, h * 8:(h + 1) * 8], in0=sblk[:, h * 8:(h + 1) * 8],
                                                                          scalar1=mx8[:, h * 8 + ti:h * 8 + ti + 1], scalar2=1.0,
                                                                          op0=ALU.is_ge, op1=ALU.subtract),
                                 r=[b_sblk, b_mx8], w=[b_bias01])
                        if own < 8:
                            K.op("dve", lambda e, own=own: e.memset(bias01[:].rearrange("p (h n) -> p h n", h=8)[:, :, own:own + 1], 0.0), w=[b_bias01])
                        pstb = ps[7].bitcast(BF16)
                        K.op("pe", lambda e: e.transpose(out=pstb[0:64, 0:128], in_=bias01[:], identity=ident), r=[b_bias01, b_cbf], w=[psb[7]])
                        K.op("dve", lambda e, i=i: e.tensor_copy(out=biasT[0:64, i * 128:(i + 1) * 128], in_=pstb[0:64, 0:128]), r=[psb[7]], w=[b_biasT[i]])

    o_mT = sb("o_mT", [128, 4, NTOK], BF16)
    b_omT = Buf("o_mT")
    rl = xt[1][:, 0:512]
    b_rl = b_xt[1]

    def mb_P(t):
        h, g, j = steps_m[t]
        a = t % 2
        c, p0 = h // 2, (h % 2) * 64
        q0 = max(j - 4 * g, 0) * 128
        r_ = h * 8 + j // 2
        K.op("pe", lambda e: e.matmul(ps[a][:, q0:512], lhsT=KmT[p0:p0 + 64, c, j * 128:(j + 1) * 128],
                                      rhs=QmT[p0:p0 + 64, c, g * 512 + q0:(g + 1) * 512], start=True, stop=False),
             r=[b_KmT[j]] + [b_QmT[4 * g + u] for u in range(4)], w=[psb[a]])
        K.op("pe", lambda e: e.matmul(ps[a][:, q0:512], lhsT=selm[r_ // 32][:, r_ % 32, :], rhs=biasT[:, g * 512 + q0:(g + 1) * 512],
                                      start=False, stop=(j < 4 * g)),
             r=[b_wst[r_ // 32], b_selm] + [b_biasT[4 * g + u] for u in range(4)], w=[psb[a]])
        if j >= 4 * g:
            K.op("pe", lambda e: e.matmul(ps[a][:, q0:q0 + 128], lhsT=ident, rhs=mmb, start=False, stop=True), r=[b_cbf], w=[psb[a]])
        K.op("act", lambda e: e.activation(out=A_sb[a][:, q0:], in_=ps[a][:, q0:], func=AF.Exp), r=[psb[a]], w=[b_A[a]])

    def mb_AV(t):
        h, g, j = steps_m[t]
        a = t % 2
        c, p0 = h // 2, (h % 2) * 64
        q0 = max(j - 4 * g, 0) * 128
        ob = 4 + (h * 4 + g) % 2
        lb = 2 + (h * 4 + g) % 2
        first, last = (j == 0), (j == 4 * g + 3)
        K.op("pe", lambda e: e.matmul(ps[ob][:, q0:], lhsT=Vm[:, j, c * 128:(c + 1) * 128], rhs=A_sb[a][:, q0:], start=first, stop=last),
             r=[b_A[a], b_Vm[j]], w=[psb[ob]])
        K.op("pe", lambda e: e.matmul(ps[lb][:, q0:], lhsT=ones_bf[:], rhs=A_sb[a][:, q0:], start=first, stop=last),
             r=[b_A[a], b_ones], w=[psb[lb]])
        if last:
            K.op("dve", lambda e: e.reciprocal(out=rl[p0:p0 + 64, :], in_=ps[lb][p0:p0 + 64, :]), r=[psb[lb]], w=[b_rl])
            K.op("dve", lambda e: e.tensor_tensor(out=o_mT[p0:p0 + 64, c, g * 512:(g + 1) * 512], in0=ps[ob][p0:p0 + 64, :],
                                                  in1=rl[p0:p0 + 64, :], op=ALU.mult), r=[psb[ob], b_rl], w=[b_omT])

    selm = [wst[u][:].rearrange("p a b -> p (a b)").bitcast(BF16).rearrange("p (r m) -> p r m", r=32) for u in range(2)]
    if STAGE >= 3:
        for u in range(2):
            K.op("dve", lambda e, u=u: e.tensor_copy(out=selm[u][0:64], in_=id30k[:, 32 * u:32 * u + 32].unsqueeze(2).broadcast_to([64, 32, 128])),
                 r=[b_selm], w=[b_wst[u]])
            K.op("dve", lambda e, u=u: e.memset(selm[u][64:128], 0.0), w=[b_wst[u]])
    steps_m = []
    for h in range(8):
        for g in range(4):
            for j in range(0, 4 * g + 4):
                steps_m.append((h, g, j))
    if STAGE >= 3 and os.environ.get('KB2', '1') == '1':
        mb_P(0)
        for t in range(len(steps_m)):
            if t + 1 < len(steps_m):
                mb_P(t + 1)
            mb_AV(t)
        K.dma("sp", lambda e: e.dma_start(out=dbg_o[1, :, :, 0:S], in_=o_mT[:, :, 0:S]), r=[b_omT])

    if STAGE >= 4 and (not SIM or os.environ.get("KBR", "sb") in ("mb", "both")):
        sample_attn("mb")
    if STAGE >= 4:
        if SIM and os.environ.get("KBR", "sb") == "sb":
            K.op("pool", lambda e: e.memset(o_mT[:, :, S:NTOK], 0.0), w=[b_omT])
        if SIM and os.environ.get("KBR", "sb") == "mb":
            K.op("pool", lambda e: e.memset(o_sT[:, :, S:NTOK], 0.0), w=[b_osT])
        K.dma("sp", lambda e: e.dma_start(out=dbg_o[0, :, :, S:NTOK], in_=o_sT[:, :, S:NTOK]), r=[b_osT])
        K.dma("sp", lambda e: e.dma_start(out=dbg_o[1, :, :, S:NTOK], in_=o_mT[:, :, S:NTOK]), r=[b_omT])

    if STAGE >= 5:
        K.barrier()
        mT = [QsT, KsT]
        bm_T = [Buf(f"mT{i}") for i in range(NT)]
        Wps_h = view(Vs, BF16, 0, [128, 4, 512])
        Wpm_h = view(Vs, BF16, 4096, [128, 4, 512])
        sg = [view(Vs, F32, 8192, [128, 512]), view(Vs, F32, 10240, [128, 512])]
        mm_ = [view(Vs, F32, 12288, [128, 512]), view(Vs, F32, 14336, [128, 512])]
        bWp, bsg, bmm = Buf("Wp"), [Buf("sg0"), Buf("sg1")], [Buf("mm0"), Buf("mm1")]
        bwst2, bwb2 = [Buf("wst0c"), Buf("wst1c")], [Buf("wb0c"), Buf("wb1c")]

        def load_any(src_halves, dsts, bdst):
            for hh, (src, dst) in enumerate(zip(src_halves, dsts)):
                u = hh % 2
                shp = list(src.shape)
                stv = view(wst[u], F32, 0, shp)
                K.dma("sp", lambda e, src=src, stv=stv: e.dma_start(out=stv, in_=src), w=[bwst2[u]])
                K.op("pool", lambda e, dst=dst, stv=stv: e.tensor_copy(out=dst, in_=stv), r=[bwst2[u]], w=[bdst])

        w_ps_v = w_ps.rearrange("(k p) n -> p k n", p=128)
        w_pm_v = w_pm.rearrange("(k p) n -> p k n", p=128)
        w_o_v = w_o.rearrange("(k p) n -> p k n", p=128)
        mb16 = hb
        for cb in range(2):
            load_any([w_in_v[:, 0:4, (6 + cb) * 512:(7 + cb) * 512], w_in_v[:, 4:8, (6 + cb) * 512:(7 + cb) * 512]],
                     [wb[0][:, 0:4, :], wb[0][:, 4:8, :]], bwb2[0])
            load_any([w_in_v[:, 0:4, (8 + cb) * 512:(9 + cb) * 512], w_in_v[:, 4:8, (8 + cb) * 512:(9 + cb) * 512]],
                     [wb[1][:, 0:4, :], wb[1][:, 4:8, :]], bwb2[1])
            load_any([w_ps_v[:, :, cb * 512:(cb + 1) * 512], w_pm_v[:, :, cb * 512:(cb + 1) * 512]], [Wps_h, Wpm_h], bWp)
            for i in range(NT):
                tsl = slice(i * 128, (i + 1) * 128)
                for kc in range(8):
                    K.op("pe", lambda e, kc=kc, tsl=tsl: e.matmul(ps[0][:], lhsT=xnT[:, kc, tsl], rhs=wb[0][:, kc, :], start=(kc == 0), stop=(kc == 7)),
                         r=[b_xnT[i], bwb2[0]], w=[psb[0]])
                for kc in range(8):
                    K.op("pe", lambda e, kc=kc, tsl=tsl: e.matmul(ps[1][:], lhsT=xnT[:, kc, tsl], rhs=wb[1][:, kc, :], start=(kc == 0), stop=(kc == 7)),
                         r=[b_xnT[i], bwb2[1]], w=[psb[1]])
                for c in range(4):
                    K.op("pe", lambda e, c=c, tsl=tsl: e.matmul(ps[2][:], lhsT=o_sT[:, c, tsl], rhs=Wps_h[:, c, :], start=(c == 0), stop=(c == 3)),
                         r=[b_osT, bWp], w=[psb[2]])
                for c in range(4):
                    K.op("pe", lambda e, c=c, tsl=tsl: e.matmul(ps[3][:], lhsT=o_mT[:, c, tsl], rhs=Wpm_h[:, c, :], start=(c == 0), stop=(c == 3)),
                         r=[b_omT, bWp], w=[psb[3]])
                K.op("act", lambda e: e.activation(out=sg[0], in_=ps[0][:], func=AF.Sigmoid), r=[psb[0]], w=[bsg[0]])
                K.op("act", lambda e: e.activation(out=sg[1], in_=ps[1][:], func=AF.Sigmoid), r=[psb[1]], w=[bsg[1]])
                K.op("dve", lambda e: e.tensor_tensor(out=mm_[0], in0=sg[0], in1=ps[2][:], op=ALU.mult), r=[bsg[0], psb[2]], w=[bmm[0]])
                K.op("dve", lambda e: e.tensor_tensor(out=mm_[1], in0=sg[1], in1=ps[3][:], op=ALU.mult), r=[bsg[1], psb[3]], w=[bmm[1]])
                b = i % 2
                K.op("pool", lambda e, b=b: e.tensor_tensor(out=mb16[b][:], in0=mm_[0], in1=mm_[1], op=ALU.add), r=bmm, w=[b_hb[b]])
                to_T(mb16[b], b_hb[b], mT[cb], bm_T[i], i, 4 + i % 2)

        K.barrier()
        load_any([w_o_v[:, 0:4, 0:512], w_o_v[:, 4:8, 0:512]], [wb[0][:, 0:4, :], wb[0][:, 4:8, :]], bwb2[0])
        load_any([w_o_v[:, 0:4, 512:1024], w_o_v[:, 4:8, 512:1024]], [wb[1][:, 0:4, :], wb[1][:, 4:8, :]], bwb2[1])
        bg2 = Buf("gbc2")
        K.dma("sp", lambda e: e.dma_start(out=gbc[:], in_=g_ffn.partition_broadcast(128)), w=[bg2])
        xn2f = view(Vs, F32, 0, [128, D])
        xTf = view(Vs, F32, 4096, [128, 8, 128])
        combT = view(Vs, BF16, 8192, [128, NTOK])
        SelE = view(Vs, BF16, 8192 + 2 * NTOK, [128, 16, 128])
        bx2, bxT, bcT, bSel = Buf("xn2f"), Buf("xTf"), Buf("combT"), Buf("SelE")
        wr = sb("wr", [128, 8, 20], F32)
        brb = sb("brb", [128, 20], F32)
        bwr = Buf("wr")
        K.dma("sp", lambda e: e.dma_start(out=wr[:], in_=w_r.rearrange("(k p) n -> p k n", p=128)), w=[bwr])
        K.dma("sp", lambda e: e.dma_start(out=brb[:], in_=b_r.partition_broadcast(128)), w=[bwr])
        rt = sb("rt", [128, 64], F32)
        comb = sb("comb", [128, 16], F32)
        combb = sb("combb", [128, 16], BF16)
        brt = Buf("rt")
        b_x1d = [Buf(f"x1d{i}") for i in range(NT)]
        K.op("dve", lambda e: e.tensor_copy(out=SelE[0:16], in_=ident[0:16, 0:16].unsqueeze(2).broadcast_to([16, 16, 128])), r=[b_cbf], w=[bSel])
        lgs, ohg, esel, msk1, e2, msk2, c4 = rt[:, 0:20], rt[:, 20:24], rt[:, 24:28], rt[:, 28:32], rt[:, 32:36], rt[:, 36:40], rt[:, 40:44]
        sc = lambda k: rt[:, 48 + k:49 + k]
        eg = rt[:, 44:48]

        def R(fn, eng="dve"):
            K.op(eng, fn, r=[brt], w=[brt])

        for i in range(NT):
            j = i % 2
            tsl = slice(i * 128, (i + 1) * 128)
            K.dma("sp", lambda e, j=j, tsl=tsl: e.dma_start(out=xt[j][:], in_=xin[tsl, :]), w=[b_xt[j]])
            for half in range(2):
                for kc in range(8):
                    K.op("pe", lambda e, kc=kc, half=half, tsl=tsl: e.matmul(ps[half][:], lhsT=mT[kc // 4][:, kc % 4, tsl], rhs=wb[half][:, kc, :],
                                                                           start=(kc == 0), stop=(kc == 7)), r=[bm_T[i], bwb2[half]], w=[psb[half]])
                K.op("dve", lambda e, j=j, half=half: e.tensor_tensor(out=xt[j][:, half * 512:(half + 1) * 512], in0=xt[j][:, half * 512:(half + 1) * 512],
                                                                    in1=ps[half][:], op=ALU.add), r=[b_xt[j], psb[half]], w=[b_xt[j]])
            K.dma("sp", lambda e, j=j, tsl=tsl: e.dma_start(out=y_out[tsl, :], in_=xt[j][:]), r=[b_xt[j]], w=[b_x1d[i]])
            K.op("act", lambda e, i=i, j=j: e.activation(out=junk[:], in_=xt[j][:], func=AF.Square, accum_out=ss[:, i:i + 1]),
                 r=[b_xt[j]], w=[b_junk, b_ss[i]])
            K.op("act", lambda e, i=i: e.activation(out=rs[:, i:i + 1], in_=ss[:, i:i + 1], func=AF.Sqrt, bias=1e-6, scale=1.0 / D), r=[b_ss[i]], w=[b_rs[i]])
            K.op("dve", lambda e, i=i: e.reciprocal(out=rstd[:, i:i + 1], in_=rs[:, i:i + 1]), r=[b_rs[i]], w=[b_rstd[i]])
            K.op("dve", lambda e, i=i, j=j: e.scalar_tensor_tensor(out=xn2f, in0=xt[j][:], scalar=rstd[:, i:i + 1], in1=gbc[:], op0=ALU.mult, op1=ALU.mult),
                 r=[b_xt[j], b_rstd[i], bg2], w=[bx2])
            K.op("pool", lambda e, j=j: e.tensor_copy(out=xn[j][:], in_=xn2f), r=[bx2], w=[b_xn[j]])
            pst = ps[2].bitcast(BF16)
            for kc in range(8):
                K.op("pe", lambda e, kc=kc, j=j, pst=pst: e.transpose(out=pst[:, kc * 128:(kc + 1) * 128], in_=xn[j][:, kc * 128:(kc + 1) * 128], identity=ident),
                     r=[b_xn[j], b_cbf], w=[psb[2]])
            K.op("act", lambda e, tsl=tsl, pst=pst: e.activation(out=xnT[:, :, tsl], in_=pst.rearrange("p (k t) -> p k t", k=8), func=AF.Copy),
                 r=[psb[2]], w=[b_xnT[i]])
            for half in range(2):
                for kk in range(4):
                    kc = half * 4 + kk
                    K.op("pe", lambda e, kc=kc, kk=kk, half=half: e.transpose(out=ps[3 + half][:, kk * 128:(kk + 1) * 128], in_=xn2f[:, kc * 128:(kc + 1) * 128], identity=identf),
                         r=[bx2, b_cst], w=[psb[3 + half]])
                K.op("act" if half == 0 else "dve",
                     (lambda e, half=half: e.activation(out=xTf[:, half * 4:half * 4 + 4, :], in_=ps[3 + half][:].rearrange("p (k t) -> p k t", k=4), func=AF.Copy)) if half == 0 else
                     (lambda e, half=half: e.tensor_copy(out=xTf[:, half * 4:half * 4 + 4, :], in_=ps[3 + half][:].rearrange("p (k t) -> p k t", k=4))),
                     r=[psb[3 + half]], w=[bxT])
            for kc in range(8):
                K.op("pe", lambda e, kc=kc: e.matmul(ps[5][:, 0:20], lhsT=xTf[:, kc, :], rhs=wr[:, kc, :], start=(kc == 0), stop=(kc == 7)),
                     r=[bxT, bwr], w=[psb[5]])
            K.op("dve", lambda e: e.tensor_tensor(out=lgs, in0=ps[5][:, 0:20], in1=brb[:], op=ALU.add), r=[psb[5], bwr, brt], w=[brt])
            R(lambda e: e.tensor_reduce(out=sc(0), in_=lgs[:, 0:4], axis=AX.X, op=ALU.max))
            R(lambda e: e.tensor_scalar(out=ohg, in0=lgs[:, 0:4], scalar1=sc(0), scalar2=None, op0=ALU.is_ge))
            R(lambda e: e.tensor_scalar(out=sc(1), in0=sc(0), scalar1=-1.0, scalar2=None, op0=ALU.mult))
            R(lambda e: e.activation(out=eg, in_=lgs[:, 0:4], func=AF.Exp, bias=sc(1), accum_out=sc(2)), "act")
            R(lambda e: e.reciprocal(out=sc(3), in_=sc(2)))
            R(lambda e: e.tensor_scalar(out=esel, in0=lgs[:, 4:8], scalar1=ohg[:, 0:1], scalar2=None, op0=ALU.mult))
            for g in range(1, 4):
                R(lambda e, g=g: e.scalar_tensor_tensor(out=esel, in0=lgs[:, 4 + 4 * g:8 + 4 * g], scalar=ohg[:, g:g + 1], in1=esel, op0=ALU.mult, op1=ALU.add))
            R(lambda e: e.tensor_reduce(out=sc(4), in_=esel, axis=AX.X, op=ALU.max))
            R(lambda e: e.tensor_scalar(out=msk1, in0=esel, scalar1=sc(4), scalar2=None, op0=ALU.is_ge))
            R(lambda e: e.scalar_tensor_tensor(out=e2, in0=msk1, scalar=-1e30, in1=esel, op0=ALU.mult, op1=ALU.add))
            R(lambda e: e.tensor_reduce(out=sc(5), in_=e2, axis=AX.X, op=ALU.max))
            R(lambda e: e.tensor_scalar(out=msk2, in0=e2, scalar1=sc(5), scalar2=None, op0=ALU.is_ge))
            R(lambda e: e.tensor_tensor(out=sc(6), in0=sc(5), in1=sc(4), op=ALU.subtract))
            R(lambda e: e.activation(out=sc(7), in_=sc(6), func=AF.Exp), "act")
            R(lambda e: e.tensor_scalar(out=sc(8), in0=sc(7), scalar1=1.0, scalar2=None, op0=ALU.add))
            R(lambda e: e.reciprocal(out=sc(9), in_=sc(8)))
            R(lambda e: e.tensor_tensor(out=sc(10), in0=sc(3), in1=sc(9), op=ALU.mult))
            R(lambda e: e.tensor_tensor(out=sc(11), in0=sc(10), in1=sc(7), op=ALU.mult))
            R(lambda e: e.tensor_scalar(out=c4, in0=msk1, scalar1=sc(10), scalar2=None, op0=ALU.mult))
            R(lambda e: e.scalar_tensor_tensor(out=c4, in0=msk2, scalar=sc(11), in1=c4, op0=ALU.mult, op1=ALU.add))
            for g in range(4):
                R(lambda e, g=g: e.tensor_scalar(out=comb[:, 4 * g:4 * g + 4], in0=c4, scalar1=ohg[:, g:g + 1], scalar2=None, op0=ALU.mult))
            R(lambda e: e.tensor_copy(out=combb[:], in_=comb[:]))
            pst5 = ps[5].bitcast(BF16)
            K.op("pe", lambda e: e.transpose(out=pst5[0:16, 128:256], in_=combb[:], identity=ident), r=[brt, b_cbf], w=[psb[5]])
            K.op("dve", lambda e, tsl=tsl: e.tensor_copy(out=combT[0:16, tsl], in_=pst5[0:16, 128:256]), r=[psb[5]], w=[bcT])

        K.barrier()
        bg3 = Buf("gbc3")
        K.dma("sp", lambda e: e.dma_start(out=gbc[:], in_=g_fin.partition_broadcast(128)), w=[bg3])
        hdn = [view(QsT, BF16, 0, [128, 16, 512]), view(KsT, BF16, 0, [128, 16, 512])]
        bhd = Buf("hdn")
        sa = [view(xn[0], F32, 0, [128, 512]), view(xn[1], F32, 0, [128, 512])]
        tt = [xn2f[:, 0:512], xn2f[:, 512:1024]]
        bsa, btt = [Buf("sa0"), Buf("sa1")], [Buf("tt0"), Buf("tt1")]
        wdb = [view(o_sT, BF16, 0, [128, 2, D]), view(o_mT, BF16, 0, [128, 2, D])]
        bwd = [Buf("wd0"), Buf("wd1")]
        w_eg_v = w_eg.rearrange("e (k p) n -> e p k n", p=128)
        w_eu_v = w_eu.rearrange("e (k p) n -> e p k n", p=128)
        w_ed_v = w_ed.rearrange("e (k p) n -> e p k n", p=128)
        groups = [(g * 512, 512) for g in range(4)] + [(S, 128)]
        it_ = 0
        for (t0, nt) in groups:
            for ex in range(16):
                u = ex % 2
                load_any([w_eg_v[ex], w_eu_v[ex]], [wb[u][:, :, 0:256], wb[u][:, :, 256:512]], bwb2[u])
                K.op("pe", lambda e, ex=ex, t0=t0, nt=nt: e.matmul(ps[4][:, 0:nt], lhsT=SelE[0:16, ex, :], rhs=combT[0:16, t0:t0 + nt], start=True, stop=True),
                     r=[bSel, bcT], w=[psb[4]])
                for fc in range(2):
                    a = it_ % 2
                    it_ += 1
                    for kc in range(8):
                        K.op("pe", lambda e, kc=kc, u=u, fc=fc, a=a, t0=t0, nt=nt: e.matmul(ps[a][:, 0:nt], lhsT=wb[u][:, kc, fc * 128:(fc + 1) * 128],
                                                                                          rhs=xnT[:, kc, t0:t0 + nt], start=(kc == 0), stop=(kc == 7)),
                             r=[bwb2[u]] + [b_xnT[t0 // 128 + q] for q in range(nt // 128)], w=[psb[a]])
                    for kc in range(8):
                        K.op("pe", lambda e, kc=kc, u=u, fc=fc, a=a, t0=t0, nt=nt: e.matmul(ps[2 + a][:, 0:nt], lhsT=wb[u][:, kc, 256 + fc * 128:256 + (fc + 1) * 128],
                                                                                          rhs=xnT[:, kc, t0:t0 + nt], start=(kc == 0), stop=(kc == 7)),
                             r=[bwb2[u]] + [b_xnT[t0 // 128 + q] for q in range(nt // 128)], w=[psb[2 + a]])
                    K.op("act", lambda e, a=a, nt=nt: e.activation(out=sa[a][:, 0:nt], in_=ps[a][:, 0:nt], func=AF.Silu), r=[psb[a]], w=[bsa[a]])
                    K.op("dve", lambda e, a=a, nt=nt: e.tensor_tensor(out=tt[a][:, 0:nt], in0=sa[a][:, 0:nt], in1=ps[2 + a][:, 0:nt], op=ALU.mult),
                         r=[bsa[a], psb[2 + a]], w=[btt[a]])
                    ch = ex * 2 + fc
                    K.op("dve", lambda e, a=a, nt=nt, ch=ch: e.tensor_tensor(out=hdn[ch // 16][:, ch % 16, 0:nt], in0=tt[a][:, 0:nt], in1=ps[4][:, 0:nt], op=ALU.mult),
                         r=[btt[a], psb[4]], w=[bhd])
            ntile = nt // 128
            for ex in range(16):
                u = ex % 2
                stv = view(wst[u], F32, 0, [128, 2, D])
                K.dma("sp", lambda e, ex=ex, stv=stv: e.dma_start(out=stv, in_=w_ed_v[ex]), w=[bwst2[u]])
                K.op("pool", lambda e, u=u, stv=stv: e.tensor_copy(out=wdb[u], in_=stv), r=[bwst2[u]], w=[bwd[u]])
                for fc in range(2):
                    ch = ex * 2 + fc
                    for q in range(ntile):
                        for half in range(2):
                            K.op("pe", lambda e, ch=ch, q=q, half=half, u=u, fc=fc: e.matmul(ps[q * 2 + half][:], lhsT=hdn[ch // 16][:, ch % 16, q * 128:(q + 1) * 128],
                                                                                           rhs=wdb[u][:, fc, half * 512:(half + 1) * 512],
                                                                                           start=(ch == 0), stop=(ch == 31)),
                                 r=[bhd, bwd[u]], w=[psb[q * 2 + half]])
            for q in range(ntile):
                i = t0 // 128 + q
                j = i % 2
                tsl = slice(i * 128, (i + 1) * 128)
                K.dma("sp", lambda e, j=j, tsl=tsl: e.dma_start(out=xt[j][:], in_=y_out[tsl, :]), r=[b_x1d[i]], w=[b_xt[j]])
                for half in range(2):
                    K.op("dve", lambda e, j=j, half=half, q=q: e.tensor_tensor(out=xt[j][:, half * 512:(half + 1) * 512], in0=xt[j][:, half * 512:(half + 1) * 512],
                                                                             in1=ps[q * 2 + half][:], op=ALU.add), r=[b_xt[j], psb[q * 2 + half]], w=[b_xt[j]])
                K.op("act", lambda e, i=i, j=j: e.activation(out=junk[:], in_=xt[j][:], func=AF.Square, accum_out=ss[:, i:i + 1]),
                     r=[b_xt[j]], w=[b_junk, b_ss[i]])
                K.op("act", lambda e, i=i: e.activation(out=rs[:, i:i + 1], in_=ss[:, i:i + 1], func=AF.Sqrt, bias=1e-6, scale=1.0 / D), r=[b_ss[i]], w=[b_rs[i]])
                K.op("dve", lambda e, i=i: e.reciprocal(out=rstd[:, i:i + 1], in_=rs[:, i:i + 1]), r=[b_rs[i]], w=[b_rstd[i]])
                K.op("dve", lambda e, i=i, j=j: e.scalar_tensor_tensor(out=xt[j][:], in0=xt[j][:], scalar=rstd[:, i:i + 1], in1=gbc[:], op0=ALU.mult, op1=ALU.mult),
                     r=[b_xt[j], b_rstd[i], bg3], w=[b_xt[j]])
                K.dma("sp", lambda e, j=j, tsl=tsl: e.dma_start(out=y_out[tsl, :], in_=xt[j][:]), r=[b_xt[j], b_x1d[i]], w=[b_x1d[i]])

    K.emit()
    return nc


_NC = None


def kernel(**inputs):
    global _NC
    if _NC is None:
        _NC = build()
    nc = _NC
    xp = np.asarray(inputs["x_prompt"], np.float32)
    xs = np.asarray(inputs["x_sample"], np.float32)
    cst = make_consts()
    tok = np.arange(128)
    col = np.arange(512)
    s_of, t_of = col // 32, col % 4
    same = (tok[:, None] // 4 == s_of[None, :]) & (tok[:, None] < 64)
    m01 = np.stack([same & ((tok[:, None] % 4) < t_of[None, :]), same & ((tok[:, None] % 4) <= t_of[None, :])]).astype(np.float32)
    pt = np.asarray(inputs["page_table"], np.int32)
    in_maps = []
    for c in range(NCORES):
        xin = np.zeros((NTOK, D), np.float32)
        xin[:S] = xp[c]
        xin[S:S + 64] = xs[16 * c:16 * c + 16].reshape(64, D)
        in_maps.append({
            "xin": xin, "cst": cst,
            "g_mix": np.ascontiguousarray(inputs["g_mix"][0][None, :], np.float32),
            "w_in": np.ascontiguousarray(inputs["w_in"][0], np.float32),
            "m01": m01,
            "w_proj_sb": np.ascontiguousarray(inputs["w_proj_sb"][0], np.float32),
            "w_proj_moba": np.ascontiguousarray(inputs["w_proj_moba"][0], np.float32),
            "w_out": np.ascontiguousarray(inputs["w_out"][0], np.float32),
            "g_ffn": np.ascontiguousarray(inputs["g_ffn"][0][None, :], np.float32),
            "g_final": np.ascontiguousarray(np.asarray(inputs["g_final"])[None, :], np.float32),
            "w_router": np.ascontiguousarray(np.concatenate([inputs["w_router_group"][0], inputs["w_router_expert"][0]], axis=1), np.float32),
            "b_router": np.ascontiguousarray(np.concatenate([inputs["b_router_group"][0], inputs["b_router_expert"][0]])[None, :], np.float32),
            "w_expert_gate": np.ascontiguousarray(inputs["w_expert_gate"][0], np.float32),
            "w_expert_up": np.ascontiguousarray(inputs["w_expert_up"][0], np.float32),
            "w_expert_down": np.ascontiguousarray(inputs["w_expert_down"][0], np.float32),
        })
        m = in_maps[-1]
        if SMALL:
            rows = pt[16 * c:16 * c + 16].reshape(-1)
            for nm, key in (("sb_k", "cache_sb_k"), ("sb_v", "cache_sb_v"), ("mb_k", "cache_moba_k"), ("mb_v", "cache_moba_v")):
                m["cache_" + nm] = np.ascontiguousarray(inputs[key][0][rows].reshape(256 * 128, 512)) if (c == 0 or not SIM) else np.zeros((256 * 128, 512), np.float32)
            m["ptab"] = np.arange(256, dtype=np.int32)[None, :]
        else:
            for nm, key in (("sb_k", "cache_sb_k"), ("sb_v", "cache_sb_v"), ("mb_k", "cache_moba_k"), ("mb_v", "cache_moba_v")):
                m["cache_" + nm] = inputs[key][0].reshape(2560 * 128, 512)
            m["ptab"] = np.ascontiguousarray(pt[16 * c:16 * c + 16].reshape(1, 256))
    res = run_bass_kernel_spmd(nc, in_maps, core_ids=list(range(NCORES)))
    outs = res.results
    global _LAST
    _LAST = outs
    kv = np.stack([o["kv_out"] for o in outs])
    y = np.stack([o["y_out"] for o in outs])
    y_prompt = y[:, :S, :]
    y_sample = y[:, S:S + 64, :].reshape(128, 4, D)
    p = [kv[:, k, :S, :].reshape(1, 8, S, 8, 64) for k in range(4)]
    s = [kv[:, k, S:S + 64, :].reshape(1, 128, 4, 8, 64) for k in range(4)]
    return (y_prompt, y_sample, p[0], p[1], p[2], p[3], s[0], s[1], s[2], s[3])
```

```python
import numpy as np
import concourse.bass as bass
import concourse.mybir as mybir
from concourse.bass_utils import run_bass_kernel_spmd

F32 = mybir.dt.float32
BF16 = mybir.dt.bfloat16
I32 = mybir.dt.int32
U32 = mybir.dt.uint32
AF = mybir.ActivationFunctionType
ALU = mybir.AluOpType
AX = mybir.AxisListType

NCORES = 8
D = 1024
S = 2048
NT = 17
NTOK = NT * 128
NEG = -30000.0
import os
STAGE = int(os.environ.get("KSTAGE", "99"))
SIM = os.environ.get("KSIM", "0") == "1"
SMALL = SIM or os.environ.get("KSMALL", "0") == "1"
NSEQ = int(os.environ.get("KNSEQ", "1")) if SIM else 16


class Buf:
    __slots__ = ("name", "lw", "rd", "excl", "last", "last_w")

    def __init__(self, name, excl=False):
        self.name = name
        self.lw = None
        self.rd = []
        self.excl = excl
        self.last = None
        self.last_w = False


class Op:
    __slots__ = ("eng", "fn", "deps", "sig", "need", "isdma", "semidx")

    def __init__(self, eng, fn, isdma):
        self.eng = eng
        self.fn = fn
        self.deps = {}
        self.sig = None
        self.need = False
        self.isdma = isdma
        self.semidx = -1


class Sched:
    ENG = ["pe", "act", "dve", "pool", "sp"]
    NDMA = 24
    NSW = int(os.environ.get("KNSW", "16"))
    ROLL = 30000

    def __init__(self, nc):
        self.nc = nc
        self.ops = {e: [] for e in self.ENG}
        self.dma_last = [None] * self.NDMA
        self.dma_rr = 0
        self.dmas = []
        self.sw_last = [None] * self.NSW
        self.sw_rr = 0
        self.sw_fresh = SIM

    def _deps(self, o, r, w):
        for b in r:
            if b.excl:
                if b.last is not None and b.last is not o:
                    o.deps.setdefault(b.last, "raw" if b.last_w else "war")
                b.last, b.last_w = o, False
                continue
            if b.lw is not None and b.lw is not o:
                o.deps.setdefault(b.lw, "raw")
            b.rd.append(o)
        for b in w:
            if b.excl:
                if b.last is not None and b.last is not o:
                    o.deps[b.last] = "raw"
                b.last, b.last_w = o, True
                continue
            if b.lw is not None and b.lw is not o:
                o.deps[b.lw] = "raw"
            for x in b.rd:
                if x is not o:
                    o.deps.setdefault(x, "war")
            b.rd = []
            b.lw = o

    def op(self, eng, fn, r=(), w=()):
        o = Op(eng, fn, False)
        self._deps(o, r, w)
        self.ops[eng].append(o)
        return o

    def barrier(self):
        lasts = []
        for e in self.ENG:
            comp = [o for o in self.ops[e] if not o.isdma]
            if comp:
                lasts.append(comp[-1])
        lasts += [d for d in self.dma_last if d is not None] + [d for d in self.sw_last if d is not None]
        if self.sw_fresh:
            lasts += [d for d in self.dmas if d.semidx < 0]
        for e in self.ENG:
            o = Op(e, lambda eng: eng.nop(), False)
            for d in lasts:
                o.deps[d] = "raw"
            self.ops[e].append(o)

    def dma(self, q, fn, r=(), w=(), sw=False):
        o = Op(q, fn, True)
        if sw and self.sw_fresh:
            o.semidx = -1
        elif sw:
            i = self.sw_rr
            self.sw_rr = (i + 1) % self.NSW
            if self.sw_last[i] is not None:
                o.deps[self.sw_last[i]] = "raw"
            self.sw_last[i] = o
            o.semidx = self.NDMA + i
        else:
            i = self.dma_rr
            self.dma_rr = (i + 1) % self.NDMA
            if self.dma_last[i] is not None:
                o.deps[self.dma_last[i]] = "raw"
            self.dma_last[i] = o
            o.semidx = i
        self._deps(o, r, w)
        self.ops[q].append(o)
        self.dmas.append(o)
        return o

    def emit(self):
        nc = self.nc
        for e in self.ENG:
            for o in self.ops[e]:
                keep = {}
                for d, kind in o.deps.items():
                    if d.isdma:
                        keep[d] = kind
                    elif d.eng == o.eng and not o.isdma:
                        if o.eng == "pe":
                            continue
                        keep[d] = kind
                    else:
                        keep[d] = kind
                o.deps = keep
                for d in keep:
                    d.need = True
        nsem = self.NDMA + self.NSW
        dma_sems = [nc.alloc_semaphore(name=f"dq{i}") for i in range(nsem)]
        dma_cnt = [0] * nsem
        for o in self.dmas:
            if o.semidx < 0:
                dma_sems.append(nc.alloc_semaphore(name=f"dsw{len(dma_sems)}"))
                dma_cnt.append(0)
                o.semidx = len(dma_sems) - 1
            dma_cnt[o.semidx] += 16
            o.sig = (dma_sems[o.semidx], dma_cnt[o.semidx])
        for e in self.ENG:
            sem = None
            cnt = 0
            k = 0
            for o in self.ops[e]:
                if o.isdma or not o.need:
                    continue
                if sem is None or cnt >= self.ROLL:
                    sem = nc.alloc_semaphore(name=f"e_{e}_{k}")
                    k += 1
                    cnt = 0
                cnt += 1
                o.sig = (sem, cnt)
        handles = {"pe": "tensor", "act": "scalar", "dve": "vector", "pool": "gpsimd", "sp": "sync"}
        final = [(dma_sems[i], dma_cnt[i]) for i in range(len(dma_sems)) if dma_cnt[i] > 0]
        sched = self

        with nc.Block() as block:
            def run(e, eng):
                known = {}
                for o in sched.ops[e]:
                    waits = {}
                    for d in o.deps:
                        s, v = d.sig
                        if waits.get(s, 0) < v:
                            waits[s] = v
                    for s, v in waits.items():
                        if known.get(s, 0) >= v:
                            continue
                        eng.wait_ge(s, v)
                        known[s] = v
                    ins = o.fn(eng)
                    if o.sig is not None:
                        ins.then_inc(o.sig[0], 16 if o.isdma else 1)
                if e == "sp":
                    for s, v in final:
                        eng.wait_ge(s, v)

            @block.tensor
            def _(eng):
                run("pe", eng)

            @block.scalar
            def _(eng):
                run("act", eng)

            @block.vector
            def _(eng):
                run("dve", eng)

            @block.gpsimd
            def _(eng):
                run("pool", eng)

            @block.sync
            def _(eng):
                run("sp", eng)


C_ID, C_NTRI, C_MSB, C_MMB = 0, 128, 256, 384
C_COS, C_SIN = 512, 512 + NT * 8
C_W = 512 + 2 * NT * 8


def make_consts():
    c = np.zeros((128, C_W), np.float32)
    p = np.arange(128)
    c[:, C_ID:C_ID + 128] = np.eye(128, dtype=np.float32)
    c[:, C_NTRI:C_NTRI + 128] = -(p[:, None] >= p[None, :]).astype(np.float32)
    c[:, C_MSB:C_MSB + 128] = NEG * (p[:, None] >= p[None, :])
    c[:, C_MMB:C_MMB + 128] = NEG * (p[:, None] > p[None, :])
    half = 8
    inv = np.float32(500000.0) ** (-np.arange(half, dtype=np.float32) / half)
    pos = np.zeros((NT, 128), np.float32)
    for i in range(16):
        pos[i] = i * 128 + p
    pos[16, :64] = 2048 + (p[:64] % 4)
    ang = pos[:, :, None].astype(np.float32) * inv[None, None, :]
    cs = np.cos(ang).astype(np.float32).transpose(1, 0, 2)
    sn = np.sin(ang).astype(np.float32).transpose(1, 0, 2)
    c[:, C_COS:C_COS + NT * 8] = cs.reshape(128, NT * 8)
    c[:, C_SIN:C_SIN + NT * 8] = sn.reshape(128, NT * 8)
    return c


def build():
    nc = bass.Bass("TRN2", target_bir_lowering=False)
    K = Sched(nc)

    def dram(name, shape, dt, kind):
        return nc.dram_tensor(name, list(shape), dt, kind=kind).ap()

    xin = dram("xin", [NTOK, D], F32, "ExternalInput")
    cst_d = dram("cst", [128, C_W], F32, "ExternalInput")
    g_mix = dram("g_mix", [1, D], F32, "ExternalInput")
    w_in = dram("w_in", [D, 5120], F32, "ExternalInput")
    NROWS = (256 if SMALL else 2560) * 128
    cache = {nm: dram("cache_" + nm, [NROWS, 512], F32, "ExternalInput") for nm in ("sb_k", "sb_v", "mb_k", "mb_v")}
    ptab = dram("ptab", [1, 256], I32, "ExternalInput")
    m01_d = dram("m01", [2, 128, 512], F32, "ExternalInput")
    w_ps = dram("w_proj_sb", [512, D], F32, "ExternalInput")
    w_pm = dram("w_proj_moba", [512, D], F32, "ExternalInput")
    w_o = dram("w_out", [D, D], F32, "ExternalInput")
    g_ffn = dram("g_ffn", [1, D], F32, "ExternalInput")
    g_fin = dram("g_final", [1, D], F32, "ExternalInput")
    w_r = dram("w_router", [D, 20], F32, "ExternalInput")
    b_r = dram("b_router", [1, 20], F32, "ExternalInput")
    w_eg = dram("w_expert_gate", [16, D, 256], F32, "ExternalInput")
    w_eu = dram("w_expert_up", [16, D, 256], F32, "ExternalInput")
    w_ed = dram("w_expert_down", [16, 256, D], F32, "ExternalInput")
    kv_out = dram("kv_out", [4, NTOK, 512], F32, "ExternalOutput")
    y_out = dram("y_out", [NTOK, D], F32, "ExternalOutput")

    def sb(name, shape, dt):
        return nc.alloc_sbuf_tensor("sb_" + name, list(shape), dt).ap()

    ps = [nc.alloc_psum_tensor(f"ps{i}", [128, 512], F32).ap() for i in range(8)]
    psb = [Buf(f"ps{i}", excl=True) for i in range(8)]

    cst = sb("cst", [128, C_W], F32)
    b_cst = Buf("cst")
    K.dma("sp", lambda e: e.dma_start(out=cst[:], in_=cst_d[:]), w=[b_cst])
    cbf = sb("cbf", [128, 512], BF16)
    b_cbf = Buf("cbf")
    K.op("dve", lambda e: e.tensor_copy(out=cbf[:], in_=cst[:, 0:512]), r=[b_cst], w=[b_cbf])
    ident = cbf[:, C_ID:C_ID + 128]
    ntri = cbf[:, C_NTRI:C_NTRI + 128]
    msb = cbf[:, C_MSB:C_MSB + 128]
    mmb = cbf[:, C_MMB:C_MMB + 128]
    cosT = cst[:, C_COS:C_COS + NT * 8].rearrange("p (t f) -> p t f", f=8)
    sinT = cst[:, C_SIN:C_SIN + NT * 8].rearrange("p (t f) -> p t f", f=8)
    gbc = sb("gbc", [128, D], F32)
    b_gbc = Buf("gbc")
    K.dma("sp", lambda e: e.dma_start(out=gbc[:], in_=g_mix.partition_broadcast(128)), w=[b_gbc])

    xnT = sb("xnT", [128, 8, NTOK], BF16)
    b_xnT = [Buf(f"xnT{i}") for i in range(NT)]
    QsT = sb("QsT", [128, 4, NTOK], BF16)
    KsT = sb("KsT", [128, 4, NTOK], BF16)
    Vs = sb("Vs", [128, NT, 512], BF16)
    b_QsT = [Buf(f"QsT{i}") for i in range(NT)]
    b_KsT = [Buf(f"KsT{i}") for i in range(NT)]
    b_Vs = [Buf(f"Vs{i}") for i in range(NT)]

    xt = [sb(f"xt{j}", [128, D], F32) for j in range(2)]
    b_xt = [Buf(f"xt{j}") for j in range(2)]
    junk = sb("junk", [128, D], BF16)
    b_junk = Buf("junk")
    ss = sb("ss", [128, NT], F32)
    rs = sb("rs", [128, NT], F32)
    rstd = sb("rstd", [128, NT], F32)
    b_ss = [Buf(f"ss{i}") for i in range(NT)]
    b_rs = [Buf(f"rs{i}") for i in range(NT)]
    b_rstd = [Buf(f"rstd{i}") for i in range(NT)]
    xn = [sb(f"xn{j}", [128, D], BF16) for j in range(2)]
    b_xn = [Buf(f"xn{j}") for j in range(2)]

    for i in range(int(os.environ.get("KNT", NT))):
        j = i % 2
        K.dma("sp", lambda e, i=i, j=j: e.dma_start(out=xt[j][:], in_=xin[i * 128:(i + 1) * 128, :]), w=[b_xt[j]])
        K.op("act", lambda e, i=i, j=j: e.activation(out=junk[:], in_=xt[j][:], func=AF.Square,
                                                       accum_out=ss[:, i:i + 1]),
             r=[b_xt[j]], w=[b_junk, b_ss[i]])
        K.op("act", lambda e, i=i: e.activation(out=rs[:, i:i + 1], in_=ss[:, i:i + 1], func=AF.Sqrt,
                                                 bias=1e-6, scale=1.0 / D), r=[b_ss[i]], w=[b_rs[i]])
        K.op("dve", lambda e, i=i: e.reciprocal(out=rstd[:, i:i + 1], in_=rs[:, i:i + 1]), r=[b_rs[i]], w=[b_rstd[i]])
        K.op("dve", lambda e, i=i, j=j: e.scalar_tensor_tensor(out=xn[j][:], in0=xt[j][:], scalar=rstd[:, i:i + 1],
                                                                in1=gbc[:], op0=ALU.mult, op1=ALU.mult),
             r=[b_xt[j], b_rstd[i], b_gbc], w=[b_xn[j]])
        pb = i % 2
        pst = ps[pb].bitcast(BF16)
        for kc in range(8):
            K.op("pe", lambda e, kc=kc, j=j, pst=pst: e.transpose(out=pst[:, kc * 128:(kc + 1) * 128],
                                                                     in_=xn[j][:, kc * 128:(kc + 1) * 128],
                                                                     identity=ident),
                 r=[b_xn[j], b_cbf], w=[psb[pb]])
        K.op("act", lambda e, i=i, pst=pst: e.activation(out=xnT[:, :, i * 128:(i + 1) * 128],
                                                          in_=pst.rearrange("p (k t) -> p k t", k=8), func=AF.Copy),
             r=[psb[pb]], w=[b_xnT[i]])

    wb = [sb(f"wb{j}", [128, 8, 512], BF16) for j in range(2)]
    b_wb = [Buf(f"wb{j}") for j in range(2)]
    w_in_v = w_in.rearrange("(k p) n -> p k n", p=128)
    wcount = [0]

    wst = [sb(f"wst{j}", [128, 4, 512], F32) for j in range(2)]
    b_wst = [Buf(f"wst{j}") for j in range(2)]

    def load_w(c):
        j = wcount[0] % 2
        wcount[0] += 1
        for h in range(2):
            K.dma("sp", lambda e, c=c, h=h: e.dma_start(out=wst[h][:], in_=w_in_v[:, 4 * h:4 * h + 4, c * 512:(c + 1) * 512]),
                  w=[b_wst[h]])
            K.op("pool", lambda e, j=j, h=h: e.tensor_copy(out=wb[j][:, 4 * h:4 * h + 4, :], in_=wst[h][:]),
                 r=[b_wst[h]], w=[b_wb[j]])
        return j

    hf = [sb(f"hf{j}", [128, 512], F32) for j in range(2)]
    b_hf = [Buf(f"hf{j}") for j in range(2)]
    hb = [sb(f"hb{j}", [128, 512], BF16) for j in range(2)]
    b_hb = [Buf(f"hb{j}") for j in range(2)]
    cnt = {"proj": 0, "hf": 0, "hb": 0}

    def project(c, i, j):
        pb = 2 + cnt["proj"] % 2
        cnt["proj"] += 1
        for kc in range(8):
            K.op("pe", lambda e, kc=kc, i=i, j=j, pb=pb: e.matmul(ps[pb][:], lhsT=xnT[:, kc, i * 128:(i + 1) * 128],
                                                                    rhs=wb[j][:, kc, :], start=(kc == 0), stop=(kc == 7)),
                 r=[b_xnT[i], b_wb[j]], w=[psb[pb]])
        return pb

    def to_T(src_bf, b_src, dstT, b_dst, i, pbank, dst_ap=None):
        pst = ps[pbank].bitcast(BF16)
        if dst_ap is None:
            dst_ap = dstT[:, :, i * 128:(i + 1) * 128]
        for cc in range(4):
            K.op("pe", lambda e, cc=cc: e.transpose(out=pst[:, cc * 128:(cc + 1) * 128],
                                                     in_=src_bf[:, cc * 128:(cc + 1) * 128], identity=ident),
                 r=[b_src, b_cbf], w=[psb[pbank]])
        K.op("dve", lambda e: e.tensor_copy(out=dst_ap,
                                            in_=pst[:, 0:512].rearrange("p (k t) -> p k t", k=4)),
             r=[psb[pbank]], w=[b_dst])

    def kv_store(which, i, src, b_src):
        K.dma("sp", lambda e: e.dma_start(out=kv_out[which, i * 128:(i + 1) * 128, :], in_=src[:]), r=[b_src])

    samp = {}
    for br in ("sb", "mb"):
        samp[br] = dict(QT=sb(f"sQT_{br}", [128, 4, 128], BF16), KT=sb(f"sKT_{br}", [128, 4, 128], BF16),
                        V=sb(f"sV_{br}", [128, 512], BF16), b=Buf(f"samp_{br}"))
    sQTf = sb("sQTf", [128, 4, 128], F32)
    b_sQTf = Buf("sQTf")

    for (c, kind) in (((1, "k"), (2, "v"), (0, "q"))[:int(os.environ.get("KA1", 3))] if STAGE >= 1 else ()):
        j = load_w(c)
        for i in range(int(os.environ.get("KA1T", NT))):
            pb = project(c, i, j)
            if kind in ("k", "v"):
                a = cnt["hf"] % 2
                cnt["hf"] += 1
                K.op("act", lambda e, a=a, pb=pb: e.activation(out=hf[a][:], in_=ps[pb][:], func=AF.Copy),
                     r=[psb[pb]], w=[b_hf[a]])
                kv_store(0 if kind == "k" else 1, i, hf[a], b_hf[a])
            if kind == "k":
                b = cnt["hb"] % 2
                cnt["hb"] += 1
                K.op("dve", lambda e, b=b, pb=pb: e.tensor_copy(out=hb[b][:], in_=ps[pb][:]), r=[psb[pb]], w=[b_hb[b]])
                if i == 16:
                    to_T(hb[b], b_hb[b], None, samp["sb"]["b"], i, 4 + i % 2, dst_ap=samp["sb"]["KT"][:])
                else:
                    to_T(hb[b], b_hb[b], KsT, b_KsT[i], i, 4 + i % 2)
            elif kind == "v":
                if i == 16:
                    K.op("dve", lambda e, pb=pb: e.tensor_copy(out=samp["sb"]["V"][:], in_=ps[pb][:]), r=[psb[pb]], w=[samp["sb"]["b"]])
                else:
                    K.op("dve", lambda e, i=i, pb=pb: e.tensor_copy(out=Vs[:, i, :], in_=ps[pb][:]), r=[psb[pb]], w=[b_Vs[i]])
            else:
                b = cnt["hb"] % 2
                cnt["hb"] += 1
                K.op("act", lambda e, b=b, pb=pb: e.activation(out=hb[b][:], in_=ps[pb][:], func=AF.Copy, scale=0.125),
                     r=[psb[pb]], w=[b_hb[b]])
                if i == 16:
                    to_T(hb[b], b_hb[b], None, samp["sb"]["b"], i, 4 + i % 2, dst_ap=samp["sb"]["QT"][:])
                else:
                    to_T(hb[b], b_hb[b], QsT, b_QsT[i], i, 4 + i % 2)

    o_sT = sb("o_sT", [128, 4, NTOK], BF16)
    b_osT = Buf("o_sT")
    negones = sb("negones", [128, 128], BF16)
    ones_bf = sb("ones_bf", [128, 128], BF16)
    b_ones = Buf("ones")
    K.op("pool", lambda e: e.memset(negones[:], -1.0), w=[b_ones])
    K.op("pool", lambda e: e.memset(ones_bf[:], 1.0), w=[b_ones])
    e_sb = [xt[j][:, 0:512] for j in range(2)]
    sp_sb = [sb(f"sp_sb{j}", [128, 512], BF16) for j in range(2)]
    A_sb = [junk[:, 512 * j:512 * (j + 1)] for j in range(2)]
    srun = [sb(f"srun{j}", [128, 512], BF16) for j in range(3)]
    b_e = b_xt
    b_sp = [Buf(f"sp{j}") for j in range(2)]
    b_A = [Buf(f"A{j}") for j in range(2)]
    b_srun = [Buf(f"srun{j}") for j in range(3)]
    dbg_o = dram("dbg_o", [2, 128, 4, NTOK], BF16, "ExternalOutput")

    steps = []
    for h in range(8):
        for g in range(4):
            for j in range(4 * g + 3, -1, -1):
                steps.append((h, g, j))

    def zmm(P, pbank, h, g, j, q0, last):
        c, p0 = h // 2, (h % 2) * 64
        K.op("pe", lambda e: e.matmul(P[:, q0:512], lhsT=KsT[p0:p0 + 64, c, j * 128:(j + 1) * 128],
                                      rhs=QsT[p0:p0 + 64, c, g * 512 + q0:(g + 1) * 512], start=True,
                                      stop=(last and j < 4 * g)),
             r=[b_KsT[j]] + [b_QsT[4 * g + t] for t in range(4)], w=[psb[pbank]])
        if j >= 4 * g:
            K.op("pe", lambda e: e.matmul(P[:, q0:q0 + 128], lhsT=ident, rhs=msb, start=False, stop=last),
                 r=[b_cbf], w=[psb[pbank]])

    def sb_X(t):
        h, g, j = steps[t]
        a = t % 2
        q0 = max(j - 4 * g, 0) * 128
        first = (j == 4 * g + 3)
        zmm(ps[a], a, h, g, j, q0, True)
        K.op("act", lambda e: e.activation(out=e_sb[a][:, q0:], in_=ps[a][:, q0:], func=AF.Exp), r=[psb[a]], w=[b_e[a]])
        K.op("act", lambda e: e.activation(out=sp_sb[a][:, q0:], in_=e_sb[a][:, q0:], func=AF.Ln, bias=1.0),
             r=[b_e[a]], w=[b_sp[a]])
        cu, nx = t % 3, (t + 1) % 3
        if first:
            K.op("pool", lambda e: e.memset(srun[nx][:, 0:q0], 0.0), w=[b_srun[nx]])
            K.op("pool", lambda e: e.tensor_copy(out=srun[nx][:, q0:], in_=sp_sb[a][:, q0:]), r=[b_sp[a]], w=[b_srun[nx]])
        elif j > 0:
            if q0 > 0:
                K.op("pool", lambda e: e.tensor_copy(out=srun[nx][:, 0:q0], in_=srun[cu][:, 0:q0]), r=[b_srun[cu]], w=[b_srun[nx]])
            K.op("pool", lambda e: e.tensor_tensor(out=srun[nx][:, q0:], in0=srun[cu][:, q0:], in1=sp_sb[a][:, q0:], op=ALU.add),
                 r=[b_srun[cu], b_sp[a]], w=[b_srun[nx]])

    def sb_Y(t):
        h, g, j = steps[t]
        a = t % 2
        c, p0 = h // 2, (h % 2) * 64
        q0 = max(j - 4 * g, 0) * 128
        first = (j == 4 * g + 3)
        P2 = ps[2 + a]
        zmm(P2, 2 + a, h, g, j, q0, False)
        K.op("pe", lambda e: e.matmul(P2[:, q0:], lhsT=ntri, rhs=sp_sb[a][:, q0:], start=False, stop=first),
             r=[b_sp[a], b_cbf], w=[psb[2 + a]])
        if not first:
            K.op("pe", lambda e: e.matmul(P2[:, q0:], lhsT=negones[:], rhs=srun[t % 3][:, q0:], start=False, stop=True),
                 r=[b_srun[t % 3], b_ones], w=[psb[2 + a]])
        K.op("act", lambda e: e.activation(out=A_sb[a][:, q0:], in_=P2[:, q0:], func=AF.Exp), r=[psb[2 + a]], w=[b_A[a]])
        ob = 4 + (h * 4 + g) % 2
        K.op("pe", lambda e: e.matmul(ps[ob][:, q0:], lhsT=Vs[:, j, c * 128:(c + 1) * 128], rhs=A_sb[a][:, q0:],
                                      start=first, stop=(j == 0), skip_group_check=True),
             r=[b_A[a], b_Vs[j]], w=[psb[ob]])
        if j == 0:
            K.op("dve", lambda e: e.tensor_copy(out=o_sT[p0:p0 + 64, c, g * 512:(g + 1) * 512], in_=ps[ob][p0:p0 + 64, :]),
                 r=[psb[ob]], w=[b_osT])

    if STAGE >= 2:
        sb_X(0)
        for t in range(len(steps)):
            if t + 1 < len(steps):
                sb_X(t + 1)
            sb_Y(t)
        K.dma("sp", lambda e: e.dma_start(out=dbg_o[0, :, :, 0:S], in_=o_sT[:, :, 0:S]), r=[b_osT])

    def view(t, dt, off, shape):
        nd = len(t.shape)
        f = t[:]
        if nd > 2:
            names = " ".join(f"a{i}" for i in range(nd - 1))
            f = f.rearrange(f"p {names} -> p ({names})")
        isz = {F32: 4, I32: 4, BF16: 2}[dt]
        osz = {F32: 4, I32: 4, BF16: 2}[t.dtype]
        if isz != osz:
            f = f.bitcast(dt)
        n = 1
        for d_ in shape[1:]:
            n *= d_
        v = f[0:shape[0], off // isz:off // isz + n]
        if len(shape) > 2:
            names = " ".join(f"b{i}" for i in range(len(shape) - 1))
            kw = {f"b{i}": shape[i + 1] for i in range(len(shape) - 1)}
            v = v.rearrange(f"p ({names}) -> p {names}", **kw)
        return v

    offs = sb("offs", [128, 256], I32)
    ptb = xn[0][:, 0:512].bitcast(I32)
    iot = sb("iot", [128, 1], I32)
    b_offs = Buf("offs")
    K.dma("sp", lambda e: e.dma_start(out=ptb[:], in_=ptab.partition_broadcast(128)), r=[b_xn[0]], w=[b_offs, b_xn[0]])
    K.op("pool", lambda e: e.iota(iot[:], pattern=[[0, 1]], base=0, channel_multiplier=1), w=[b_offs])
    K.op("pool", lambda e: e.tensor_scalar(out=offs[:], in0=ptb[:], scalar1=128.0, scalar2=None, op0=ALU.mult), r=[b_offs], w=[b_offs])
    K.op("pool", lambda e: e.tensor_tensor(out=offs[:], in0=offs[:], in1=iot[:].broadcast_to([128, 256]), op=ALU.add), r=[b_offs], w=[b_offs])

    def sample_attn(br):
        K.barrier()
        is_sb = (br == "sb")
        sm = samp[br]
        poolK, poolV = cache[br + "_k"], cache[br + "_v"]
        o_T, b_oT = (o_sT, b_osT) if is_sb else (o_mT, b_omT)
        Kg = view(QsT, BF16, 0, [128, 16, 512])
        Vg = view(KsT, BF16, 0, [128, 16, 512])
        alt = [view(xt[0], BF16, 0, [128, 4, 512]), view(xt[1], BF16, 0, [128, 4, 512]), view(hf[0], BF16, 0, [128, 2, 512]),
               view(hf[1], BF16, 0, [128, 2, 512])]
        altp = ([alt[0][:, q, :] for q in range(4)] + [alt[1][:, q, :] for q in range(4)] + [alt[k][:, q, :] for k in range(2, 4) for q in range(2)]
                + [sp_sb[0][:], sp_sb[1][:], srun[0][:], srun[1][:]])
        Vpg = [[Vg[:, pg, :] for pg in range(16)], altp]
        KTs = view(Vs, BF16, 0, [128, 4, 2048])
        Qbdf = view(wst[0], F32, 0, [128, 4, 16, 32])
        Qbd = view(wst[1], BF16, 0, [128, 4, 16, 32])
        IndN = view(wst[1], BF16, 4096, [128, 16, 128])
        e_s = view(wb[0], F32, 0, [128, 512])
        sp_s = view(wb[0], BF16, 2048, [128, 512])
        A_s = view(wb[0], BF16, 3072, [128, 512])
        en = view(wb[0], F32, 4096, [128, 512])
        spn = view(wb[0], BF16, 6144, [128, 512])
        An = view(wb[0], BF16, 7168, [128, 512])
        m01 = view(wb[1], F32, 0, [128, 512])
        kmTs = view(wb[1], F32, 2048, [128, 4, 8])
        sblk_s = view(wb[1], F32, 2304, [128, 8])
        mx_s = view(wb[1], F32, 2432, [128, 8])
        b01_s = view(wb[1], BF16, 2560, [128, 8])
        biasTs = view(wb[1], BF16, 2624, [128, 32])
        rl_s = view(wb[1], F32, 4096, [128, 512])
        tmpn = view(wb[1], F32, 6144, [128, 512])
        bq, bm, bn, bkg, bkt = Buf("s_q"), Buf("s_m"), Buf("s_n"), Buf("s_kg"), Buf("s_kt")
        bvg2 = [Buf("s_vg0"), Buf("s_vg1")]
        be, bsp, bA, bkm, bsb_, bbt, brl = Buf("s_e"), Buf("s_sp"), Buf("s_A"), Buf("s_km"), Buf("s_sblk"), Buf("s_bt"), Buf("s_rl")
        K.dma("sp", lambda e: e.dma_start(out=m01, in_=m01_d[0 if is_sb else 1]), w=[bm])
        K.op("pool", lambda e: e.memset(Qbd, 0.0), w=[bq])
        if not is_sb:
            K.op("pool", lambda e: e.memset(Qbdf, 0.0), w=[bq])
            K.op("pool", lambda e: e.memset(IndN[0:8], 0.0), w=[bq])
            K.op("dve", lambda e: e.tensor_copy(out=IndN[0:8].rearrange("p (n u) m -> p n (u m)", u=2),
                                                in_=id30k[0:8, 0:8].unsqueeze(2).broadcast_to([8, 8, 256])), r=[b_selm], w=[bq])
        for h in range(8):
            c, p0 = h // 2, (h % 2) * 64
            K.op("dve", lambda e, c=c, p0=p0, h=h: e.tensor_copy(out=Qbd[p0:p0 + 64, c, :, h * 4:(h + 1) * 4],
                                                               in_=sm["QT"][p0:p0 + 64, c, 0:64].rearrange("p (s t) -> p s t", t=4)),
                 r=[sm["b"]], w=[bq])
            if not is_sb:
                K.op("dve", lambda e, c=c, p0=p0, h=h: e.tensor_copy(out=Qbdf[p0:p0 + 64, c, :, h * 4:(h + 1) * 4],
                                                                   in_=sQTf[p0:p0 + 64, c, 0:64].rearrange("p (s t) -> p s t", t=4)),
                     r=[b_sQTf], w=[bq])
        def znew(bank):
            for s_ in range(16):
                for c in range(4):
                    K.op("pe", lambda e, s_=s_, c=c: e.matmul(ps[bank][:, s_ * 32:(s_ + 1) * 32], lhsT=sm["KT"][:, c, :], rhs=Qbd[:, c, s_, :],
                                                            start=(s_ == 0 and c == 0), stop=False, skip_group_check=True),
                         r=[sm["b"], bq], w=[psb[bank]])
        znew(4)
        K.op("act", lambda e: e.activation(out=en, in_=ps[4][:], func=AF.Exp), r=[psb[4]], w=[bn])
        K.op("dve", lambda e: e.tensor_tensor(out=en, in0=en, in1=m01, op=ALU.mult), r=[bn, bm], w=[bn])
        if is_sb:
            K.op("act", lambda e: e.activation(out=spn, in_=en, func=AF.Ln, bias=1.0), r=[bn], w=[bn])
            znew(5)
            K.op("pe", lambda e: e.matmul(ps[5][:], lhsT=ntri, rhs=spn, start=False, stop=True, skip_group_check=True), r=[bn, b_cbf], w=[psb[5]])
            K.op("act", lambda e: e.activation(out=tmpn, in_=ps[5][:], func=AF.Exp), r=[psb[5]], w=[bn])
            K.op("dve", lambda e: e.tensor_tensor(out=An, in0=tmpn, in1=m01, op=ALU.mult), r=[bn, bm], w=[bn])
        else:
            K.op("dve", lambda e: e.tensor_copy(out=An, in_=en), r=[bn], w=[bn])
        first_o = [True]
        for s_ in range(NSEQ):
            par = s_ % 2
            bvg = bvg2[par]
            order = []
            for pg in range(16):
                order += [(Kg[:, pg, :], poolK, bkg, pg), (Vpg[par][pg], poolV, bvg, pg)]
            for (dstg, poolg, bg_, pg) in order:
                col = s_ * 16 + pg
                K.dma("pool", lambda e, pg=pg, col=col, dstg=dstg, poolg=poolg: e.indirect_dma_start(
                    out=dstg, out_offset=None, in_=poolg[:, :],
                    in_offset=bass.IndirectOffsetOnAxis(ap=offs[:, col:col + 1], axis=0)), r=[b_offs], w=[bg_], sw=True)
            for pg in range(16):
                bk = 6 + pg % 2
                pst = ps[bk].bitcast(BF16)
                for c in range(4):
                    K.op("pe", lambda e, pg=pg, c=c, pst=pst: e.transpose(out=pst[:, c * 128:(c + 1) * 128], in_=Kg[:, pg, c * 128:(c + 1) * 128], identity=ident),
                         r=[bkg, b_cbf], w=[psb[bk]])
                if pg % 2 == 0:
                    K.op("act", lambda e, pg=pg, pst=pst: e.activation(out=KTs[:, :, pg * 128:(pg + 1) * 128], in_=pst[:, 0:512].rearrange("p (c t) -> p c t", c=4), func=AF.Copy),
                         r=[psb[bk]], w=[bkt])
                else:
                    K.op("dve", lambda e, pg=pg, pst=pst: e.tensor_copy(out=KTs[:, :, pg * 128:(pg + 1) * 128], in_=pst[:, 0:512].rearrange("p (c t) -> p c t", c=4)),
                         r=[psb[bk]], w=[bkt])
            if not is_sb:
                for c in range(4):
                    for n in range(8):
                        for u in range(2):
                            K.op("pe", lambda e, c=c, n=n, u=u: e.matmul(ps[5][:, c * 8 + n:c * 8 + n + 1], lhsT=Kg[:, 2 * n + u, c * 128:(c + 1) * 128],
                                                                       rhs=ones256b[:, 0:1], start=(u == 0), stop=(u == 1)),
                                 r=[bkg, b_ones], w=[psb[5]])
                K.op("dve", lambda e: e.tensor_copy(out=kmTs, in_=ps[5][:, 0:32].rearrange("p (c n) -> p c n", c=4)), r=[psb[5]], w=[bkm])
                for c in range(4):
                    K.op("pe", lambda e, c=c, s_=s_: e.matmul(ps[5][0:32, 32:40], lhsT=Qbdf[:, c, s_, :], rhs=kmTs[:, c, :], start=(c == 0), stop=(c == 3)),
                         r=[bq, bkm], w=[psb[5]])
                K.op("dve", lambda e: e.tensor_copy(out=sblk_s[0:32], in_=ps[5][0:32, 32:40]), r=[psb[5]], w=[bsb_])
                K.op("dve", lambda e: e.max(out=mx_s[0:32], in_=sblk_s[0:32]), r=[bsb_], w=[bsb_])
                K.op("dve", lambda e: e.tensor_scalar(out=b01_s[0:32], in0=sblk_s[0:32], scalar1=mx_s[0:32, 2:3], scalar2=1.0,
                                                      op0=ALU.is_ge, op1=ALU.subtract), r=[bsb_], w=[bsb_])
                pst5 = ps[5].bitcast(BF16)
                K.op("pe", lambda e: e.transpose(out=pst5[0:8, 128:160], in_=b01_s[0:32], identity=ident[0:32, 0:32]), r=[bsb_, b_cbf], w=[psb[5]])
                K.op("dve", lambda e: e.tensor_copy(out=biasTs[0:8], in_=pst5[0:8, 128:160]), r=[psb[5]], w=[bbt])

            def zs(bank):
                for pg in range(16):
                    for c in range(4):
                        K.op("pe", lambda e, pg=pg, c=c, s_=s_: e.matmul(ps[bank][:, pg * 32:(pg + 1) * 32], lhsT=KTs[:, c, pg * 128:(pg + 1) * 128], rhs=Qbd[:, c, s_, :],
                                                                start=(pg == 0 and c == 0), stop=False, skip_group_check=True),
                             r=[bkt, bq], w=[psb[bank]])
            zs(0)
            if is_sb:
                K.op("act", lambda e: e.activation(out=e_s, in_=ps[0][:], func=AF.Exp), r=[psb[0]], w=[be])
                K.op("act", lambda e: e.activation(out=sp_s, in_=e_s, func=AF.Ln, bias=1.0), r=[be], w=[bsp])
                zs(1)
                K.op("pe", lambda e: e.matmul(ps[1][:], lhsT=ntri, rhs=sp_s, start=False, stop=False, skip_group_check=True), r=[bsp, b_cbf], w=[psb[1]])
                for pg in range(1, 16):
                    K.op("pe", lambda e, pg=pg: e.matmul(ps[1][:, 0:pg * 32].rearrange("p (a b) -> p a b", b=32), lhsT=negones[:],
                                                       rhs=sp_s[:, pg * 32:(pg + 1) * 32].unsqueeze(1).broadcast_to([128, pg, 32]),
                                                       start=False, stop=False, skip_group_check=True), r=[bsp, b_ones], w=[psb[1]])
                K.op("pe", lambda e, s_=s_: e.matmul(ps[1][:].rearrange("p (a b) -> p a b", b=32), lhsT=negones[:],
                                              rhs=spn[:, s_ * 32:(s_ + 1) * 32].unsqueeze(1).broadcast_to([128, 16, 32]),
                                              start=False, stop=True, skip_group_check=True), r=[bn, b_ones], w=[psb[1]])
                K.op("act", lambda e: e.activation(out=A_s, in_=ps[1][:], func=AF.Exp), r=[psb[1]], w=[bA])
            else:
                for pg in range(16):
                    K.op("pe", lambda e, pg=pg: e.matmul(ps[0][:, pg * 32:(pg + 1) * 32], lhsT=IndN[0:8, pg, :], rhs=biasTs[0:8, :],
                                                       start=False, stop=(pg == 15), skip_group_check=True), r=[bq, bbt], w=[psb[0]])
                K.op("act", lambda e: e.activation(out=A_s, in_=ps[0][:], func=AF.Exp), r=[psb[0]], w=[bA])
            for pg in range(17):
                for h in range(8):
                    c = h // 2
                    if pg < 16:
                        lh, rh, rr = Vpg[par][pg][:, c * 128:(c + 1) * 128], A_s[:, pg * 32 + h * 4:pg * 32 + h * 4 + 4], [bvg, bA]
                    else:
                        lh, rh, rr = sm["V"][:, c * 128:(c + 1) * 128], An[:, s_ * 32 + h * 4:s_ * 32 + h * 4 + 4], [sm["b"], bn]
                    K.op("pe", lambda e, lh=lh, rh=rh, h=h, st=first_o[0], s_=s_: e.matmul(ps[2][:, s_ * 32 + h * 4:s_ * 32 + h * 4 + 4], lhsT=lh, rhs=rh,
                                                                                  start=st, stop=False, skip_group_check=True), r=rr, w=[psb[2]])
                    first_o[0] = False
                if not is_sb:
                    rh = A_s[:, pg * 32:(pg + 1) * 32] if pg < 16 else An[:, s_ * 32:(s_ + 1) * 32]
                    K.op("pe", lambda e, rh=rh, st=(s_ == 0 and pg == 0), s_=s_: e.matmul(ps[3][:, s_ * 32:(s_ + 1) * 32], lhsT=ones_bf[:], rhs=rh,
                                                                                 start=st, stop=False, skip_group_check=True),
                         r=[bA, bn, b_ones], w=[psb[3]])
        ncol = NSEQ * 32
        if not is_sb:
            K.op("dve", lambda e: e.reciprocal(out=rl_s[:, 0:ncol], in_=ps[3][:, 0:ncol]), r=[psb[3]], w=[brl])
        for h in range(8):
            c, p0 = h // 2, (h % 2) * 64
            dst = o_T[p0:p0 + 64, c, S:S + 4 * NSEQ].rearrange("p (s t) -> p s t", t=4)
            src = ps[2][p0:p0 + 64, 0:ncol].rearrange("p (s x) -> p s x", x=32)[:, :, h * 4:(h + 1) * 4]
            if is_sb:
                K.op("dve", lambda e, dst=dst, src=src: e.tensor_copy(out=dst, in_=src), r=[psb[2]], w=[b_oT])
            else:
                rsrc = rl_s[p0:p0 + 64, 0:ncol].rearrange("p (s x) -> p s x", x=32)[:, :, h * 4:(h + 1) * 4]
                K.op("dve", lambda e, dst=dst, src=src, rsrc=rsrc: e.tensor_tensor(out=dst, in0=src, in1=rsrc, op=ALU.mult), r=[psb[2], brl], w=[b_oT])
        K.op("pool", lambda e: e.memset(o_T[:, :, S + 4 * NSEQ:NTOK], 0.0), w=[b_oT])
        K.barrier()

    ones256b = sb("ones256b", [128, 2], BF16)
    K.op("pool", lambda e: e.memset(ones256b[:], 1.0 / 256.0), w=[b_ones])
    if STAGE >= 4 and os.environ.get("KBR", "sb") in ("sb", "both") or (STAGE >= 4 and not SIM):
        sample_attn("sb")

    QmT, KmT, Vm = QsT, KsT, Vs
    b_QmT, b_KmT, b_Vm = b_QsT, b_KsT, b_Vs
    kmf = [sb(f"kmf{j}", [128, 512], F32) for j in range(2)]
    b_kmf = [Buf(f"kmf{j}") for j in range(2)]
    rot = sb("rot", [128, 4, 64], F32)
    b_rot = Buf("rot")
    ones256 = sb("ones256", [128, 2], F32)
    identf = cst[:, C_ID:C_ID + 128]
    kmT = sb("kmT", [128, 4, 8], F32)
    b_kmT = Buf("kmT")
    biasT = sb("biasT", [128, S], BF16)
    b_biasT = [Buf(f"biasT{i}") for i in range(16)]
    b_selm = Buf("selm")
    qTf = sb("qTf", [128, 4, 128], F32)
    b_qTf = Buf("qTf")
    sblk = sb("sblk", [128, 64], F32)
    mx8 = sb("mx8", [128, 64], F32)
    bias01 = sb("bias01", [128, 64], BF16)
    b_sblk, b_mx8, b_bias01 = Buf("sblk"), Buf("mx8"), Buf("bias01")
    id30k = sb("id30k", [64, 64], BF16)

    def rotary(t, b_t, i):
        tv = t[:].rearrange("p (h d) -> p h d", h=8)
        x1, x2 = tv[:, :, 0:8], tv[:, :, 8:16]
        cs = cosT[:, i, :].unsqueeze(1).broadcast_to([128, 8, 8])
        sn = sinT[:, i, :].unsqueeze(1).broadcast_to([128, 8, 8])
        K.op("dve", lambda e: e.tensor_tensor(out=rot[:, 0, :].rearrange("p (h f) -> p h f", h=8), in0=x1, in1=cs, op=ALU.mult), r=[b_t, b_cst], w=[b_rot])
        K.op("dve", lambda e: e.tensor_tensor(out=rot[:, 1, :].rearrange("p (h f) -> p h f", h=8), in0=x2, in1=sn, op=ALU.mult), r=[b_t, b_cst], w=[b_rot])
        K.op("dve", lambda e: e.tensor_tensor(out=rot[:, 2, :].rearrange("p (h f) -> p h f", h=8), in0=x1, in1=sn, op=ALU.mult), r=[b_t, b_cst], w=[b_rot])
        K.op("dve", lambda e: e.tensor_tensor(out=rot[:, 3, :].rearrange("p (h f) -> p h f", h=8), in0=x2, in1=cs, op=ALU.mult), r=[b_t, b_cst], w=[b_rot])
        K.op("dve", lambda e: e.tensor_tensor(out=x1, in0=rot[:, 0, :].rearrange("p (h f) -> p h f", h=8),
                                              in1=rot[:, 1, :].rearrange("p (h f) -> p h f", h=8), op=ALU.subtract), r=[b_rot], w=[b_t])
        K.op("dve", lambda e: e.tensor_tensor(out=x2, in0=rot[:, 2, :].rearrange("p (h f) -> p h f", h=8),
                                              in1=rot[:, 3, :].rearrange("p (h f) -> p h f", h=8), op=ALU.add), r=[b_rot], w=[b_t])

    if STAGE >= 3:
        K.op("pool", lambda e: e.memset(ones256[:], 1.0 / 256.0), w=[b_ones])
        K.op("dve", lambda e: e.tensor_scalar(out=id30k[:], in0=cst[0:64, C_ID:C_ID + 64], scalar1=30000.0, scalar2=None, op0=ALU.mult),
             r=[b_cst], w=[b_selm])
        K.op("pool", lambda e: e.memset(biasT[0:64, 0:256], 0.0), w=[b_biasT[0], b_biasT[1]])
        K.op("pool", lambda e: e.memset(biasT[64:128, :], 0.0), w=[b_selm])
        for (c, kind) in ((4, "k"), (5, "v"), (3, "q")):
            j = load_w(c)
            for i in range(NT):
                pb = project(c, i, j)
                if kind == "v":
                    a = cnt["hf"] % 2
                    cnt["hf"] += 1
                    K.op("act", lambda e, a=a, pb=pb: e.activation(out=hf[a][:], in_=ps[pb][:], func=AF.Copy), r=[psb[pb]], w=[b_hf[a]])
                    kv_store(3, i, hf[a], b_hf[a])
                    if i == 16:
                        K.op("dve", lambda e, pb=pb: e.tensor_copy(out=samp["mb"]["V"][:], in_=ps[pb][:]), r=[psb[pb]], w=[samp["mb"]["b"]])
                    else:
                        K.op("dve", lambda e, i=i, pb=pb: e.tensor_copy(out=Vm[:, i, :], in_=ps[pb][:]), r=[psb[pb]], w=[b_Vm[i]])
                    continue
                a = i % 2
                K.op("act", lambda e, a=a, pb=pb, kind=kind: e.activation(out=kmf[a][:], in_=ps[pb][:], func=AF.Copy,
                                                                         scale=(1.0 if kind == "k" else 0.125)),
                     r=[psb[pb]], w=[b_kmf[a]])
                rotary(kmf[a], b_kmf[a], i)
                b = cnt["hb"] % 2
                cnt["hb"] += 1
                K.op("pool", lambda e, a=a, b=b: e.tensor_copy(out=hb[b][:], in_=kmf[a][:]), r=[b_kmf[a]], w=[b_hb[b]])
                if kind == "k":
                    kv_store(2, i, kmf[a], b_kmf[a])
                    if i == 16:
                        to_T(hb[b], b_hb[b], None, samp["mb"]["b"], i, 4 + i % 2, dst_ap=samp["mb"]["KT"][:])
                    else:
                        to_T(hb[b], b_hb[b], KmT, b_KmT[i], i, 4 + i % 2)
                    if i < 16 and i % 2 == 1:
                        n = i // 2
                        for ch in range(4):
                            for u in range(2):
                                src = kmf[1 - a] if u == 0 else kmf[a]
                                K.op("pe", lambda e, ch=ch, u=u, src=src, n=n: e.matmul(ps[6][:, ch * 8 + n:ch * 8 + n + 1],
                                                                                    lhsT=src[:, ch * 128:(ch + 1) * 128],
                                                                                    rhs=ones256[:, 0:1], start=(u == 0), stop=(u == 1)),
                                     r=[b_kmf[0], b_kmf[1], b_ones], w=[psb[6]])
                        if i == 15:
                            K.op("dve", lambda e: e.tensor_copy(out=kmT[:], in_=ps[6][:, 0:32].rearrange("p (c n) -> p c n", c=4)),
                                 r=[psb[6]], w=[b_kmT])
                else:
                    if i == 16:
                        to_T(hb[b], b_hb[b], None, samp["mb"]["b"], i, 4 + i % 2, dst_ap=samp["mb"]["QT"][:])
                        for cc in range(4):
                            K.op("pe", lambda e, cc=cc, a=a: e.transpose(out=ps[7][:, cc * 128:(cc + 1) * 128], in_=kmf[a][:, cc * 128:(cc + 1) * 128],
                                                                      identity=identf), r=[b_kmf[a], b_cst], w=[psb[7]])
                        K.op("act", lambda e: e.activation(out=sQTf[:], in_=ps[7][:].rearrange("p (c t) -> p c t", c=4), func=AF.Copy),
                             r=[psb[7]], w=[b_sQTf])
                    else:
                        to_T(hb[b], b_hb[b], QmT, b_QmT[i], i, 4 + i % 2)
                    own = i // 2
                    if i < 16 and own >= 1:
                        for cc in range(4):
                            K.op("pe", lambda e, cc=cc, a=a: e.transpose(out=ps[7][:, cc * 128:(cc + 1) * 128], in_=kmf[a][:, cc * 128:(cc + 1) * 128],
                                                                      identity=identf), r=[b_kmf[a], b_cst], w=[psb[7]])
                        K.op("act", lambda e: e.activation(out=qTf[:], in_=ps[7][:].rearrange("p (c t) -> p c t", c=4), func=AF.Copy),
                             r=[psb[7]], w=[b_qTf])
                        for h in range(8):
                            c2, p0 = h // 2, (h % 2) * 64
                            K.op("pe", lambda e, h=h, c2=c2, p0=p0: e.matmul(ps[6][:, 64 + h * 8:64 + h * 8 + 8], lhsT=qTf[p0:p0 + 64, c2, :],
                                                                         rhs=kmT[p0:p0 + 64, c2, :], start=True, stop=True),
                                 r=[b_qTf, b_kmT], w=[psb[6]])
                        K.op("dve", lambda e: e.tensor_copy(out=sblk[:], in_=ps[6][:, 64:128]), r=[psb[6]], w=[b_sblk])
                        if own < 8:
                            K.op("dve", lambda e, own=own: e.memset(sblk[:].rearrange("p (h n) -> p h n", h=8)[:, :, own:8], -1e30), w=[b_sblk])
                        for h in range(8):
                            K.op("dve", lambda e, h=h: e.max(out=mx8[:, h * 8:(h + 1) * 8], in_=sblk[:, h * 8:(h + 1) * 8]), r=[b_sblk], w=[b_mx8])
                        ti = min(own, 3) - 1
                        for h in range(8):
                            K.op("dve", lambda e, h=h, ti=ti: e.tensor_scalar(out=bias01[:, h * 8:(h + 1) * 8], in0=sblk[:, h * 8:(h + 1) * 8],
                                                                          scalar1=mx8[:, h * 8 + ti:h * 8 + ti + 1], scalar2=1.0,
                                                                          op0=ALU.is_ge, op1=ALU.subtract),
                                 r=[b_sblk, b_mx8], w=[b_bias01])
                        if own < 8:
                            K.op("dve", lambda e, own=own: e.memset(bias01[:].rearrange("p (h n) -> p h n", h=8)[:, :, own:own + 1], 0.0), w=[b_bias01])
                        pstb = ps[7].bitcast(BF16)
                        K.op("pe", lambda e: e.transpose(out=pstb[0:64, 0:128], in_=bias01[:], identity=ident), r=[b_bias01, b_cbf], w=[psb[7]])
                        K.op("dve", lambda e, i=i: e.tensor_copy(out=biasT[0:64, i * 128:(i + 1) * 128], in_=pstb[0:64, 0:128]), r=[psb[7]], w=[b_biasT[i]])

    o_mT = sb("o_mT", [128, 4, NTOK], BF16)
    b_omT = Buf("o_mT")
    rl = xt[1][:, 0:512]
    b_rl = b_xt[1]

    def mb_P(t):
        h, g, j = steps_m[t]
        a = t % 2
        c, p0 = h // 2, (h % 2) * 64
        q0 = max(j - 4 * g, 0) * 128
        r_ = h * 8 + j // 2
        K.op("pe", lambda e: e.matmul(ps[a][:, q0:512], lhsT=KmT[p0:p0 + 64, c, j * 128:(j + 1) * 128],
                                      rhs=QmT[p0:p0 + 64, c, g * 512 + q0:(g + 1) * 512], start=True, stop=False),
             r=[b_KmT[j]] + [b_QmT[4 * g + u] for u in range(4)], w=[psb[a]])
        K.op("pe", lambda e: e.matmul(ps[a][:, q0:512], lhsT=selm[r_ // 32][:, r_ % 32, :], rhs=biasT[:, g * 512 + q0:(g + 1) * 512],
                                      start=False, stop=(j < 4 * g)),
             r=[b_wst[r_ // 32], b_selm] + [b_biasT[4 * g + u] for u in range(4)], w=[psb[a]])
        if j >= 4 * g:
            K.op("pe", lambda e: e.matmul(ps[a][:, q0:q0 + 128], lhsT=ident, rhs=mmb, start=False, stop=True), r=[b_cbf], w=[psb[a]])
        K.op("act", lambda e: e.activation(out=A_sb[a][:, q0:], in_=ps[a][:, q0:], func=AF.Exp), r=[psb[a]], w=[b_A[a]])

    def mb_AV(t):
        h, g, j = steps_m[t]
        a = t % 2
        c, p0 = h // 2, (h % 2) * 64
        q0 = max(j - 4 * g, 0) * 128
        ob = 4 + (h * 4 + g) % 2
        lb = 2 + (h * 4 + g) % 2
        first, last = (j == 0), (j == 4 * g + 3)
        K.op("pe", lambda e: e.matmul(ps[ob][:, q0:], lhsT=Vm[:, j, c * 128:(c + 1) * 128], rhs=A_sb[a][:, q0:], start=first, stop=last),
             r=[b_A[a], b_Vm[j]], w=[psb[ob]])
        K.op("pe", lambda e: e.matmul(ps[lb][:, q0:], lhsT=ones_bf[:], rhs=A_sb[a][:, q0:], start=first, stop=last),
             r=[b_A[a], b_ones], w=[psb[lb]])
        if last:
            K.op("dve", lambda e: e.reciprocal(out=rl[p0:p0 + 64, :], in_=ps[lb][p0:p0 + 64, :]), r=[psb[lb]], w=[b_rl])
            K.op("dve", lambda e: e.tensor_tensor(out=o_mT[p0:p0 + 64, c, g * 512:(g + 1) * 512], in0=ps[ob][p0:p0 + 64, :],
                                                  in1=rl[p0:p0 + 64, :], op=ALU.mult), r=[psb[ob], b_rl], w=[b_omT])

    selm = [wst[u][:].rearrange("p a b -> p (a b)").bitcast(BF16).rearrange("p (r m) -> p r m", r=32) for u in range(2)]
    if STAGE >= 3:
        for u in range(2):
            K.op("dve", lambda e, u=u: e.tensor_copy(out=selm[u][0:64], in_=id30k[:, 32 * u:32 * u + 32].unsqueeze(2).broadcast_to([64, 32, 128])),
                 r=[b_selm], w=[b_wst[u]])
            K.op("dve", lambda e, u=u: e.memset(selm[u][64:128], 0.0), w=[b_wst[u]])
    steps_m = []
    for h in range(8):
        for g in range(4):
            for j in range(0, 4 * g + 4):
                steps_m.append((h, g, j))
    if STAGE >= 3 and os.environ.get('KB2', '1') == '1':
        mb_P(0)
        for t in range(len(steps_m)):
            if t + 1 < len(steps_m):
                mb_P(t + 1)
            mb_AV(t)
        K.dma("sp", lambda e: e.dma_start(out=dbg_o[1, :, :, 0:S], in_=o_mT[:, :, 0:S]), r=[b_omT])

    if STAGE >= 4 and (not SIM or os.environ.get("KBR", "sb") in ("mb", "both")):
        sample_attn("mb")
    if STAGE >= 4:
        if SIM and os.environ.get("KBR", "sb") == "sb":
            K.op("pool", lambda e: e.memset(o_mT[:, :, S:NTOK], 0.0), w=[b_omT])
        if SIM and os.environ.get("KBR", "sb") == "mb":
            K.op("pool", lambda e: e.memset(o_sT[:, :, S:NTOK], 0.0), w=[b_osT])
        K.dma("sp", lambda e: e.dma_start(out=dbg_o[0, :, :, S:NTOK], in_=o_sT[:, :, S:NTOK]), r=[b_osT])
        K.dma("sp", lambda e: e.dma_start(out=dbg_o[1, :, :, S:NTOK], in_=o_mT[:, :, S:NTOK]), r=[b_omT])

    if STAGE >= 5:
        K.barrier()
        mT = [QsT, KsT]
        bm_T = [Buf(f"mT{i}") for i in range(NT)]
        Wps_h = view(Vs, BF16, 0, [128, 4, 512])
        Wpm_h = view(Vs, BF16, 4096, [128, 4, 512])
        sg = [view(Vs, F32, 8192, [128, 512]), view(Vs, F32, 10240, [128, 512])]
        mm_ = [view(Vs, F32, 12288, [128, 512]), view(Vs, F32, 14336, [128, 512])]
        bWp, bsg, bmm = Buf("Wp"), [Buf("sg0"), Buf("sg1")], [Buf("mm0"), Buf("mm1")]
        bwst2, bwb2 = [Buf("wst0c"), Buf("wst1c")], [Buf("wb0c"), Buf("wb1c")]

        def load_any(src_halves, dsts, bdst, engs=("pool", "pool")):
            for hh, (src, dst) in enumerate(zip(src_halves, dsts)):
                u = hh % 2
                shp = list(src.shape)
                stv = view(wst[u], F32, 0, shp)
                K.dma("sp", lambda e, src=src, stv=stv: e.dma_start(out=stv, in_=src), w=[bwst2[u]])
                if engs[hh] == "pool":
                    K.op("pool", lambda e, dst=dst, stv=stv: e.tensor_copy(out=dst, in_=stv), r=[bwst2[u]], w=[bdst])
                else:
                    K.op("act", lambda e, dst=dst, stv=stv: e.activation(out=dst, in_=stv, func=AF.Copy), r=[bwst2[u]], w=[bdst])

        w_ps_v = w_ps.rearrange("(k p) n -> p k n", p=128)
        w_pm_v = w_pm.rearrange("(k p) n -> p k n", p=128)
        w_o_v = w_o.rearrange("(k p) n -> p k n", p=128)
        mb16 = hb
        for cb in range(2):
            load_any([w_in_v[:, 0:4, (6 + cb) * 512:(7 + cb) * 512], w_in_v[:, 4:8, (6 + cb) * 512:(7 + cb) * 512]],
                     [wb[0][:, 0:4, :], wb[0][:, 4:8, :]], bwb2[0])
            load_any([w_in_v[:, 0:4, (8 + cb) * 512:(9 + cb) * 512], w_in_v[:, 4:8, (8 + cb) * 512:(9 + cb) * 512]],
                     [wb[1][:, 0:4, :], wb[1][:, 4:8, :]], bwb2[1])
            load_any([w_ps_v[:, :, cb * 512:(cb + 1) * 512], w_pm_v[:, :, cb * 512:(cb + 1) * 512]], [Wps_h, Wpm_h], bWp)
            for i in range(NT):
                tsl = slice(i * 128, (i + 1) * 128)
                for kc in range(8):
                    K.op("pe", lambda e, kc=kc, tsl=tsl: e.matmul(ps[0][:], lhsT=xnT[:, kc, tsl], rhs=wb[0][:, kc, :], start=(kc == 0), stop=(kc == 7)),
                         r=[b_xnT[i], bwb2[0]], w=[psb[0]])
                for kc in range(8):
                    K.op("pe", lambda e, kc=kc, tsl=tsl: e.matmul(ps[1][:], lhsT=xnT[:, kc, tsl], rhs=wb[1][:, kc, :], start=(kc == 0), stop=(kc == 7)),
                         r=[b_xnT[i], bwb2[1]], w=[psb[1]])
                for c in range(4):
                    K.op("pe", lambda e, c=c, tsl=tsl: e.matmul(ps[2][:], lhsT=o_sT[:, c, tsl], rhs=Wps_h[:, c, :], start=(c == 0), stop=(c == 3)),
                         r=[b_osT, bWp], w=[psb[2]])
                for c in range(4):
                    K.op("pe", lambda e, c=c, tsl=tsl: e.matmul(ps[3][:], lhsT=o_mT[:, c, tsl], rhs=Wpm_h[:, c, :], start=(c == 0), stop=(c == 3)),
                         r=[b_omT, bWp], w=[psb[3]])
                K.op("act", lambda e: e.activation(out=sg[0], in_=ps[0][:], func=AF.Sigmoid), r=[psb[0]], w=[bsg[0]])
                K.op("act", lambda e: e.activation(out=sg[1], in_=ps[1][:], func=AF.Sigmoid), r=[psb[1]], w=[bsg[1]])
                K.op("dve", lambda e: e.tensor_tensor(out=mm_[0], in0=sg[0], in1=ps[2][:], op=ALU.mult), r=[bsg[0], psb[2]], w=[bmm[0]])
                K.op("dve", lambda e: e.tensor_tensor(out=mm_[1], in0=sg[1], in1=ps[3][:], op=ALU.mult), r=[bsg[1], psb[3]], w=[bmm[1]])
                b = i % 2
                K.op("pool", lambda e, b=b: e.tensor_tensor(out=mb16[b][:], in0=mm_[0], in1=mm_[1], op=ALU.add), r=bmm, w=[b_hb[b]])
                to_T(mb16[b], b_hb[b], mT[cb], bm_T[i], i, 4 + i % 2)

        K.barrier()
        load_any([w_o_v[:, 0:4, 0:512], w_o_v[:, 4:8, 0:512]], [wb[0][:, 0:4, :], wb[0][:, 4:8, :]], bwb2[0])
        load_any([w_o_v[:, 0:4, 512:1024], w_o_v[:, 4:8, 512:1024]], [wb[1][:, 0:4, :], wb[1][:, 4:8, :]], bwb2[1])
        bg2 = Buf("gbc2")
        K.dma("sp", lambda e: e.dma_start(out=gbc[:], in_=g_ffn.partition_broadcast(128)), w=[bg2])
        xn2f = view(Vs, F32, 0, [128, D])
        xTf = view(Vs, F32, 4096, [128, 8, 128])
        combT = view(Vs, BF16, 8192, [128, NTOK])
        SelE = view(Vs, BF16, 8192 + 2 * NTOK, [128, 16, 128])
        bx2, bxT, bcT, bSel = Buf("xn2f"), Buf("xTf"), Buf("combT"), Buf("SelE")
        wr = sb("wr", [128, 8, 20], F32)
        brb = sb("brb", [128, 20], F32)
        bwr = Buf("wr")
        K.dma("sp", lambda e: e.dma_start(out=wr[:], in_=w_r.rearrange("(k p) n -> p k n", p=128)), w=[bwr])
        K.dma("sp", lambda e: e.dma_start(out=brb[:], in_=b_r.partition_broadcast(128)), w=[bwr])
        rt = sb("rt", [128, 64], F32)
        comb = sb("comb", [128, 16], F32)
        combb = sb("combb", [128, 16], BF16)
        brt = Buf("rt")
        b_x1d = [Buf(f"x1d{i}") for i in range(NT)]
        K.op("dve", lambda e: e.tensor_copy(out=SelE[0:16], in_=ident[0:16, 0:16].unsqueeze(2).broadcast_to([16, 16, 128])), r=[b_cbf], w=[bSel])
        lgs, ohg, esel, msk1, e2, msk2, c4 = rt[:, 0:20], rt[:, 20:24], rt[:, 24:28], rt[:, 28:32], rt[:, 32:36], rt[:, 36:40], rt[:, 40:44]
        sc = lambda k: rt[:, 48 + k:49 + k]
        eg = rt[:, 44:48]

        def R(fn, eng="dve"):
            K.op(eng, fn, r=[brt], w=[brt])

        for i in range(NT):
            j = i % 2
            tsl = slice(i * 128, (i + 1) * 128)
            K.dma("sp", lambda e, j=j, tsl=tsl: e.dma_start(out=xt[j][:], in_=xin[tsl, :]), w=[b_xt[j]])
            for half in range(2):
                for kc in range(8):
                    K.op("pe", lambda e, kc=kc, half=half, tsl=tsl: e.matmul(ps[half][:], lhsT=mT[kc // 4][:, kc % 4, tsl], rhs=wb[half][:, kc, :],
                                                                           start=(kc == 0), stop=(kc == 7)), r=[bm_T[i], bwb2[half]], w=[psb[half]])
                K.op("dve", lambda e, j=j, half=half: e.tensor_tensor(out=xt[j][:, half * 512:(half + 1) * 512], in0=xt[j][:, half * 512:(half + 1) * 512],
                                                                    in1=ps[half][:], op=ALU.add), r=[b_xt[j], psb[half]], w=[b_xt[j]])
            K.dma("sp", lambda e, j=j, tsl=tsl: e.dma_start(out=y_out[tsl, :], in_=xt[j][:]), r=[b_xt[j]], w=[b_x1d[i]])
            K.op("act", lambda e, i=i, j=j: e.activation(out=junk[:], in_=xt[j][:], func=AF.Square, accum_out=ss[:, i:i + 1]),
                 r=[b_xt[j]], w=[b_junk, b_ss[i]])
            K.op("act", lambda e, i=i: e.activation(out=rs[:, i:i + 1], in_=ss[:, i:i + 1], func=AF.Sqrt, bias=1e-6, scale=1.0 / D), r=[b_ss[i]], w=[b_rs[i]])
            K.op("dve", lambda e, i=i: e.reciprocal(out=rstd[:, i:i + 1], in_=rs[:, i:i + 1]), r=[b_rs[i]], w=[b_rstd[i]])
            K.op("dve", lambda e, i=i, j=j: e.scalar_tensor_tensor(out=xn2f, in0=xt[j][:], scalar=rstd[:, i:i + 1], in1=gbc[:], op0=ALU.mult, op1=ALU.mult),
                 r=[b_xt[j], b_rstd[i], bg2], w=[bx2])
            K.op("pool", lambda e, j=j: e.tensor_copy(out=xn[j][:], in_=xn2f), r=[bx2], w=[b_xn[j]])
            pst = ps[2].bitcast(BF16)
            for kc in range(8):
                K.op("pe", lambda e, kc=kc, j=j, pst=pst: e.transpose(out=pst[:, kc * 128:(kc + 1) * 128], in_=xn[j][:, kc * 128:(kc + 1) * 128], identity=ident),
                     r=[b_xn[j], b_cbf], w=[psb[2]])
            K.op("act", lambda e, tsl=tsl, pst=pst: e.activation(out=xnT[:, :, tsl], in_=pst.rearrange("p (k t) -> p k t", k=8), func=AF.Copy),
                 r=[psb[2]], w=[b_xnT[i]])
            for half in range(2):
                for kk in range(4):
                    kc = half * 4 + kk
                    K.op("pe", lambda e, kc=kc, kk=kk, half=half: e.transpose(out=ps[3 + half][:, kk * 128:(kk + 1) * 128], in_=xn2f[:, kc * 128:(kc + 1) * 128], identity=identf),
                         r=[bx2, b_cst], w=[psb[3 + half]])
                K.op("act" if half == 0 else "dve",
                     (lambda e, half=half: e.activation(out=xTf[:, half * 4:half * 4 + 4, :], in_=ps[3 + half][:].rearrange("p (k t) -> p k t", k=4), func=AF.Copy)) if half == 0 else
                     (lambda e, half=half: e.tensor_copy(out=xTf[:, half * 4:half * 4 + 4, :], in_=ps[3 + half][:].rearrange("p (k t) -> p k t", k=4))),
                     r=[psb[3 + half]], w=[bxT])
            for kc in range(8):
                K.op("pe", lambda e, kc=kc: e.matmul(ps[5][:, 0:20], lhsT=xTf[:, kc, :], rhs=wr[:, kc, :], start=(kc == 0), stop=(kc == 7)),
                     r=[bxT, bwr], w=[psb[5]])
            K.op("dve", lambda e: e.tensor_tensor(out=lgs, in0=ps[5][:, 0:20], in1=brb[:], op=ALU.add), r=[psb[5], bwr, brt], w=[brt])
            R(lambda e: e.tensor_reduce(out=sc(0), in_=lgs[:, 0:4], axis=AX.X, op=ALU.max))
            R(lambda e: e.tensor_scalar(out=ohg, in0=lgs[:, 0:4], scalar1=sc(0), scalar2=None, op0=ALU.is_ge))
            R(lambda e: e.tensor_scalar(out=sc(1), in0=sc(0), scalar1=-1.0, scalar2=None, op0=ALU.mult))
            R(lambda e: e.activation(out=eg, in_=lgs[:, 0:4], func=AF.Exp, bias=sc(1), accum_out=sc(2)), "act")
            R(lambda e: e.reciprocal(out=sc(3), in_=sc(2)))
            R(lambda e: e.tensor_scalar(out=esel, in0=lgs[:, 4:8], scalar1=ohg[:, 0:1], scalar2=None, op0=ALU.mult))
            for g in range(1, 4):
                R(lambda e, g=g: e.scalar_tensor_tensor(out=esel, in0=lgs[:, 4 + 4 * g:8 + 4 * g], scalar=ohg[:, g:g + 1], in1=esel, op0=ALU.mult, op1=ALU.add))
            R(lambda e: e.tensor_reduce(out=sc(4), in_=esel, axis=AX.X, op=ALU.max))
            R(lambda e: e.tensor_scalar(out=msk1, in0=esel, scalar1=sc(4), scalar2=None, op0=ALU.is_ge))
            R(lambda e: e.scalar_tensor_tensor(out=e2, in0=msk1, scalar=-1e30, in1=esel, op0=ALU.mult, op1=ALU.add))
            R(lambda e: e.tensor_reduce(out=sc(5), in_=e2, axis=AX.X, op=ALU.max))
            R(lambda e: e.tensor_scalar(out=msk2, in0=e2, scalar1=sc(5), scalar2=None, op0=ALU.is_ge))
            R(lambda e: e.tensor_tensor(out=sc(6), in0=sc(5), in1=sc(4), op=ALU.subtract))
            R(lambda e: e.activation(out=sc(7), in_=sc(6), func=AF.Exp), "act")
            R(lambda e: e.tensor_scalar(out=sc(8), in0=sc(7), scalar1=1.0, scalar2=None, op0=ALU.add))
            R(lambda e: e.reciprocal(out=sc(9), in_=sc(8)))
            R(lambda e: e.tensor_tensor(out=sc(10), in0=sc(3), in1=sc(9), op=ALU.mult))
            R(lambda e: e.tensor_tensor(out=sc(11), in0=sc(10), in1=sc(7), op=ALU.mult))
            R(lambda e: e.tensor_scalar(out=c4, in0=msk1, scalar1=sc(10), scalar2=None, op0=ALU.mult))
            R(lambda e: e.scalar_tensor_tensor(out=c4, in0=msk2, scalar=sc(11), in1=c4, op0=ALU.mult, op1=ALU.add))
            for g in range(4):
                R(lambda e, g=g: e.tensor_scalar(out=comb[:, 4 * g:4 * g + 4], in0=c4, scalar1=ohg[:, g:g + 1], scalar2=None, op0=ALU.mult))
            R(lambda e: e.tensor_copy(out=combb[:], in_=comb[:]))
            pst5 = ps[5].bitcast(BF16)
            K.op("pe", lambda e: e.transpose(out=pst5[0:16, 128:256], in_=combb[:], identity=ident), r=[brt, b_cbf], w=[psb[5]])
            K.op("dve", lambda e, tsl=tsl: e.tensor_copy(out=combT[0:16, tsl], in_=pst5[0:16, 128:256]), r=[psb[5]], w=[bcT])

        K.barrier()
        bg3 = Buf("gbc3")
        K.dma("sp", lambda e: e.dma_start(out=gbc[:], in_=g_fin.partition_broadcast(128)), w=[bg3])
        hdn = [view(QsT, BF16, 0, [128, 16, 512]), view(KsT, BF16, 0, [128, 16, 512])]
        bhd = Buf("hdn")
        sa = [view(xn[0], F32, 0, [128, 512]), view(xn[1], F32, 0, [128, 512])]
        tt = [xn2f[:, 0:512], xn2f[:, 512:1024]]
        bsa, btt = [Buf("sa0"), Buf("sa1")], [Buf("tt0"), Buf("tt1")]
        wdb = [view(o_sT, BF16, 0, [128, 2, D]), view(o_mT, BF16, 0, [128, 2, D])]
        bwd = [Buf("wd0"), Buf("wd1")]
        w_eg_v = w_eg.rearrange("e (k p) n -> e p k n", p=128)
        w_eu_v = w_eu.rearrange("e (k p) n -> e p k n", p=128)
        w_ed_v = w_ed.rearrange("e (k p) n -> e p k n", p=128)
        groups = [(g * 512, 512) for g in range(4)] + [(S, 128)]
        it_ = 0
        for (t0, nt) in groups:
            for ex in range(16):
                u = ex % 2
                load_any([w_eg_v[ex], w_eu_v[ex]], [wb[u][:, :, 0:256], wb[u][:, :, 256:512]], bwb2[u],
                         engs=(("pool", "act") if os.environ.get("KUP", "act") == "act" else ("pool", "pool")))
                K.op("pe", lambda e, ex=ex, t0=t0, nt=nt: e.matmul(ps[4][:, 0:nt], lhsT=SelE[0:16, ex, :], rhs=combT[0:16, t0:t0 + nt], start=True, stop=True),
                     r=[bSel, bcT], w=[psb[4]])
                for fc in range(2):
                    a = it_ % 2
                    it_ += 1
                    for kc in range(8):
                        K.op("pe", lambda e, kc=kc, u=u, fc=fc, a=a, t0=t0, nt=nt: e.matmul(ps[a][:, 0:nt], lhsT=wb[u][:, kc, fc * 128:(fc + 1) * 128],
                                                                                          rhs=xnT[:, kc, t0:t0 + nt], start=(kc == 0), stop=(kc == 7)),
                             r=[bwb2[u]] + [b_xnT[t0 // 128 + q] for q in range(nt // 128)], w=[psb[a]])
                    for kc in range(8):
                        K.op("pe", lambda e, kc=kc, u=u, fc=fc, a=a, t0=t0, nt=nt: e.matmul(ps[2 + a][:, 0:nt], lhsT=wb[u][:, kc, 256 + fc * 128:256 + (fc + 1) * 128],
                                                                                          rhs=xnT[:, kc, t0:t0 + nt], start=(kc == 0), stop=(kc == 7)),
                             r=[bwb2[u]] + [b_xnT[t0 // 128 + q] for q in range(nt // 128)], w=[psb[2 + a]])
                    K.op("act", lambda e, a=a, nt=nt: e.activation(out=sa[a][:, 0:nt], in_=ps[a][:, 0:nt], func=AF.Silu), r=[psb[a]], w=[bsa[a]])
                    K.op("dve", lambda e, a=a, nt=nt: e.tensor_tensor(out=tt[a][:, 0:nt], in0=sa[a][:, 0:nt], in1=ps[2 + a][:, 0:nt], op=ALU.mult),
                         r=[bsa[a], psb[2 + a]], w=[btt[a]])
                    ch = ex * 2 + fc
                    K.op("dve", lambda e, a=a, nt=nt, ch=ch: e.tensor_tensor(out=hdn[ch // 16][:, ch % 16, 0:nt], in0=tt[a][:, 0:nt], in1=ps[4][:, 0:nt], op=ALU.mult),
                         r=[btt[a], psb[4]], w=[bhd])
            ntile = nt // 128
            for ex in range(16):
                u = ex % 2
                stv = view(wst[u], F32, 0, [128, 2, D])
                K.dma("sp", lambda e, ex=ex, stv=stv: e.dma_start(out=stv, in_=w_ed_v[ex]), w=[bwst2[u]])
                K.op("act", lambda e, u=u, stv=stv: e.activation(out=wdb[u], in_=stv, func=AF.Copy), r=[bwst2[u]], w=[bwd[u]])
                for fc in range(2):
                    ch = ex * 2 + fc
                    for q in range(ntile):
                        for half in range(2):
                            K.op("pe", lambda e, ch=ch, q=q, half=half, u=u, fc=fc: e.matmul(ps[q * 2 + half][:], lhsT=hdn[ch // 16][:, ch % 16, q * 128:(q + 1) * 128],
                                                                                           rhs=wdb[u][:, fc, half * 512:(half + 1) * 512],
                                                                                           start=(ch == 0), stop=(ch == 31)),
                                 r=[bhd, bwd[u]], w=[psb[q * 2 + half]])
            for q in range(ntile):
                i = t0 // 128 + q
                j = i % 2
                tsl = slice(i * 128, (i + 1) * 128)
                K.dma("sp", lambda e, j=j, tsl=tsl: e.dma_start(out=xt[j][:], in_=y_out[tsl, :]), r=[b_x1d[i]], w=[b_xt[j]])
                for half in range(2):
                    K.op("dve", lambda e, j=j, half=half, q=q: e.tensor_tensor(out=xt[j][:, half * 512:(half + 1) * 512], in0=xt[j][:, half * 512:(half + 1) * 512],
                                                                             in1=ps[q * 2 + half][:], op=ALU.add), r=[b_xt[j], psb[q * 2 + half]], w=[b_xt[j]])
                K.op("act", lambda e, i=i, j=j: e.activation(out=junk[:], in_=xt[j][:], func=AF.Square, accum_out=ss[:, i:i + 1]),
                     r=[b_xt[j]], w=[b_junk, b_ss[i]])
                K.op("act", lambda e, i=i: e.activation(out=rs[:, i:i + 1], in_=ss[:, i:i + 1], func=AF.Sqrt, bias=1e-6, scale=1.0 / D), r=[b_ss[i]], w=[b_rs[i]])
                K.op("dve", lambda e, i=i: e.reciprocal(out=rstd[:, i:i + 1], in_=rs[:, i:i + 1]), r=[b_rs[i]], w=[b_rstd[i]])
                K.op("dve", lambda e, i=i, j=j: e.scalar_tensor_tensor(out=xt[j][:], in0=xt[j][:], scalar=rstd[:, i:i + 1], in1=gbc[:], op0=ALU.mult, op1=ALU.mult),
                     r=[b_xt[j], b_rstd[i], bg3], w=[b_xt[j]])
                K.dma("sp", lambda e, j=j, tsl=tsl: e.dma_start(out=y_out[tsl, :], in_=xt[j][:]), r=[b_xt[j], b_x1d[i]], w=[b_x1d[i]])

    K.emit()
    return nc


_NC = None


def kernel(**inputs):
    global _NC
    if _NC is None:
        _NC = build()
    nc = _NC
    xp = np.asarray(inputs["x_prompt"], np.float32)
    xs = np.asarray(inputs["x_sample"], np.float32)
    cst = make_consts()
    tok = np.arange(128)
    col = np.arange(512)
    s_of, t_of = col // 32, col % 4
    same = (tok[:, None] // 4 == s_of[None, :]) & (tok[:, None] < 64)
    m01 = np.stack([same & ((tok[:, None] % 4) < t_of[None, :]), same & ((tok[:, None] % 4) <= t_of[None, :])]).astype(np.float32)
    pt = np.asarray(inputs["page_table"], np.int32)
    in_maps = []
    for c in range(NCORES):
        xin = np.zeros((NTOK, D), np.float32)
        xin[:S] = xp[c]
        xin[S:S + 64] = xs[16 * c:16 * c + 16].reshape(64, D)
        in_maps.append({
            "xin": xin, "cst": cst,
            "g_mix": np.ascontiguousarray(inputs["g_mix"][0][None, :], np.float32),
            "w_in": np.ascontiguousarray(inputs["w_in"][0], np.float32),
            "m01": m01,
            "w_proj_sb": np.ascontiguousarray(inputs["w_proj_sb"][0], np.float32),
            "w_proj_moba": np.ascontiguousarray(inputs["w_proj_moba"][0], np.float32),
            "w_out": np.ascontiguousarray(inputs["w_out"][0], np.float32),
            "g_ffn": np.ascontiguousarray(inputs["g_ffn"][0][None, :], np.float32),
            "g_final": np.ascontiguousarray(np.asarray(inputs["g_final"])[None, :], np.float32),
            "w_router": np.ascontiguousarray(np.concatenate([inputs["w_router_group"][0], inputs["w_router_expert"][0]], axis=1), np.float32),
            "b_router": np.ascontiguousarray(np.concatenate([inputs["b_router_group"][0], inputs["b_router_expert"][0]])[None, :], np.float32),
            "w_expert_gate": np.ascontiguousarray(inputs["w_expert_gate"][0], np.float32),
            "w_expert_up": np.ascontiguousarray(inputs["w_expert_up"][0], np.float32),
            "w_expert_down": np.ascontiguousarray(inputs["w_expert_down"][0], np.float32),
        })
        m = in_maps[-1]
        if SMALL:
            rows = pt[16 * c:16 * c + 16].reshape(-1)
            for nm, key in (("sb_k", "cache_sb_k"), ("sb_v", "cache_sb_v"), ("mb_k", "cache_moba_k"), ("mb_v", "cache_moba_v")):
                m["cache_" + nm] = np.ascontiguousarray(inputs[key][0][rows].reshape(256 * 128, 512)) if (c == 0 or not SIM) else np.zeros((256 * 128, 512), np.float32)
            m["ptab"] = np.arange(256, dtype=np.int32)[None, :]
        else:
            for nm, key in (("sb_k", "cache_sb_k"), ("sb_v", "cache_sb_v"), ("mb_k", "cache_moba_k"), ("mb_v", "cache_moba_v")):
                m["cache_" + nm] = inputs[key][0].reshape(2560 * 128, 512)
            m["ptab"] = np.ascontiguousarray(pt[16 * c:16 * c + 16].reshape(1, 256))
    res = run_bass_kernel_spmd(nc, in_maps, core_ids=list(range(NCORES)), **({"trace": True} if os.environ.get("KTRACE") else {}))
    if os.environ.get("KTRACE"):
        print("EXEC_TIME_NS", res.exec_time_ns)
    outs = res.results
    global _LAST
    _LAST = outs
    kv = np.stack([o["kv_out"] for o in outs])
    y = np.stack([o["y_out"] for o in outs])
    y_prompt = y[:, :S, :]
    y_sample = y[:, S:S + 64, :].reshape(128, 4, D)
    p = [kv[:, k, :S, :].reshape(1, 8, S, 8, 64) for k in range(4)]
    s = [kv[:, k, S:S + 64, :].reshape(1, 128, 4, 8, 64) for k in range(4)]
    return (y_prompt, y_sample, p[0], p[1], p[2], p[3], s[0], s[1], s[2], s[3])
```

```python
import numpy as np
import concourse.bass as bass
import concourse.mybir as mybir
from concourse.bass_utils import run_bass_kernel_spmd

F32 = mybir.dt.float32
BF16 = mybir.dt.bfloat16
I32 = mybir.dt.int32
U32 = mybir.dt.uint32
AF = mybir.ActivationFunctionType
ALU = mybir.AluOpType
AX = mybir.AxisListType

NCORES = 8
D = 1024
S = 2048
NT = 17
NTOK = NT * 128
NEG = -30000.0
import os
STAGE = int(os.environ.get("KSTAGE", "99"))
SIM = os.environ.get("KSIM", "0") == "1"
SMALL = SIM or os.environ.get("KSMALL", "0") == "1"
NSEQ = int(os.environ.get("KNSEQ", "1")) if SIM else 16


class Buf:
    __slots__ = ("name", "lw", "rd", "excl", "last", "last_w")

    def __init__(self, name, excl=False):
        self.name = name
        self.lw = None
        self.rd = []
        self.excl = excl
        self.last = None
        self.last_w = False


class Op:
    __slots__ = ("eng", "fn", "deps", "sig", "need", "isdma", "semidx")

    def __init__(self, eng, fn, isdma):
        self.eng = eng
        self.fn = fn
        self.deps = {}
        self.sig = None
        self.need = False
        self.isdma = isdma
        self.semidx = -1


class Sched:
    ENG = ["pe", "act", "dve", "pool", "sp"]
    NDMA = 24
    NSW = int(os.environ.get("KNSW", "16"))
    ROLL = 30000

    def __init__(self, nc):
        self.nc = nc
        self.ops = {e: [] for e in self.ENG}
        self.dma_last = [None] * self.NDMA
        self.dma_rr = 0
        self.dmas = []
        self.sw_last = [None] * self.NSW
        self.sw_rr = 0
        self.sw_fresh = SIM

    def _deps(self, o, r, w):
        for b in r:
            if b.excl:
                if b.last is not None and b.last is not o:
                    o.deps.setdefault(b.last, "raw" if b.last_w else "war")
                b.last, b.last_w = o, False
                continue
            if b.lw is not None and b.lw is not o:
                o.deps.setdefault(b.lw, "raw")
            b.rd.append(o)
        for b in w:
            if b.excl:
                if b.last is not None and b.last is not o:
                    o.deps[b.last] = "raw"
                b.last, b.last_w = o, True
                continue
            if b.lw is not None and b.lw is not o:
                o.deps[b.lw] = "raw"
            for x in b.rd:
                if x is not o:
                    o.deps.setdefault(x, "war")
            b.rd = []
            b.lw = o

    def op(self, eng, fn, r=(), w=()):
        o = Op(eng, fn, False)
        self._deps(o, r, w)
        self.ops[eng].append(o)
        return o

    def barrier(self):
        lasts = []
        for e in self.ENG:
            comp = [o for o in self.ops[e] if not o.isdma]
            if comp:
                lasts.append(comp[-1])
        lasts += [d for d in self.dma_last if d is not None] + [d for d in self.sw_last if d is not None]
        if self.sw_fresh:
            lasts += [d for d in self.dmas if d.semidx < 0]
        for e in self.ENG:
            o = Op(e, lambda eng: eng.nop(), False)
            for d in lasts:
                o.deps[d] = "raw"
            self.ops[e].append(o)

    def dma(self, q, fn, r=(), w=(), sw=False):
        o = Op(q, fn, True)
        if sw and self.sw_fresh:
            o.semidx = -1
        elif sw:
            i = self.sw_rr
            self.sw_rr = (i + 1) % self.NSW
            if self.sw_last[i] is not None:
                o.deps[self.sw_last[i]] = "raw"
            self.sw_last[i] = o
            o.semidx = self.NDMA + i
        else:
            i = self.dma_rr
            self.dma_rr = (i + 1) % self.NDMA
            if self.dma_last[i] is not None:
                o.deps[self.dma_last[i]] = "raw"
            self.dma_last[i] = o
            o.semidx = i
        self._deps(o, r, w)
        self.ops[q].append(o)
        self.dmas.append(o)
        return o

    def emit(self):
        nc = self.nc
        for e in self.ENG:
            for o in self.ops[e]:
                keep = {}
                for d, kind in o.deps.items():
                    if d.isdma:
                        keep[d] = kind
                    elif d.eng == o.eng and not o.isdma:
                        if o.eng == "pe":
                            continue
                        keep[d] = kind
                    else:
                        keep[d] = kind
                o.deps = keep
                for d in keep:
                    d.need = True
        nsem = self.NDMA + self.NSW
        dma_sems = [nc.alloc_semaphore(name=f"dq{i}") for i in range(nsem)]
        dma_cnt = [0] * nsem
        for o in self.dmas:
            if o.semidx < 0:
                dma_sems.append(nc.alloc_semaphore(name=f"dsw{len(dma_sems)}"))
                dma_cnt.append(0)
                o.semidx = len(dma_sems) - 1
            dma_cnt[o.semidx] += 16
            o.sig = (dma_sems[o.semidx], dma_cnt[o.semidx])
        for e in self.ENG:
            sem = None
            cnt = 0
            k = 0
            for o in self.ops[e]:
                if o.isdma or not o.need:
                    continue
                if sem is None or cnt >= self.ROLL:
                    sem = nc.alloc_semaphore(name=f"e_{e}_{k}")
                    k += 1
                    cnt = 0
                cnt += 1
                o.sig = (sem, cnt)
        handles = {"pe": "tensor", "act": "scalar", "dve": "vector", "pool": "gpsimd", "sp": "sync"}
        final = [(dma_sems[i], dma_cnt[i]) for i in range(len(dma_sems)) if dma_cnt[i] > 0]
        sched = self

        with nc.Block() as block:
            def run(e, eng):
                known = {}
                for o in sched.ops[e]:
                    waits = {}
                    for d in o.deps:
                        s, v = d.sig
                        if waits.get(s, 0) < v:
                            waits[s] = v
                    for s, v in waits.items():
                        if known.get(s, 0) >= v:
                            continue
                        eng.wait_ge(s, v)
                        known[s] = v
                    ins = o.fn(eng)
                    if o.sig is not None:
                        ins.then_inc(o.sig[0], 16 if o.isdma else 1)
                if e == "sp":
                    for s, v in final:
                        eng.wait_ge(s, v)

            @block.tensor
            def _(eng):
                run("pe", eng)

            @block.scalar
            def _(eng):
                run("act", eng)

            @block.vector
            def _(eng):
                run("dve", eng)

            @block.gpsimd
            def _(eng):
                run("pool", eng)

            @block.sync
            def _(eng):
                run("sp", eng)


C_ID, C_NTRI, C_MSB, C_MMB = 0, 128, 256, 384
C_COS, C_SIN = 512, 512 + NT * 8
C_W = 512 + 2 * NT * 8


def make_consts():
    c = np.zeros((128, C_W), np.float32)
    p = np.arange(128)
    c[:, C_ID:C_ID + 128] = np.eye(128, dtype=np.float32)
    c[:, C_NTRI:C_NTRI + 128] = -(p[:, None] >= p[None, :]).astype(np.float32)
    c[:, C_MSB:C_MSB + 128] = NEG * (p[:, None] >= p[None, :])
    c[:, C_MMB:C_MMB + 128] = NEG * (p[:, None] > p[None, :])
    half = 8
    inv = np.float32(500000.0) ** (-np.arange(half, dtype=np.float32) / half)
    pos = np.zeros((NT, 128), np.float32)
    for i in range(16):
        pos[i] = i * 128 + p
    pos[16, :64] = 2048 + (p[:64] % 4)
    ang = pos[:, :, None].astype(np.float32) * inv[None, None, :]
    cs = np.cos(ang).astype(np.float32).transpose(1, 0, 2)
    sn = np.sin(ang).astype(np.float32).transpose(1, 0, 2)
    c[:, C_COS:C_COS + NT * 8] = cs.reshape(128, NT * 8)
    c[:, C_SIN:C_SIN + NT * 8] = sn.reshape(128, NT * 8)
    return c


def build():
    nc = bass.Bass("TRN2", target_bir_lowering=False)
    K = Sched(nc)

    def dram(name, shape, dt, kind):
        return nc.dram_tensor(name, list(shape), dt, kind=kind).ap()

    xin = dram("xin", [NTOK, D], F32, "ExternalInput")
    cst_d = dram("cst", [128, C_W], F32, "ExternalInput")
    g_mix = dram("g_mix", [1, D], F32, "ExternalInput")
    w_in = dram("w_in", [D, 5120], F32, "ExternalInput")
    NROWS = (256 if SMALL else 2560) * 128
    cache = {nm: dram("cache_" + nm, [NROWS, 512], F32, "ExternalInput") for nm in ("sb_k", "sb_v", "mb_k", "mb_v")}
    ptab = dram("ptab", [1, 256], I32, "ExternalInput")
    m01_d = dram("m01", [2, 128, 512], F32, "ExternalInput")
    w_ps = dram("w_proj_sb", [512, D], F32, "ExternalInput")
    w_pm = dram("w_proj_moba", [512, D], F32, "ExternalInput")
    w_o = dram("w_out", [D, D], F32, "ExternalInput")
    g_ffn = dram("g_ffn", [1, D], F32, "ExternalInput")
    g_fin = dram("g_final", [1, D], F32, "ExternalInput")
    w_r = dram("w_router", [D, 20], F32, "ExternalInput")
    b_r = dram("b_router", [1, 20], F32, "ExternalInput")
    w_eg = dram("w_expert_gate", [16, D, 256], F32, "ExternalInput")
    w_eu = dram("w_expert_up", [16, D, 256], F32, "ExternalInput")
    w_ed = dram("w_expert_down", [16, 256, D], F32, "ExternalInput")
    kv_out = dram("kv_out", [4, NTOK, 512], F32, "ExternalOutput")
    y_out = dram("y_out", [NTOK, D], F32, "ExternalOutput")

    def sb(name, shape, dt):
        return nc.alloc_sbuf_tensor("sb_" + name, list(shape), dt).ap()

    ps = [nc.alloc_psum_tensor(f"ps{i}", [128, 512], F32).ap() for i in range(8)]
    psb = [Buf(f"ps{i}", excl=True) for i in range(8)]

    cst = sb("cst", [128, C_W], F32)
    b_cst = Buf("cst")
    K.dma("sp", lambda e: e.dma_start(out=cst[:], in_=cst_d[:]), w=[b_cst])
    cbf = sb("cbf", [128, 512], BF16)
    b_cbf = Buf("cbf")
    K.op("dve", lambda e: e.tensor_copy(out=cbf[:], in_=cst[:, 0:512]), r=[b_cst], w=[b_cbf])
    ident = cbf[:, C_ID:C_ID + 128]
    ntri = cbf[:, C_NTRI:C_NTRI + 128]
    msb = cbf[:, C_MSB:C_MSB + 128]
    mmb = cbf[:, C_MMB:C_MMB + 128]
    cosT = cst[:, C_COS:C_COS + NT * 8].rearrange("p (t f) -> p t f", f=8)
    sinT = cst[:, C_SIN:C_SIN + NT * 8].rearrange("p (t f) -> p t f", f=8)
    gbc = sb("gbc", [128, D], F32)
    b_gbc = Buf("gbc")
    K.dma("sp", lambda e: e.dma_start(out=gbc[:], in_=g_mix.partition_broadcast(128)), w=[b_gbc])

    xnT = sb("xnT", [128, 8, NTOK], BF16)
    b_xnT = [Buf(f"xnT{i}") for i in range(NT)]
    QsT = sb("QsT", [128, 4, NTOK], BF16)
    KsT = sb("KsT", [128, 4, NTOK], BF16)
    Vs = sb("Vs", [128, NT, 512], BF16)
    b_QsT = [Buf(f"QsT{i}") for i in range(NT)]
    b_KsT = [Buf(f"KsT{i}") for i in range(NT)]
    b_Vs = [Buf(f"Vs{i}") for i in range(NT)]

    xt = [sb(f"xt{j}", [128, D], F32) for j in range(2)]
    b_xt = [Buf(f"xt{j}") for j in range(2)]
    junk = sb("junk", [128, D], BF16)
    b_junk = Buf("junk")
    ss = sb("ss", [128, NT], F32)
    rs = sb("rs", [128, NT], F32)
    rstd = sb("rstd", [128, NT], F32)
    b_ss = [Buf(f"ss{i}") for i in range(NT)]
    b_rs = [Buf(f"rs{i}") for i in range(NT)]
    b_rstd = [Buf(f"rstd{i}") for i in range(NT)]
    xn = [sb(f"xn{j}", [128, D], BF16) for j in range(2)]
    b_xn = [Buf(f"xn{j}") for j in range(2)]

    for i in range(int(os.environ.get("KNT", NT))):
        j = i % 2
        K.dma("sp", lambda e, i=i, j=j: e.dma_start(out=xt[j][:], in_=xin[i * 128:(i + 1) * 128, :]), w=[b_xt[j]])
        K.op("act", lambda e, i=i, j=j: e.activation(out=junk[:], in_=xt[j][:], func=AF.Square,
                                                       accum_out=ss[:, i:i + 1]),
             r=[b_xt[j]], w=[b_junk, b_ss[i]])
        K.op("act", lambda e, i=i: e.activation(out=rs[:, i:i + 1], in_=ss[:, i:i + 1], func=AF.Sqrt,
                                                 bias=1e-6, scale=1.0 / D), r=[b_ss[i]], w=[b_rs[i]])
        K.op("dve", lambda e, i=i: e.reciprocal(out=rstd[:, i:i + 1], in_=rs[:, i:i + 1]), r=[b_rs[i]], w=[b_rstd[i]])
        K.op("dve", lambda e, i=i, j=j: e.scalar_tensor_tensor(out=xn[j][:], in0=xt[j][:], scalar=rstd[:, i:i + 1],
                                                                in1=gbc[:], op0=ALU.mult, op1=ALU.mult),
             r=[b_xt[j], b_rstd[i], b_gbc], w=[b_xn[j]])
        pb = i % 2
        pst = ps[pb].bitcast(BF16)
        for kc in range(8):
            K.op("pe", lambda e, kc=kc, j=j, pst=pst: e.transpose(out=pst[:, kc * 128:(kc + 1) * 128],
                                                                     in_=xn[j][:, kc * 128:(kc + 1) * 128],
                                                                     identity=ident),
                 r=[b_xn[j], b_cbf], w=[psb[pb]])
        K.op("act", lambda e, i=i, pst=pst: e.activation(out=xnT[:, :, i * 128:(i + 1) * 128],
                                                          in_=pst.rearrange("p (k t) -> p k t", k=8), func=AF.Copy),
             r=[psb[pb]], w=[b_xnT[i]])

    wb = [sb(f"wb{j}", [128, 8, 512], BF16) for j in range(2)]
    b_wb = [Buf(f"wb{j}") for j in range(2)]
    w_in_v = w_in.rearrange("(k p) n -> p k n", p=128)
    wcount = [0]

    wst = [sb(f"wst{j}", [128, 4, 512], F32) for j in range(2)]
    b_wst = [Buf(f"wst{j}") for j in range(2)]

    def load_w(c):
        j = wcount[0] % 2
        wcount[0] += 1
        for h in range(2):
            K.dma("sp", lambda e, c=c, h=h: e.dma_start(out=wst[h][:], in_=w_in_v[:, 4 * h:4 * h + 4, c * 512:(c + 1) * 512]),
                  w=[b_wst[h]])
            K.op("pool", lambda e, j=j, h=h: e.tensor_copy(out=wb[j][:, 4 * h:4 * h + 4, :], in_=wst[h][:]),
                 r=[b_wst[h]], w=[b_wb[j]])
        return j

    hf = [sb(f"hf{j}", [128, 512], F32) for j in range(2)]
    b_hf = [Buf(f"hf{j}") for j in range(2)]
    hb = [sb(f"hb{j}", [128, 512], BF16) for j in range(2)]
    b_hb = [Buf(f"hb{j}") for j in range(2)]
    cnt = {"proj": 0, "hf": 0, "hb": 0}

    def project(c, i, j):
        pb = 2 + cnt["proj"] % 2
        cnt["proj"] += 1
        for kc in range(8):
            K.op("pe", lambda e, kc=kc, i=i, j=j, pb=pb: e.matmul(ps[pb][:], lhsT=xnT[:, kc, i * 128:(i + 1) * 128],
                                                                    rhs=wb[j][:, kc, :], start=(kc == 0), stop=(kc == 7)),
                 r=[b_xnT[i], b_wb[j]], w=[psb[pb]])
        return pb

    def to_T(src_bf, b_src, dstT, b_dst, i, pbank, dst_ap=None):
        pst = ps[pbank].bitcast(BF16)
        if dst_ap is None:
            dst_ap = dstT[:, :, i * 128:(i + 1) * 128]
        for cc in range(4):
            K.op("pe", lambda e, cc=cc: e.transpose(out=pst[:, cc * 128:(cc + 1) * 128],
                                                     in_=src_bf[:, cc * 128:(cc + 1) * 128], identity=ident),
                 r=[b_src, b_cbf], w=[psb[pbank]])
        K.op("dve", lambda e: e.tensor_copy(out=dst_ap,
                                            in_=pst[:, 0:512].rearrange("p (k t) -> p k t", k=4)),
             r=[psb[pbank]], w=[b_dst])

    def kv_store(which, i, src, b_src):
        K.dma("sp", lambda e: e.dma_start(out=kv_out[which, i * 128:(i + 1) * 128, :], in_=src[:]), r=[b_src])

    samp = {}
    for br in ("sb", "mb"):
        samp[br] = dict(QT=sb(f"sQT_{br}", [128, 4, 128], BF16), KT=sb(f"sKT_{br}", [128, 4, 128], BF16),
                        V=sb(f"sV_{br}", [128, 512], BF16), b=Buf(f"samp_{br}"))
    sQTf = sb("sQTf", [128, 4, 128], F32)
    b_sQTf = Buf("sQTf")

    for (c, kind) in (((1, "k"), (2, "v"), (0, "q"))[:int(os.environ.get("KA1", 3))] if STAGE >= 1 else ()):
        j = load_w(c)
        for i in range(int(os.environ.get("KA1T", NT))):
            pb = project(c, i, j)
            if kind in ("k", "v"):
                a = cnt["hf"] % 2
                cnt["hf"] += 1
                K.op("act", lambda e, a=a, pb=pb: e.activation(out=hf[a][:], in_=ps[pb][:], func=AF.Copy),
                     r=[psb[pb]], w=[b_hf[a]])
                kv_store(0 if kind == "k" else 1, i, hf[a], b_hf[a])
            if kind == "k":
                b = cnt["hb"] % 2
                cnt["hb"] += 1
                K.op("dve", lambda e, b=b, pb=pb: e.tensor_copy(out=hb[b][:], in_=ps[pb][:]), r=[psb[pb]], w=[b_hb[b]])
                if i == 16:
                    to_T(hb[b], b_hb[b], None, samp["sb"]["b"], i, 4 + i % 2, dst_ap=samp["sb"]["KT"][:])
                else:
                    to_T(hb[b], b_hb[b], KsT, b_KsT[i], i, 4 + i % 2)
            elif kind == "v":
                if i == 16:
                    K.op("dve", lambda e, pb=pb: e.tensor_copy(out=samp["sb"]["V"][:], in_=ps[pb][:]), r=[psb[pb]], w=[samp["sb"]["b"]])
                else:
                    K.op("dve", lambda e, i=i, pb=pb: e.tensor_copy(out=Vs[:, i, :], in_=ps[pb][:]), r=[psb[pb]], w=[b_Vs[i]])
            else:
                b = cnt["hb"] % 2
                cnt["hb"] += 1
                K.op("act", lambda e, b=b, pb=pb: e.activation(out=hb[b][:], in_=ps[pb][:], func=AF.Copy, scale=0.125),
                     r=[psb[pb]], w=[b_hb[b]])
                if i == 16:
                    to_T(hb[b], b_hb[b], None, samp["sb"]["b"], i, 4 + i % 2, dst_ap=samp["sb"]["QT"][:])
                else:
                    to_T(hb[b], b_hb[b], QsT, b_QsT[i], i, 4 + i % 2)

    o_sT = sb("o_sT", [128, 4, NTOK], BF16)
    b_osT = Buf("o_sT")
    negones = sb("negones", [128, 128], BF16)
    ones_bf = sb("ones_bf", [128, 128], BF16)
    b_ones = Buf("ones")
    K.op("pool", lambda e: e.memset(negones[:], -1.0), w=[b_ones])
    K.op("pool", lambda e: e.memset(ones_bf[:], 1.0), w=[b_ones])
    e_sb = [xt[j][:, 0:512] for j in range(2)]
    sp_sb = [sb(f"sp_sb{j}", [128, 512], BF16) for j in range(2)]
    A_sb = [junk[:, 512 * j:512 * (j + 1)] for j in range(2)]
    srun = [sb(f"srun{j}", [128, 512], BF16) for j in range(3)]
    b_e = b_xt
    b_sp = [Buf(f"sp{j}") for j in range(2)]
    b_A = [Buf(f"A{j}") for j in range(2)]
    b_srun = [Buf(f"srun{j}") for j in range(3)]
    dbg_o = dram("dbg_o", [2, 128, 4, NTOK], BF16, "ExternalOutput")

    steps = []
    for h in range(8):
        for g in range(4):
            for j in range(4 * g + 3, -1, -1):
                steps.append((h, g, j))

    def zmm(P, pbank, h, g, j, q0, last):
        c, p0 = h // 2, (h % 2) * 64
        K.op("pe", lambda e: e.matmul(P[:, q0:512], lhsT=KsT[p0:p0 + 64, c, j * 128:(j + 1) * 128],
                                      rhs=QsT[p0:p0 + 64, c, g * 512 + q0:(g + 1) * 512], start=True,
                                      stop=(last and j < 4 * g)),
             r=[b_KsT[j]] + [b_QsT[4 * g + t] for t in range(4)], w=[psb[pbank]])
        if j >= 4 * g:
            K.op("pe", lambda e: e.matmul(P[:, q0:q0 + 128], lhsT=ident, rhs=msb, start=False, stop=last),
                 r=[b_cbf], w=[psb[pbank]])

    def sb_X(t):
        h, g, j = steps[t]
        a = t % 2
        q0 = max(j - 4 * g, 0) * 128
        first = (j == 4 * g + 3)
        zmm(ps[a], a, h, g, j, q0, True)
        K.op("act", lambda e: e.activation(out=e_sb[a][:, q0:], in_=ps[a][:, q0:], func=AF.Exp), r=[psb[a]], w=[b_e[a]])
        K.op("act", lambda e: e.activation(out=sp_sb[a][:, q0:], in_=e_sb[a][:, q0:], func=AF.Ln, bias=1.0),
             r=[b_e[a]], w=[b_sp[a]])
        cu, nx = t % 3, (t + 1) % 3
        if first:
            K.op("pool", lambda e: e.memset(srun[nx][:, 0:q0], 0.0), w=[b_srun[nx]])
            K.op("pool", lambda e: e.tensor_copy(out=srun[nx][:, q0:], in_=sp_sb[a][:, q0:]), r=[b_sp[a]], w=[b_srun[nx]])
        elif j > 0:
            if q0 > 0:
                K.op("pool", lambda e: e.tensor_copy(out=srun[nx][:, 0:q0], in_=srun[cu][:, 0:q0]), r=[b_srun[cu]], w=[b_srun[nx]])
            K.op("pool", lambda e: e.tensor_tensor(out=srun[nx][:, q0:], in0=srun[cu][:, q0:], in1=sp_sb[a][:, q0:], op=ALU.add),
                 r=[b_srun[cu], b_sp[a]], w=[b_srun[nx]])

    def sb_Y(t):
        h, g, j = steps[t]
        a = t % 2
        c, p0 = h // 2, (h % 2) * 64
        q0 = max(j - 4 * g, 0) * 128
        first = (j == 4 * g + 3)
        P2 = ps[2 + a]
        zmm(P2, 2 + a, h, g, j, q0, False)
        K.op("pe", lambda e: e.matmul(P2[:, q0:], lhsT=ntri, rhs=sp_sb[a][:, q0:], start=False, stop=first),
             r=[b_sp[a], b_cbf], w=[psb[2 + a]])
        if not first:
            K.op("pe", lambda e: e.matmul(P2[:, q0:], lhsT=negones[:], rhs=srun[t % 3][:, q0:], start=False, stop=True),
                 r=[b_srun[t % 3], b_ones], w=[psb[2 + a]])
        K.op("act", lambda e: e.activation(out=A_sb[a][:, q0:], in_=P2[:, q0:], func=AF.Exp), r=[psb[2 + a]], w=[b_A[a]])
        ob = 4 + (h * 4 + g) % 2
        K.op("pe", lambda e: e.matmul(ps[ob][:, q0:], lhsT=Vs[:, j, c * 128:(c + 1) * 128], rhs=A_sb[a][:, q0:],
                                      start=first, stop=(j == 0), skip_group_check=True),
             r=[b_A[a], b_Vs[j]], w=[psb[ob]])
        if j == 0:
            K.op("dve", lambda e: e.tensor_copy(out=o_sT[p0:p0 + 64, c, g * 512:(g + 1) * 512], in_=ps[ob][p0:p0 + 64, :]),
                 r=[psb[ob]], w=[b_osT])

    if STAGE >= 2:
        sb_X(0)
        for t in range(len(steps)):
            if t + 1 < len(steps):
                sb_X(t + 1)
            sb_Y(t)
        K.dma("sp", lambda e: e.dma_start(out=dbg_o[0, :, :, 0:S], in_=o_sT[:, :, 0:S]), r=[b_osT])

    def view(t, dt, off, shape):
        nd = len(t.shape)
        f = t[:]
        if nd > 2:
            names = " ".join(f"a{i}" for i in range(nd - 1))
            f = f.rearrange(f"p {names} -> p ({names})")
        isz = {F32: 4, I32: 4, BF16: 2}[dt]
        osz = {F32: 4, I32: 4, BF16: 2}[t.dtype]
        if isz != osz:
            f = f.bitcast(dt)
        n = 1
        for d_ in shape[1:]:
            n *= d_
        v = f[0:shape[0], off // isz:off // isz + n]
        if len(shape) > 2:
            names = " ".join(f"b{i}" for i in range(len(shape) - 1))
            kw = {f"b{i}": shape[i + 1] for i in range(len(shape) - 1)}
            v = v.rearrange(f"p ({names}) -> p {names}", **kw)
        return v

    offs = sb("offs", [128, 256], I32)
    ptb = xn[0][:, 0:512].bitcast(I32)
    iot = sb("iot", [128, 1], I32)
    b_offs = Buf("offs")
    K.dma("sp", lambda e: e.dma_start(out=ptb[:], in_=ptab.partition_broadcast(128)), r=[b_xn[0]], w=[b_offs, b_xn[0]])
    K.op("pool", lambda e: e.iota(iot[:], pattern=[[0, 1]], base=0, channel_multiplier=1), w=[b_offs])
    K.op("pool", lambda e: e.tensor_scalar(out=offs[:], in0=ptb[:], scalar1=128.0, scalar2=None, op0=ALU.mult), r=[b_offs], w=[b_offs])
    K.op("pool", lambda e: e.tensor_tensor(out=offs[:], in0=offs[:], in1=iot[:].broadcast_to([128, 256]), op=ALU.add), r=[b_offs], w=[b_offs])

    offs2 = sb("offs2", [128, 128], I32)
    ptb2 = ptb.rearrange("p (j two) -> p j two", two=2)
    K.op("pool", lambda e: e.tensor_copy(out=offs2[0:64, :], in_=ptb2[0:64, :, 0]), r=[b_offs], w=[b_offs])
    K.op("pool", lambda e: e.tensor_copy(out=offs2[64:128, :], in_=ptb2[64:128, :, 1]), r=[b_offs], w=[b_offs])
    K.op("pool", lambda e: e.tensor_scalar(out=offs2[:], in0=offs2[:], scalar1=64.0, scalar2=None, op0=ALU.mult), r=[b_offs], w=[b_offs])
    K.op("pool", lambda e: e.tensor_tensor(out=offs2[:], in0=offs2[:], in1=iot[:].broadcast_to([128, 128]), op=ALU.add), r=[b_offs], w=[b_offs])
    K.op("pool", lambda e: e.tensor_scalar(out=offs2[64:128, :], in0=offs2[64:128, :], scalar1=-64.0, scalar2=None, op0=ALU.add), r=[b_offs], w=[b_offs])

    def sample_attn(br):
        K.barrier()
        is_sb = (br == "sb")
        sm = samp[br]
        poolK, poolV = cache[br + "_k"], cache[br + "_v"]
        o_T, b_oT = (o_sT, b_osT) if is_sb else (o_mT, b_omT)
        Kg = view(QsT, BF16, 0, [128, 16, 512])
        Vg = view(KsT, BF16, 0, [128, 16, 512])
        alt = [view(xt[0], BF16, 0, [128, 4, 512]), view(xt[1], BF16, 0, [128, 4, 512]), view(hf[0], BF16, 0, [128, 2, 512]),
               view(hf[1], BF16, 0, [128, 2, 512])]
        altp = ([alt[0][:, q, :] for q in range(4)] + [alt[1][:, q, :] for q in range(4)] + [alt[k][:, q, :] for k in range(2, 4) for q in range(2)]
                + [sp_sb[0][:], sp_sb[1][:], srun[0][:], srun[1][:]])
        Vpg = [[Vg[:, pg, :] for pg in range(16)], altp]
        KTs = view(Vs, BF16, 0, [128, 4, 2048])
        Qbdf = view(wst[0], F32, 0, [128, 4, 16, 32])
        Qbd = view(wst[1], BF16, 0, [128, 4, 16, 32])
        IndN = view(wst[1], BF16, 4096, [128, 16, 128])
        e_s = view(wb[0], F32, 0, [128, 512])
        sp_s = view(wb[0], BF16, 2048, [128, 512])
        A_s = view(wb[0], BF16, 3072, [128, 512])
        en = view(wb[0], F32, 4096, [128, 512])
        spn = view(wb[0], BF16, 6144, [128, 512])
        An = view(wb[0], BF16, 7168, [128, 512])
        m01 = view(wb[1], F32, 0, [128, 512])
        kmTs = view(wb[1], F32, 2048, [128, 4, 8])
        sblk_s = view(wb[1], F32, 2304, [128, 8])
        mx_s = view(wb[1], F32, 2432, [128, 8])
        b01_s = view(wb[1], BF16, 2560, [128, 8])
        biasTs = view(wb[1], BF16, 2624, [128, 32])
        rl_s = view(wb[1], F32, 4096, [128, 512])
        tmpn = view(wb[1], F32, 6144, [128, 512])
        bq, bm, bn, bkg, bkt = Buf("s_q"), Buf("s_m"), Buf("s_n"), Buf("s_kg"), Buf("s_kt")
        bvg2 = [Buf("s_vg0"), Buf("s_vg1")]
        be, bsp, bA, bkm, bsb_, bbt, brl = Buf("s_e"), Buf("s_sp"), Buf("s_A"), Buf("s_km"), Buf("s_sblk"), Buf("s_bt"), Buf("s_rl")
        K.dma("sp", lambda e: e.dma_start(out=m01, in_=m01_d[0 if is_sb else 1]), w=[bm])
        K.op("pool", lambda e: e.memset(Qbd, 0.0), w=[bq])
        if not is_sb:
            K.op("pool", lambda e: e.memset(Qbdf, 0.0), w=[bq])
            K.op("pool", lambda e: e.memset(IndN[0:8], 0.0), w=[bq])
            K.op("dve", lambda e: e.tensor_copy(out=IndN[0:8].rearrange("p (n u) m -> p n (u m)", u=2),
                                                in_=id30k[0:8, 0:8].unsqueeze(2).broadcast_to([8, 8, 256])), r=[b_selm], w=[bq])
        for h in range(8):
            c, p0 = h // 2, (h % 2) * 64
            K.op("dve", lambda e, c=c, p0=p0, h=h: e.tensor_copy(out=Qbd[p0:p0 + 64, c, :, h * 4:(h + 1) * 4],
                                                               in_=sm["QT"][p0:p0 + 64, c, 0:64].rearrange("p (s t) -> p s t", t=4)),
                 r=[sm["b"]], w=[bq])
            if not is_sb:
                K.op("dve", lambda e, c=c, p0=p0, h=h: e.tensor_copy(out=Qbdf[p0:p0 + 64, c, :, h * 4:(h + 1) * 4],
                                                                   in_=sQTf[p0:p0 + 64, c, 0:64].rearrange("p (s t) -> p s t", t=4)),
                     r=[b_sQTf], w=[bq])
        def znew(bank):
            for s_ in range(16):
                for c in range(4):
                    K.op("pe", lambda e, s_=s_, c=c: e.matmul(ps[bank][:, s_ * 32:(s_ + 1) * 32], lhsT=sm["KT"][:, c, :], rhs=Qbd[:, c, s_, :],
                                                            start=(s_ == 0 and c == 0), stop=False, skip_group_check=True),
                         r=[sm["b"], bq], w=[psb[bank]])
        znew(4)
        K.op("act", lambda e: e.activation(out=en, in_=ps[4][:], func=AF.Exp), r=[psb[4]], w=[bn])
        K.op("dve", lambda e: e.tensor_tensor(out=en, in0=en, in1=m01, op=ALU.mult), r=[bn, bm], w=[bn])
        if is_sb:
            K.op("act", lambda e: e.activation(out=spn, in_=en, func=AF.Ln, bias=1.0), r=[bn], w=[bn])
            znew(5)
            K.op("pe", lambda e: e.matmul(ps[5][:], lhsT=ntri, rhs=spn, start=False, stop=True, skip_group_check=True), r=[bn, b_cbf], w=[psb[5]])
            K.op("act", lambda e: e.activation(out=tmpn, in_=ps[5][:], func=AF.Exp), r=[psb[5]], w=[bn])
            K.op("dve", lambda e: e.tensor_tensor(out=An, in0=tmpn, in1=m01, op=ALU.mult), r=[bn, bm], w=[bn])
        else:
            K.op("dve", lambda e: e.tensor_copy(out=An, in_=en), r=[bn], w=[bn])
        first_o = [True]
        for s_ in range(NSEQ):
            par = s_ % 2
            bvg = bvg2[par]
            order = []
            for pg in range(16):
                order += [(Kg[:, pg, :], poolK, bkg, pg), (Vpg[par][pg], poolV, bvg, pg)]
            if not is_sb and os.environ.get("KTWO", "1") == "1":
                pK2 = poolK.rearrange("(r two) w -> r (two w)", two=2)
                pV2 = poolV.rearrange("(r two) w -> r (two w)", two=2)
                for dm in range(8):
                    col = s_ * 8 + dm
                    K.dma("pool", lambda e, dm=dm, col=col: e.indirect_dma_start(
                        out=Kg[:, 2 * dm:2 * dm + 2, :].rearrange("p a b -> p (a b)"), out_offset=None, in_=pK2[:, :],
                        in_offset=bass.IndirectOffsetOnAxis(ap=offs2[:, col:col + 1], axis=0)), r=[b_offs], w=[bkg], sw=True)
                    K.dma("pool", lambda e, dm=dm, col=col: e.indirect_dma_start(
                        out=Vg[:, 2 * dm:2 * dm + 2, :].rearrange("p a b -> p (a b)"), out_offset=None, in_=pV2[:, :],
                        in_offset=bass.IndirectOffsetOnAxis(ap=offs2[:, col:col + 1], axis=0)), r=[b_offs], w=[bvg2[0]], sw=True)
                order = []
                par = 0
                bvg = bvg2[0]
            for (dstg, poolg, bg_, pg) in order:
                col = s_ * 16 + pg
                K.dma("pool", lambda e, pg=pg, col=col, dstg=dstg, poolg=poolg: e.indirect_dma_start(
                    out=dstg, out_offset=None, in_=poolg[:, :],
                    in_offset=bass.IndirectOffsetOnAxis(ap=offs[:, col:col + 1], axis=0)), r=[b_offs], w=[bg_], sw=True)
            for pg in range(16):
                bk = 6 + pg % 2
                pst = ps[bk].bitcast(BF16)
                for c in range(4):
                    K.op("pe", lambda e, pg=pg, c=c, pst=pst: e.transpose(out=pst[:, c * 128:(c + 1) * 128], in_=Kg[:, pg, c * 128:(c + 1) * 128], identity=ident),
                         r=[bkg, b_cbf], w=[psb[bk]])
                if pg % 2 == 0:
                    K.op("act", lambda e, pg=pg, pst=pst: e.activation(out=KTs[:, :, pg * 128:(pg + 1) * 128], in_=pst[:, 0:512].rearrange("p (c t) -> p c t", c=4), func=AF.Copy),
                         r=[psb[bk]], w=[bkt])
                else:
                    K.op("dve", lambda e, pg=pg, pst=pst: e.tensor_copy(out=KTs[:, :, pg * 128:(pg + 1) * 128], in_=pst[:, 0:512].rearrange("p (c t) -> p c t", c=4)),
                         r=[psb[bk]], w=[bkt])
            if not is_sb:
                for c in range(4):
                    for n in range(8):
                        for u in range(2):
                            K.op("pe", lambda e, c=c, n=n, u=u: e.matmul(ps[5][:, c * 8 + n:c * 8 + n + 1], lhsT=Kg[:, 2 * n + u, c * 128:(c + 1) * 128],
                                                                       rhs=ones256b[:, 0:1], start=(u == 0), stop=(u == 1)),
                                 r=[bkg, b_ones], w=[psb[5]])
                K.op("dve", lambda e: e.tensor_copy(out=kmTs, in_=ps[5][:, 0:32].rearrange("p (c n) -> p c n", c=4)), r=[psb[5]], w=[bkm])
                for c in range(4):
                    K.op("pe", lambda e, c=c, s_=s_: e.matmul(ps[5][0:32, 32:40], lhsT=Qbdf[:, c, s_, :], rhs=kmTs[:, c, :], start=(c == 0), stop=(c == 3)),
                         r=[bq, bkm], w=[psb[5]])
                K.op("dve", lambda e: e.tensor_copy(out=sblk_s[0:32], in_=ps[5][0:32, 32:40]), r=[psb[5]], w=[bsb_])
                K.op("dve", lambda e: e.max(out=mx_s[0:32], in_=sblk_s[0:32]), r=[bsb_], w=[bsb_])
                K.op("dve", lambda e: e.tensor_scalar(out=b01_s[0:32], in0=sblk_s[0:32], scalar1=mx_s[0:32, 2:3], scalar2=1.0,
                                                      op0=ALU.is_ge, op1=ALU.subtract), r=[bsb_], w=[bsb_])
                pst5 = ps[5].bitcast(BF16)
                K.op("pe", lambda e: e.transpose(out=pst5[0:8, 128:160], in_=b01_s[0:32], identity=ident[0:32, 0:32]), r=[bsb_, b_cbf], w=[psb[5]])
                K.op("dve", lambda e: e.tensor_copy(out=biasTs[0:8], in_=pst5[0:8, 128:160]), r=[psb[5]], w=[bbt])

            def zs(bank):
                for pg in range(16):
                    for c in range(4):
                        K.op("pe", lambda e, pg=pg, c=c, s_=s_: e.matmul(ps[bank][:, pg * 32:(pg + 1) * 32], lhsT=KTs[:, c, pg * 128:(pg + 1) * 128], rhs=Qbd[:, c, s_, :],
                                                                start=(pg == 0 and c == 0), stop=False, skip_group_check=True),
                             r=[bkt, bq], w=[psb[bank]])
            zs(0)
            if is_sb:
                K.op("act", lambda e: e.activation(out=e_s, in_=ps[0][:], func=AF.Exp), r=[psb[0]], w=[be])
                K.op("act", lambda e: e.activation(out=sp_s, in_=e_s, func=AF.Ln, bias=1.0), r=[be], w=[bsp])
                zs(1)
                K.op("pe", lambda e: e.matmul(ps[1][:], lhsT=ntri, rhs=sp_s, start=False, stop=False, skip_group_check=True), r=[bsp, b_cbf], w=[psb[1]])
                for pg in range(1, 16):
                    K.op("pe", lambda e, pg=pg: e.matmul(ps[1][:, 0:pg * 32].rearrange("p (a b) -> p a b", b=32), lhsT=negones[:],
                                                       rhs=sp_s[:, pg * 32:(pg + 1) * 32].unsqueeze(1).broadcast_to([128, pg, 32]),
                                                       start=False, stop=False, skip_group_check=True), r=[bsp, b_ones], w=[psb[1]])
                K.op("pe", lambda e, s_=s_: e.matmul(ps[1][:].rearrange("p (a b) -> p a b", b=32), lhsT=negones[:],
                                              rhs=spn[:, s_ * 32:(s_ + 1) * 32].unsqueeze(1).broadcast_to([128, 16, 32]),
                                              start=False, stop=True, skip_group_check=True), r=[bn, b_ones], w=[psb[1]])
                K.op("act", lambda e: e.activation(out=A_s, in_=ps[1][:], func=AF.Exp), r=[psb[1]], w=[bA])
            else:
                for pg in range(16):
                    K.op("pe", lambda e, pg=pg: e.matmul(ps[0][:, pg * 32:(pg + 1) * 32], lhsT=IndN[0:8, pg, :], rhs=biasTs[0:8, :],
                                                       start=False, stop=(pg == 15), skip_group_check=True), r=[bq, bbt], w=[psb[0]])
                K.op("act", lambda e: e.activation(out=A_s, in_=ps[0][:], func=AF.Exp), r=[psb[0]], w=[bA])
            for pg in range(17):
                for h in range(8):
                    c = h // 2
                    if pg < 16:
                        lh, rh, rr = Vpg[par][pg][:, c * 128:(c + 1) * 128], A_s[:, pg * 32 + h * 4:pg * 32 + h * 4 + 4], [bvg, bA]
                    else:
                        lh, rh, rr = sm["V"][:, c * 128:(c + 1) * 128], An[:, s_ * 32 + h * 4:s_ * 32 + h * 4 + 4], [sm["b"], bn]
                    K.op("pe", lambda e, lh=lh, rh=rh, h=h, st=first_o[0], s_=s_: e.matmul(ps[2][:, s_ * 32 + h * 4:s_ * 32 + h * 4 + 4], lhsT=lh, rhs=rh,
                                                                                  start=st, stop=False, skip_group_check=True), r=rr, w=[psb[2]])
                    first_o[0] = False
                if not is_sb:
                    rh = A_s[:, pg * 32:(pg + 1) * 32] if pg < 16 else An[:, s_ * 32:(s_ + 1) * 32]
                    K.op("pe", lambda e, rh=rh, st=(s_ == 0 and pg == 0), s_=s_: e.matmul(ps[3][:, s_ * 32:(s_ + 1) * 32], lhsT=ones_bf[:], rhs=rh,
                                                                                 start=st, stop=False, skip_group_check=True),
                         r=[bA, bn, b_ones], w=[psb[3]])
        ncol = NSEQ * 32
        if not is_sb:
            K.op("dve", lambda e: e.reciprocal(out=rl_s[:, 0:ncol], in_=ps[3][:, 0:ncol]), r=[psb[3]], w=[brl])
        for h in range(8):
            c, p0 = h // 2, (h % 2) * 64
            dst = o_T[p0:p0 + 64, c, S:S + 4 * NSEQ].rearrange("p (s t) -> p s t", t=4)
            src = ps[2][p0:p0 + 64, 0:ncol].rearrange("p (s x) -> p s x", x=32)[:, :, h * 4:(h + 1) * 4]
            if is_sb:
                K.op("dve", lambda e, dst=dst, src=src: e.tensor_copy(out=dst, in_=src), r=[psb[2]], w=[b_oT])
            else:
                rsrc = rl_s[p0:p0 + 64, 0:ncol].rearrange("p (s x) -> p s x", x=32)[:, :, h * 4:(h + 1) * 4]
                K.op("dve", lambda e, dst=dst, src=src, rsrc=rsrc: e.tensor_tensor(out=dst, in0=src, in1=rsrc, op=ALU.mult), r=[psb[2], brl], w=[b_oT])
        K.op("pool", lambda e: e.memset(o_T[:, :, S + 4 * NSEQ:NTOK], 0.0), w=[b_oT])
        K.barrier()

    ones256b = sb("ones256b", [128, 2], BF16)
    K.op("pool", lambda e: e.memset(ones256b[:], 1.0 / 256.0), w=[b_ones])
    if STAGE >= 4 and os.environ.get("KBR", "sb") in ("sb", "both") or (STAGE >= 4 and not SIM):
        sample_attn("sb")

    QmT, KmT, Vm = QsT, KsT, Vs
    b_QmT, b_KmT, b_Vm = b_QsT, b_KsT, b_Vs
    kmf = [sb(f"kmf{j}", [128, 512], F32) for j in range(2)]
    b_kmf = [Buf(f"kmf{j}") for j in range(2)]
    rot = sb("rot", [128, 4, 64], F32)
    b_rot = Buf("rot")
    ones256 = sb("ones256", [128, 2], F32)
    identf = cst[:, C_ID:C_ID + 128]
    kmT = sb("kmT", [128, 4, 8], F32)
    b_kmT = Buf("kmT")
    biasT = sb("biasT", [128, S], BF16)
    b_biasT = [Buf(f"biasT{i}") for i in range(16)]
    b_selm = Buf("selm")
    qTf = sb("qTf", [128, 4, 128], F32)
    b_qTf = Buf("qTf")
    sblk = sb("sblk", [128, 64], F32)
    mx8 = sb("mx8", [128, 64], F32)
    bias01 = sb("bias01", [128, 64], BF16)
    b_sblk, b_mx8, b_bias01 = Buf("sblk"), Buf("mx8"), Buf("bias01")
    id30k = sb("id30k", [64, 64], BF16)

    def rotary(t, b_t, i):
        tv = t[:].rearrange("p (h d) -> p h d", h=8)
        x1, x2 = tv[:, :, 0:8], tv[:, :, 8:16]
        cs = cosT[:, i, :].unsqueeze(1).broadcast_to([128, 8, 8])
        sn = sinT[:, i, :].unsqueeze(1).broadcast_to([128, 8, 8])
        K.op("dve", lambda e: e.tensor_tensor(out=rot[:, 0, :].rearrange("p (h f) -> p h f", h=8), in0=x1, in1=cs, op=ALU.mult), r=[b_t, b_cst], w=[b_rot])
        K.op("dve", lambda e: e.tensor_tensor(out=rot[:, 1, :].rearrange("p (h f) -> p h f", h=8), in0=x2, in1=sn, op=ALU.mult), r=[b_t, b_cst], w=[b_rot])
        K.op("dve", lambda e: e.tensor_tensor(out=rot[:, 2, :].rearrange("p (h f) -> p h f", h=8), in0=x1, in1=sn, op=ALU.mult), r=[b_t, b_cst], w=[b_rot])
        K.op("dve", lambda e: e.tensor_tensor(out=rot[:, 3, :].rearrange("p (h f) -> p h f", h=8), in0=x2, in1=cs, op=ALU.mult), r=[b_t, b_cst], w=[b_rot])
        K.op("dve", lambda e: e.tensor_tensor(out=x1, in0=rot[:, 0, :].rearrange("p (h f) -> p h f", h=8),
                                              in1=rot[:, 1, :].rearrange("p (h f) -> p h f", h=8), op=ALU.subtract), r=[b_rot], w=[b_t])
        K.op("dve", lambda e: e.tensor_tensor(out=x2, in0=rot[:, 2, :].rearrange("p (h f) -> p h f", h=8),
                                              in1=rot[:, 3, :].rearrange("p (h f) -> p h f", h=8), op=ALU.add), r=[b_rot], w=[b_t])

    if STAGE >= 3:
        K.op("pool", lambda e: e.memset(ones256[:], 1.0 / 256.0), w=[b_ones])
        K.op("dve", lambda e: e.tensor_scalar(out=id30k[:], in0=cst[0:64, C_ID:C_ID + 64], scalar1=30000.0, scalar2=None, op0=ALU.mult),
             r=[b_cst], w=[b_selm])
        K.op("pool", lambda e: e.memset(biasT[0:64, 0:256], 0.0), w=[b_biasT[0], b_biasT[1]])
        K.op("pool", lambda e: e.memset(biasT[64:128, :], 0.0), w=[b_selm])
        for (c, kind) in ((4, "k"), (5, "v"), (3, "q")):
            j = load_w(c)
            for i in range(NT):
                pb = project(c, i, j)
                if kind == "v":
                    a = cnt["hf"] % 2
                    cnt["hf"] += 1
                    K.op("act", lambda e, a=a, pb=pb: e.activation(out=hf[a][:], in_=ps[pb][:], func=AF.Copy), r=[psb[pb]], w=[b_hf[a]])
                    kv_store(3, i, hf[a], b_hf[a])
                    if i == 16:
                        K.op("dve", lambda e, pb=pb: e.tensor_copy(out=samp["mb"]["V"][:], in_=ps[pb][:]), r=[psb[pb]], w=[samp["mb"]["b"]])
                    else:
                        K.op("dve", lambda e, i=i, pb=pb: e.tensor_copy(out=Vm[:, i, :], in_=ps[pb][:]), r=[psb[pb]], w=[b_Vm[i]])
                    continue
                a = i % 2
                K.op("act", lambda e, a=a, pb=pb, kind=kind: e.activation(out=kmf[a][:], in_=ps[pb][:], func=AF.Copy,
                                                                         scale=(1.0 if kind == "k" else 0.125)),
                     r=[psb[pb]], w=[b_kmf[a]])
                rotary(kmf[a], b_kmf[a], i)
                b = cnt["hb"] % 2
                cnt["hb"] += 1
                K.op("pool", lambda e, a=a, b=b: e.tensor_copy(out=hb[b][:], in_=kmf[a][:]), r=[b_kmf[a]], w=[b_hb[b]])
                if kind == "k":
                    kv_store(2, i, kmf[a], b_kmf[a])
                    if i == 16:
                        to_T(hb[b], b_hb[b], None, samp["mb"]["b"], i, 4 + i % 2, dst_ap=samp["mb"]["KT"][:])
                    else:
                        to_T(hb[b], b_hb[b], KmT, b_KmT[i], i, 4 + i % 2)
                    if i < 16 and i % 2 == 1:
                        n = i // 2
                        for ch in range(4):
                            for u in range(2):
                                src = kmf[1 - a] if u == 0 else kmf[a]
                                K.op("pe", lambda e, ch=ch, u=u, src=src, n=n: e.matmul(ps[6][:, ch * 8 + n:ch * 8 + n + 1],
                                                                                    lhsT=src[:, ch * 128:(ch + 1) * 128],
                                                                                    rhs=ones256[:, 0:1], start=(u == 0), stop=(u == 1)),
                                     r=[b_kmf[0], b_kmf[1], b_ones], w=[psb[6]])
                        if i == 15:
                            K.op("dve", lambda e: e.tensor_copy(out=kmT[:], in_=ps[6][:, 0:32].rearrange("p (c n) -> p c n", c=4)),
                                 r=[psb[6]], w=[b_kmT])
                else:
                    if i == 16:
                        to_T(hb[b], b_hb[b], None, samp["mb"]["b"], i, 4 + i % 2, dst_ap=samp["mb"]["QT"][:])
                        for cc in range(4):
                            K.op("pe", lambda e, cc=cc, a=a: e.transpose(out=ps[7][:, cc * 128:(cc + 1) * 128], in_=kmf[a][:, cc * 128:(cc + 1) * 128],
                                                                      identity=identf), r=[b_kmf[a], b_cst], w=[psb[7]])
                        K.op("act", lambda e: e.activation(out=sQTf[:], in_=ps[7][:].rearrange("p (c t) -> p c t", c=4), func=AF.Copy),
                             r=[psb[7]], w=[b_sQTf])
                    else:
                        to_T(hb[b], b_hb[b], QmT, b_QmT[i], i, 4 + i % 2)
                    own = i // 2
                    if i < 16 and own >= 1:
                        for cc in range(4):
                            K.op("pe", lambda e, cc=cc, a=a: e.transpose(out=ps[7][:, cc * 128:(cc + 1) * 128], in_=kmf[a][:, cc * 128:(cc + 1) * 128],
                                                                      identity=identf), r=[b_kmf[a], b_cst], w=[psb[7]])
                        K.op("act", lambda e: e.activation(out=qTf[:], in_=ps[7][:].rearrange("p (c t) -> p c t", c=4), func=AF.Copy),
                             r=[psb[7]], w=[b_qTf])
                        for h in range(8):
                            c2, p0 = h // 2, (h % 2) * 64
                            K.op("pe", lambda e, h=h, c2=c2, p0=p0: e.matmul(ps[6][:, 64 + h * 8:64 + h * 8 + 8], lhsT=qTf[p0:p0 + 64, c2, :],
                                                                         rhs=kmT[p0:p0 + 64, c2, :], start=True, stop=True),
                                 r=[b_qTf, b_kmT], w=[psb[6]])
                        K.op("dve", lambda e: e.tensor_copy(out=sblk[:], in_=ps[6][:, 64:128]), r=[psb[6]], w=[b_sblk])
                        if own < 8:
                            K.op("dve", lambda e, own=own: e.memset(sblk[:].rearrange("p (h n) -> p h n", h=8)[:, :, own:8], -1e30), w=[b_sblk])
                        for h in range(8):
                            K.op("dve", lambda e, h=h: e.max(out=mx8[:, h * 8:(h + 1) * 8], in_=sblk[:, h * 8:(h + 1) * 8]), r=[b_sblk], w=[b_mx8])
                        ti = min(own, 3) - 1
                        for h in range(8):
                            K.op("dve", lambda e, h=h, ti=ti: e.tensor_scalar(out=bias01[:, h * 8:(h + 1) * 8], in0=sblk[:, h * 8:(h + 1) * 8],
                                                                          scalar1=mx8[:, h * 8 + ti:h * 8 + ti + 1], scalar2=1.0,
                                                                          op0=ALU.is_ge, op1=ALU.subtract),
                                 r=[b_sblk, b_mx8], w=[b_bias01])
                        if own < 8:
                            K.op("dve", lambda e, own=own: e.memset(bias01[:].rearrange("p (h n) -> p h n", h=8)[:, :, own:own + 1], 0.0), w=[b_bias01])
                        pstb = ps[7].bitcast(BF16)
                        K.op("pe", lambda e: e.transpose(out=pstb[0:64, 0:128], in_=bias01[:], identity=ident), r=[b_bias01, b_cbf], w=[psb[7]])
                        K.op("dve", lambda e, i=i: e.tensor_copy(out=biasT[0:64, i * 128:(i + 1) * 128], in_=pstb[0:64, 0:128]), r=[psb[7]], w=[b_biasT[i]])

    o_mT = sb("o_mT", [128, 4, NTOK], BF16)
    b_omT = Buf("o_mT")
    rl = xt[1][:, 0:512]
    b_rl = b_xt[1]

    def mb_P(t):
        h, g, j = steps_m[t]
        a = t % 2
        c, p0 = h // 2, (h % 2) * 64
        q0 = max(j - 4 * g, 0) * 128
        r_ = h * 8 + j // 2
        K.op("pe", lambda e: e.matmul(ps[a][:, q0:512], lhsT=KmT[p0:p0 + 64, c, j * 128:(j + 1) * 128],
                                      rhs=QmT[p0:p0 + 64, c, g * 512 + q0:(g + 1) * 512], start=True, stop=False),
             r=[b_KmT[j]] + [b_QmT[4 * g + u] for u in range(4)], w=[psb[a]])
        K.op("pe", lambda e: e.matmul(ps[a][:, q0:512], lhsT=selm[r_ // 32][:, r_ % 32, :], rhs=biasT[:, g * 512 + q0:(g + 1) * 512],
                                      start=False, stop=(j < 4 * g)),
             r=[b_wst[r_ // 32], b_selm] + [b_biasT[4 * g + u] for u in range(4)], w=[psb[a]])
        if j >= 4 * g:
            K.op("pe", lambda e: e.matmul(ps[a][:, q0:q0 + 128], lhsT=ident, rhs=mmb, start=False, stop=True), r=[b_cbf], w=[psb[a]])
        K.op("act", lambda e: e.activation(out=A_sb[a][:, q0:], in_=ps[a][:, q0:], func=AF.Exp), r=[psb[a]], w=[b_A[a]])

    def mb_AV(t):
        h, g, j = steps_m[t]
        a = t % 2
        c, p0 = h // 2, (h % 2) * 64
        q0 = max(j - 4 * g, 0) * 128
        ob = 4 + (h * 4 + g) % 2
        lb = 2 + (h * 4 + g) % 2
        first, last = (j == 0), (j == 4 * g + 3)
        K.op("pe", lambda e: e.matmul(ps[ob][:, q0:], lhsT=Vm[:, j, c * 128:(c + 1) * 128], rhs=A_sb[a][:, q0:], start=first, stop=last),
             r=[b_A[a], b_Vm[j]], w=[psb[ob]])
        K.op("pe", lambda e: e.matmul(ps[lb][:, q0:], lhsT=ones_bf[:], rhs=A_sb[a][:, q0:], start=first, stop=last),
             r=[b_A[a], b_ones], w=[psb[lb]])
        if last:
            K.op("dve", lambda e: e.reciprocal(out=rl[p0:p0 + 64, :], in_=ps[lb][p0:p0 + 64, :]), r=[psb[lb]], w=[b_rl])
            K.op("dve", lambda e: e.tensor_tensor(out=o_mT[p0:p0 + 64, c, g * 512:(g + 1) * 512], in0=ps[ob][p0:p0 + 64, :],
                                                  in1=rl[p0:p0 + 64, :], op=ALU.mult), r=[psb[ob], b_rl], w=[b_omT])

    selm = [wst[u][:].rearrange("p a b -> p (a b)").bitcast(BF16).rearrange("p (r m) -> p r m", r=32) for u in range(2)]
    if STAGE >= 3:
        for u in range(2):
            K.op("dve", lambda e, u=u: e.tensor_copy(out=selm[u][0:64], in_=id30k[:, 32 * u:32 * u + 32].unsqueeze(2).broadcast_to([64, 32, 128])),
                 r=[b_selm], w=[b_wst[u]])
            K.op("dve", lambda e, u=u: e.memset(selm[u][64:128], 0.0), w=[b_wst[u]])
    steps_m = []
    for h in range(8):
        for g in range(4):
            for j in range(0, 4 * g + 4):
                steps_m.append((h, g, j))
    if STAGE >= 3 and os.environ.get('KB2', '1') == '1':
        mb_P(0)
        for t in range(len(steps_m)):
            if t + 1 < len(steps_m):
                mb_P(t + 1)
            mb_AV(t)
        K.dma("sp", lambda e: e.dma_start(out=dbg_o[1, :, :, 0:S], in_=o_mT[:, :, 0:S]), r=[b_omT])

    if STAGE >= 4 and (not SIM or os.environ.get("KBR", "sb") in ("mb", "both")):
        sample_attn("mb")
    if STAGE >= 4:
        if SIM and os.environ.get("KBR", "sb") == "sb":
            K.op("pool", lambda e: e.memset(o_mT[:, :, S:NTOK], 0.0), w=[b_omT])
        if SIM and os.environ.get("KBR", "sb") == "mb":
            K.op("pool", lambda e: e.memset(o_sT[:, :, S:NTOK], 0.0), w=[b_osT])
        K.dma("sp", lambda e: e.dma_start(out=dbg_o[0, :, :, S:NTOK], in_=o_sT[:, :, S:NTOK]), r=[b_osT])
        K.dma("sp", lambda e: e.dma_start(out=dbg_o[1, :, :, S:NTOK], in_=o_mT[:, :, S:NTOK]), r=[b_omT])

    if STAGE >= 5:
        K.barrier()
        mT = [QsT, KsT]
        bm_T = [Buf(f"mT{i}") for i in range(NT)]
        Wps_h = view(Vs, BF16, 0, [128, 4, 512])
        Wpm_h = view(Vs, BF16, 4096, [128, 4, 512])
        sg = [view(Vs, F32, 8192, [128, 512]), view(Vs, F32, 10240, [128, 512])]
        mm_ = [view(Vs, F32, 12288, [128, 512]), view(Vs, F32, 14336, [128, 512])]
        bWp, bsg, bmm = Buf("Wp"), [Buf("sg0"), Buf("sg1")], [Buf("mm0"), Buf("mm1")]
        bwst2, bwb2 = [Buf("wst0c"), Buf("wst1c")], [Buf("wb0c"), Buf("wb1c")]

        def load_any(src_halves, dsts, bdst, engs=("pool", "pool")):
            for hh, (src, dst) in enumerate(zip(src_halves, dsts)):
                u = hh % 2
                shp = list(src.shape)
                stv = view(wst[u], F32, 0, shp)
                K.dma("sp", lambda e, src=src, stv=stv: e.dma_start(out=stv, in_=src), w=[bwst2[u]])
                if engs[hh] == "pool":
                    K.op("pool", lambda e, dst=dst, stv=stv: e.tensor_copy(out=dst, in_=stv), r=[bwst2[u]], w=[bdst])
                else:
                    K.op("act", lambda e, dst=dst, stv=stv: e.activation(out=dst, in_=stv, func=AF.Copy), r=[bwst2[u]], w=[bdst])

        w_ps_v = w_ps.rearrange("(k p) n -> p k n", p=128)
        w_pm_v = w_pm.rearrange("(k p) n -> p k n", p=128)
        w_o_v = w_o.rearrange("(k p) n -> p k n", p=128)
        mb16 = hb
        for cb in range(2):
            load_any([w_in_v[:, 0:4, (6 + cb) * 512:(7 + cb) * 512], w_in_v[:, 4:8, (6 + cb) * 512:(7 + cb) * 512]],
                     [wb[0][:, 0:4, :], wb[0][:, 4:8, :]], bwb2[0])
            load_any([w_in_v[:, 0:4, (8 + cb) * 512:(9 + cb) * 512], w_in_v[:, 4:8, (8 + cb) * 512:(9 + cb) * 512]],
                     [wb[1][:, 0:4, :], wb[1][:, 4:8, :]], bwb2[1])
            load_any([w_ps_v[:, :, cb * 512:(cb + 1) * 512], w_pm_v[:, :, cb * 512:(cb + 1) * 512]], [Wps_h, Wpm_h], bWp)
            for i in range(NT):
                tsl = slice(i * 128, (i + 1) * 128)
                for kc in range(8):
                    K.op("pe", lambda e, kc=kc, tsl=tsl: e.matmul(ps[0][:], lhsT=xnT[:, kc, tsl], rhs=wb[0][:, kc, :], start=(kc == 0), stop=(kc == 7)),
                         r=[b_xnT[i], bwb2[0]], w=[psb[0]])
                for kc in range(8):
                    K.op("pe", lambda e, kc=kc, tsl=tsl: e.matmul(ps[1][:], lhsT=xnT[:, kc, tsl], rhs=wb[1][:, kc, :], start=(kc == 0), stop=(kc == 7)),
                         r=[b_xnT[i], bwb2[1]], w=[psb[1]])
                for c in range(4):
                    K.op("pe", lambda e, c=c, tsl=tsl: e.matmul(ps[2][:], lhsT=o_sT[:, c, tsl], rhs=Wps_h[:, c, :], start=(c == 0), stop=(c == 3)),
                         r=[b_osT, bWp], w=[psb[2]])
                for c in range(4):
                    K.op("pe", lambda e, c=c, tsl=tsl: e.matmul(ps[3][:], lhsT=o_mT[:, c, tsl], rhs=Wpm_h[:, c, :], start=(c == 0), stop=(c == 3)),
                         r=[b_omT, bWp], w=[psb[3]])
                K.op("act", lambda e: e.activation(out=sg[0], in_=ps[0][:], func=AF.Sigmoid), r=[psb[0]], w=[bsg[0]])
                K.op("act", lambda e: e.activation(out=sg[1], in_=ps[1][:], func=AF.Sigmoid), r=[psb[1]], w=[bsg[1]])
                K.op("dve", lambda e: e.tensor_tensor(out=mm_[0], in0=sg[0], in1=ps[2][:], op=ALU.mult), r=[bsg[0], psb[2]], w=[bmm[0]])
                K.op("dve", lambda e: e.tensor_tensor(out=mm_[1], in0=sg[1], in1=ps[3][:], op=ALU.mult), r=[bsg[1], psb[3]], w=[bmm[1]])
                b = i % 2
                K.op("pool", lambda e, b=b: e.tensor_tensor(out=mb16[b][:], in0=mm_[0], in1=mm_[1], op=ALU.add), r=bmm, w=[b_hb[b]])
                to_T(mb16[b], b_hb[b], mT[cb], bm_T[i], i, 4 + i % 2)

        K.barrier()
        load_any([w_o_v[:, 0:4, 0:512], w_o_v[:, 4:8, 0:512]], [wb[0][:, 0:4, :], wb[0][:, 4:8, :]], bwb2[0])
        load_any([w_o_v[:, 0:4, 512:1024], w_o_v[:, 4:8, 512:1024]], [wb[1][:, 0:4, :], wb[1][:, 4:8, :]], bwb2[1])
        bg2 = Buf("gbc2")
        K.dma("sp", lambda e: e.dma_start(out=gbc[:], in_=g_ffn.partition_broadcast(128)), w=[bg2])
        xn2f = view(Vs, F32, 0, [128, D])
        xTf = view(Vs, F32, 4096, [128, 8, 128])
        combT = view(Vs, BF16, 8192, [128, NTOK])
        SelE = view(Vs, BF16, 8192 + 2 * NTOK, [128, 16, 128])
        bx2, bxT, bcT, bSel = Buf("xn2f"), Buf("xTf"), Buf("combT"), Buf("SelE")
        wr = sb("wr", [128, 8, 20], F32)
        brb = sb("brb", [128, 20], F32)
        bwr = Buf("wr")
        K.dma("sp", lambda e: e.dma_start(out=wr[:], in_=w_r.rearrange("(k p) n -> p k n", p=128)), w=[bwr])
        K.dma("sp", lambda e: e.dma_start(out=brb[:], in_=b_r.partition_broadcast(128)), w=[bwr])
        rt = sb("rt", [128, 64], F32)
        comb = sb("comb", [128, 16], F32)
        combb = sb("combb", [128, 16], BF16)
        brt = Buf("rt")
        b_x1d = [Buf(f"x1d{i}") for i in range(NT)]
        K.op("dve", lambda e: e.tensor_copy(out=SelE[0:16], in_=ident[0:16, 0:16].unsqueeze(2).broadcast_to([16, 16, 128])), r=[b_cbf], w=[bSel])
        lgs, ohg, esel, msk1, e2, msk2, c4 = rt[:, 0:20], rt[:, 20:24], rt[:, 24:28], rt[:, 28:32], rt[:, 32:36], rt[:, 36:40], rt[:, 40:44]
        sc = lambda k: rt[:, 48 + k:49 + k]
        eg = rt[:, 44:48]

        def R(fn, eng="dve"):
            K.op(eng, fn, r=[brt], w=[brt])

        for i in range(NT):
            j = i % 2
            tsl = slice(i * 128, (i + 1) * 128)
            K.dma("sp", lambda e, j=j, tsl=tsl: e.dma_start(out=xt[j][:], in_=xin[tsl, :]), w=[b_xt[j]])
            for half in range(2):
                for kc in range(8):
                    K.op("pe", lambda e, kc=kc, half=half, tsl=tsl: e.matmul(ps[half][:], lhsT=mT[kc // 4][:, kc % 4, tsl], rhs=wb[half][:, kc, :],
                                                                           start=(kc == 0), stop=(kc == 7)), r=[bm_T[i], bwb2[half]], w=[psb[half]])
                K.op("dve", lambda e, j=j, half=half: e.tensor_tensor(out=xt[j][:, half * 512:(half + 1) * 512], in0=xt[j][:, half * 512:(half + 1) * 512],
                                                                    in1=ps[half][:], op=ALU.add), r=[b_xt[j], psb[half]], w=[b_xt[j]])
            K.dma("sp", lambda e, j=j, tsl=tsl: e.dma_start(out=y_out[tsl, :], in_=xt[j][:]), r=[b_xt[j]], w=[b_x1d[i]])
            K.op("act", lambda e, i=i, j=j: e.activation(out=junk[:], in_=xt[j][:], func=AF.Square, accum_out=ss[:, i:i + 1]),
                 r=[b_xt[j]], w=[b_junk, b_ss[i]])
            K.op("act", lambda e, i=i: e.activation(out=rs[:, i:i + 1], in_=ss[:, i:i + 1], func=AF.Sqrt, bias=1e-6, scale=1.0 / D), r=[b_ss[i]], w=[b_rs[i]])
            K.op("dve", lambda e, i=i: e.reciprocal(out=rstd[:, i:i + 1], in_=rs[:, i:i + 1]), r=[b_rs[i]], w=[b_rstd[i]])
            K.op("dve", lambda e, i=i, j=j: e.scalar_tensor_tensor(out=xn2f, in0=xt[j][:], scalar=rstd[:, i:i + 1], in1=gbc[:], op0=ALU.mult, op1=ALU.mult),
                 r=[b_xt[j], b_rstd[i], bg2], w=[bx2])
            K.op("pool", lambda e, j=j: e.tensor_copy(out=xn[j][:], in_=xn2f), r=[bx2], w=[b_xn[j]])
            pst = ps[2].bitcast(BF16)
            for kc in range(8):
                K.op("pe", lambda e, kc=kc, j=j, pst=pst: e.transpose(out=pst[:, kc * 128:(kc + 1) * 128], in_=xn[j][:, kc * 128:(kc + 1) * 128], identity=ident),
                     r=[b_xn[j], b_cbf], w=[psb[2]])
            K.op("act", lambda e, tsl=tsl, pst=pst: e.activation(out=xnT[:, :, tsl], in_=pst.rearrange("p (k t) -> p k t", k=8), func=AF.Copy),
                 r=[psb[2]], w=[b_xnT[i]])
            for half in range(2):
                for kk in range(4):
                    kc = half * 4 + kk
                    K.op("pe", lambda e, kc=kc, kk=kk, half=half: e.transpose(out=ps[3 + half][:, kk * 128:(kk + 1) * 128], in_=xn2f[:, kc * 128:(kc + 1) * 128], identity=identf),
                         r=[bx2, b_cst], w=[psb[3 + half]])
                K.op("act" if half == 0 else "dve",
                     (lambda e, half=half: e.activation(out=xTf[:, half * 4:half * 4 + 4, :], in_=ps[3 + half][:].rearrange("p (k t) -> p k t", k=4), func=AF.Copy)) if half == 0 else
                     (lambda e, half=half: e.tensor_copy(out=xTf[:, half * 4:half * 4 + 4, :], in_=ps[3 + half][:].rearrange("p (k t) -> p k t", k=4))),
                     r=[psb[3 + half]], w=[bxT])
            for kc in range(8):
                K.op("pe", lambda e, kc=kc: e.matmul(ps[5][:, 0:20], lhsT=xTf[:, kc, :], rhs=wr[:, kc, :], start=(kc == 0), stop=(kc == 7)),
                     r=[bxT, bwr], w=[psb[5]])
            K.op("dve", lambda e: e.tensor_tensor(out=lgs, in0=ps[5][:, 0:20], in1=brb[:], op=ALU.add), r=[psb[5], bwr, brt], w=[brt])
            R(lambda e: e.tensor_reduce(out=sc(0), in_=lgs[:, 0:4], axis=AX.X, op=ALU.max))
            R(lambda e: e.tensor_scalar(out=ohg, in0=lgs[:, 0:4], scalar1=sc(0), scalar2=None, op0=ALU.is_ge))
            R(lambda e: e.tensor_scalar(out=sc(1), in0=sc(0), scalar1=-1.0, scalar2=None, op0=ALU.mult))
            R(lambda e: e.activation(out=eg, in_=lgs[:, 0:4], func=AF.Exp, bias=sc(1), accum_out=sc(2)), "act")
            R(lambda e: e.reciprocal(out=sc(3), in_=sc(2)))
            R(lambda e: e.tensor_scalar(out=esel, in0=lgs[:, 4:8], scalar1=ohg[:, 0:1], scalar2=None, op0=ALU.mult))
            for g in range(1, 4):
                R(lambda e, g=g: e.scalar_tensor_tensor(out=esel, in0=lgs[:, 4 + 4 * g:8 + 4 * g], scalar=ohg[:, g:g + 1], in1=esel, op0=ALU.mult, op1=ALU.add))
            R(lambda e: e.tensor_reduce(out=sc(4), in_=esel, axis=AX.X, op=ALU.max))
            R(lambda e: e.tensor_scalar(out=msk1, in0=esel, scalar1=sc(4), scalar2=None, op0=ALU.is_ge))
            R(lambda e: e.scalar_tensor_tensor(out=e2, in0=msk1, scalar=-1e30, in1=esel, op0=ALU.mult, op1=ALU.add))
            R(lambda e: e.tensor_reduce(out=sc(5), in_=e2, axis=AX.X, op=ALU.max))
            R(lambda e: e.tensor_scalar(out=msk2, in0=e2, scalar1=sc(5), scalar2=None, op0=ALU.is_ge))
            R(lambda e: e.tensor_tensor(out=sc(6), in0=sc(5), in1=sc(4), op=ALU.subtract))
            R(lambda e: e.activation(out=sc(7), in_=sc(6), func=AF.Exp), "act")
            R(lambda e: e.tensor_scalar(out=sc(8), in0=sc(7), scalar1=1.0, scalar2=None, op0=ALU.add))
            R(lambda e: e.reciprocal(out=sc(9), in_=sc(8)))
            R(lambda e: e.tensor_tensor(out=sc(10), in0=sc(3), in1=sc(9), op=ALU.mult))
            R(lambda e: e.tensor_tensor(out=sc(11), in0=sc(10), in1=sc(7), op=ALU.mult))
            R(lambda e: e.tensor_scalar(out=c4, in0=msk1, scalar1=sc(10), scalar2=None, op0=ALU.mult))
            R(lambda e: e.scalar_tensor_tensor(out=c4, in0=msk2, scalar=sc(11), in1=c4, op0=ALU.mult, op1=ALU.add))
            for g in range(4):
                R(lambda e, g=g: e.tensor_scalar(out=comb[:, 4 * g:4 * g + 4], in0=c4, scalar1=ohg[:, g:g + 1], scalar2=None, op0=ALU.mult))
            R(lambda e: e.tensor_copy(out=combb[:], in_=comb[:]))
            pst5 = ps[5].bitcast(BF16)
            K.op("pe", lambda e: e.transpose(out=pst5[0:16, 128:256], in_=combb[:], identity=ident), r=[brt, b_cbf], w=[psb[5]])
            K.op("dve", lambda e, tsl=tsl: e.tensor_copy(out=combT[0:16, tsl], in_=pst5[0:16, 128:256]), r=[psb[5]], w=[bcT])

        K.barrier()
        bg3 = Buf("gbc3")
        K.dma("sp", lambda e: e.dma_start(out=gbc[:], in_=g_fin.partition_broadcast(128)), w=[bg3])
        hdn = [view(QsT, BF16, 0, [128, 16, 512]), view(KsT, BF16, 0, [128, 16, 512])]
        bhd = Buf("hdn")
        sa = [view(xn[0], F32, 0, [128, 512]), view(xn[1], F32, 0, [128, 512])]
        tt = [xn2f[:, 0:512], xn2f[:, 512:1024]]
        bsa, btt = [Buf("sa0"), Buf("sa1")], [Buf("tt0"), Buf("tt1")]
        wdb = [view(o_sT, BF16, 0, [128, 2, D]), view(o_mT, BF16, 0, [128, 2, D])]
        bwd = [Buf("wd0"), Buf("wd1")]
        w_eg_v = w_eg.rearrange("e (k p) n -> e p k n", p=128)
        w_eu_v = w_eu.rearrange("e (k p) n -> e p k n", p=128)
        w_ed_v = w_ed.rearrange("e (k p) n -> e p k n", p=128)
        groups = [(g * 512, 512) for g in range(4)] + [(S, 128)]
        it_ = 0
        for (t0, nt) in groups:
            for ex in range(16):
                u = ex % 2
                load_any([w_eg_v[ex], w_eu_v[ex]], [wb[u][:, :, 0:256], wb[u][:, :, 256:512]], bwb2[u],
                         engs=(("pool", "act") if os.environ.get("KUP", "act") == "act" else ("pool", "pool")))
                K.op("pe", lambda e, ex=ex, t0=t0, nt=nt: e.matmul(ps[4][:, 0:nt], lhsT=SelE[0:16, ex, :], rhs=combT[0:16, t0:t0 + nt], start=True, stop=True),
                     r=[bSel, bcT], w=[psb[4]])
                for fc in range(2):
                    a = it_ % 2
                    it_ += 1
                    for kc in range(8):
                        K.op("pe", lambda e, kc=kc, u=u, fc=fc, a=a, t0=t0, nt=nt: e.matmul(ps[a][:, 0:nt], lhsT=wb[u][:, kc, fc * 128:(fc + 1) * 128],
                                                                                          rhs=xnT[:, kc, t0:t0 + nt], start=(kc == 0), stop=(kc == 7)),
                             r=[bwb2[u]] + [b_xnT[t0 // 128 + q] for q in range(nt // 128)], w=[psb[a]])
                    for kc in range(8):
                        K.op("pe", lambda e, kc=kc, u=u, fc=fc, a=a, t0=t0, nt=nt: e.matmul(ps[2 + a][:, 0:nt], lhsT=wb[u][:, kc, 256 + fc * 128:256 + (fc + 1) * 128],
                                                                                          rhs=xnT[:, kc, t0:t0 + nt], start=(kc == 0), stop=(kc == 7)),
                             r=[bwb2[u]] + [b_xnT[t0 // 128 + q] for q in range(nt // 128)], w=[psb[2 + a]])
                    K.op("act", lambda e, a=a, nt=nt: e.activation(out=sa[a][:, 0:nt], in_=ps[a][:, 0:nt], func=AF.Silu), r=[psb[a]], w=[bsa[a]])
                    K.op("dve", lambda e, a=a, nt=nt: e.tensor_tensor(out=tt[a][:, 0:nt], in0=sa[a][:, 0:nt], in1=ps[2 + a][:, 0:nt], op=ALU.mult),
                         r=[bsa[a], psb[2 + a]], w=[btt[a]])
                    ch = ex * 2 + fc
                    K.op("dve", lambda e, a=a, nt=nt, ch=ch: e.tensor_tensor(out=hdn[ch // 16][:, ch % 16, 0:nt], in0=tt[a][:, 0:nt], in1=ps[4][:, 0:nt], op=ALU.mult),
                         r=[btt[a], psb[4]], w=[bhd])
            ntile = nt // 128
            for ex in range(16):
                u = ex % 2
                stv = view(wst[u], F32, 0, [128, 2, D])
                K.dma("sp", lambda e, ex=ex, stv=stv: e.dma_start(out=stv, in_=w_ed_v[ex]), w=[bwst2[u]])
                K.op("act", lambda e, u=u, stv=stv: e.activation(out=wdb[u], in_=stv, func=AF.Copy), r=[bwst2[u]], w=[bwd[u]])
                for fc in range(2):
                    ch = ex * 2 + fc
                    for q in range(ntile):
                        for half in range(2):
                            K.op("pe", lambda e, ch=ch, q=q, half=half, u=u, fc=fc: e.matmul(ps[q * 2 + half][:], lhsT=hdn[ch // 16][:, ch % 16, q * 128:(q + 1) * 128],
                                                                                           rhs=wdb[u][:, fc, half * 512:(half + 1) * 512],
                                                                                           start=(ch == 0), stop=(ch == 31)),
                                 r=[bhd, bwd[u]], w=[psb[q * 2 + half]])
            for q in range(ntile):
                i = t0 // 128 + q
                j = i % 2
                tsl = slice(i * 128, (i + 1) * 128)
                K.dma("sp", lambda e, j=j, tsl=tsl: e.dma_start(out=xt[j][:], in_=y_out[tsl, :]), r=[b_x1d[i]], w=[b_xt[j]])
                for half in range(2):
                    K.op("dve", lambda e, j=j, half=half, q=q: e.tensor_tensor(out=xt[j][:, half * 512:(half + 1) * 512], in0=xt[j][:, half * 512:(half + 1) * 512],
                                                                             in1=ps[q * 2 + half][:], op=ALU.add), r=[b_xt[j], psb[q * 2 + half]], w=[b_xt[j]])
                K.op("act", lambda e, i=i, j=j: e.activation(out=junk[:], in_=xt[j][:], func=AF.Square, accum_out=ss[:, i:i + 1]),
                     r=[b_xt[j]], w=[b_junk, b_ss[i]])
                K.op("act", lambda e, i=i: e.activation(out=rs[:, i:i + 1], in_=ss[:, i:i + 1], func=AF.Sqrt, bias=1e-6, scale=1.0 / D), r=[b_ss[i]], w=[b_rs[i]])
                K.op("dve", lambda e, i=i: e.reciprocal(out=rstd[:, i:i + 1], in_=rs[:, i:i + 1]), r=[b_rs[i]], w=[b_rstd[i]])
                K.op("dve", lambda e, i=i, j=j: e.scalar_tensor_tensor(out=xt[j][:], in0=xt[j][:], scalar=rstd[:, i:i + 1], in1=gbc[:], op0=ALU.mult, op1=ALU.mult),
                     r=[b_xt[j], b_rstd[i], bg3], w=[b_xt[j]])
                K.dma("sp", lambda e, j=j, tsl=tsl: e.dma_start(out=y_out[tsl, :], in_=xt[j][:]), r=[b_xt[j], b_x1d[i]], w=[b_x1d[i]])

    K.emit()
    return nc


_NC = None


def kernel(**inputs):
    global _NC
    if _NC is None:
        _NC = build()
    nc = _NC
    xp = np.asarray(inputs["x_prompt"], np.float32)
    xs = np.asarray(inputs["x_sample"], np.float32)
    cst = make_consts()
    tok = np.arange(128)
    col = np.arange(512)
    s_of, t_of = col // 32, col % 4
    same = (tok[:, None] // 4 == s_of[None, :]) & (tok[:, None] < 64)
    m01 = np.stack([same & ((tok[:, None] % 4) < t_of[None, :]), same & ((tok[:, None] % 4) <= t_of[None, :])]).astype(np.float32)
    pt = np.asarray(inputs["page_table"], np.int32)
    in_maps = []
    for c in range(NCORES):
        xin = np.zeros((NTOK, D), np.float32)
        xin[:S] = xp[c]
        xin[S:S + 64] = xs[16 * c:16 * c + 16].reshape(64, D)
        in_maps.append({
            "xin": xin, "cst": cst,
            "g_mix": np.ascontiguousarray(inputs["g_mix"][0][None, :], np.float32),
            "w_in": np.ascontiguousarray(inputs["w_in"][0], np.float32),
            "m01": m01,
            "w_proj_sb": np.ascontiguousarray(inputs["w_proj_sb"][0], np.float32),
            "w_proj_moba": np.ascontiguousarray(inputs["w_proj_moba"][0], np.float32),
            "w_out": np.ascontiguousarray(inputs["w_out"][0], np.float32),
            "g_ffn": np.ascontiguousarray(inputs["g_ffn"][0][None, :], np.float32),
            "g_final": np.ascontiguousarray(np.asarray(inputs["g_final"])[None, :], np.float32),
            "w_router": np.ascontiguousarray(np.concatenate([inputs["w_router_group"][0], inputs["w_router_expert"][0]], axis=1), np.float32),
            "b_router": np.ascontiguousarray(np.concatenate([inputs["b_router_group"][0], inputs["b_router_expert"][0]])[None, :], np.float32),
            "w_expert_gate": np.ascontiguousarray(inputs["w_expert_gate"][0], np.float32),
            "w_expert_up": np.ascontiguousarray(inputs["w_expert_up"][0], np.float32),
            "w_expert_down": np.ascontiguousarray(inputs["w_expert_down"][0], np.float32),
        })
        m = in_maps[-1]
        if SMALL:
            rows = pt[16 * c:16 * c + 16].reshape(-1)
            for nm, key in (("sb_k", "cache_sb_k"), ("sb_v", "cache_sb_v"), ("mb_k", "cache_moba_k"), ("mb_v", "cache_moba_v")):
                m["cache_" + nm] = np.ascontiguousarray(inputs[key][0][rows].reshape(256 * 128, 512)) if (c == 0 or not SIM) else np.zeros((256 * 128, 512), np.float32)
            m["ptab"] = np.arange(256, dtype=np.int32)[None, :]
        else:
            for nm, key in (("sb_k", "cache_sb_k"), ("sb_v", "cache_sb_v"), ("mb_k", "cache_moba_k"), ("mb_v", "cache_moba_v")):
                m["cache_" + nm] = inputs[key][0].reshape(2560 * 128, 512)
            m["ptab"] = np.ascontiguousarray(pt[16 * c:16 * c + 16].reshape(1, 256))
    res = run_bass_kernel_spmd(nc, in_maps, core_ids=list(range(NCORES)), **({"trace": True} if os.environ.get("KTRACE") else {}))
    if os.environ.get("KTRACE"):
        print("EXEC_TIME_NS", res.exec_time_ns)
    outs = res.results
    global _LAST
    _LAST = outs
    kv = np.stack([o["kv_out"] for o in outs])
    y = np.stack([o["y_out"] for o in outs])
    y_prompt = y[:, :S, :]
    y_sample = y[:, S:S + 64, :].reshape(128, 4, D)
    p = [kv[:, k, :S, :].reshape(1, 8, S, 8, 64) for k in range(4)]
    s = [kv[:, k, S:S + 64, :].reshape(1, 128, 4, 8, 64) for k in range(4)]
    return (y_prompt, y_sample, p[0], p[1], p[2], p[3], s[0], s[1], s[2], s[3])
```

```python
import numpy as np
import concourse.bass as bass
import concourse.mybir as mybir
from concourse.bass_utils import run_bass_kernel_spmd

F32 = mybir.dt.float32
BF16 = mybir.dt.bfloat16
I32 = mybir.dt.int32
U32 = mybir.dt.uint32
AF = mybir.ActivationFunctionType
ALU = mybir.AluOpType
AX = mybir.AxisListType

NCORES = 8
D = 1024
S = 2048
NT = 17
NTOK = NT * 128
NEG = -30000.0
import os
STAGE = int(os.environ.get("KSTAGE", "99"))
SIM = os.environ.get("KSIM", "0") == "1"
SMALL = SIM or os.environ.get("KSMALL", "0") == "1"
NSEQ = int(os.environ.get("KNSEQ", "1")) if SIM else 16


class Buf:
    __slots__ = ("name", "lw", "rd", "excl", "last", "last_w")

    def __init__(self, name, excl=False):
        self.name = name
        self.lw = None
        self.rd = []
        self.excl = excl
        self.last = None
        self.last_w = False


class Op:
    __slots__ = ("eng", "fn", "deps", "sig", "need", "isdma", "semidx")

    def __init__(self, eng, fn, isdma):
        self.eng = eng
        self.fn = fn
        self.deps = {}
        self.sig = None
        self.need = False
        self.isdma = isdma
        self.semidx = -1


class Sched:
    ENG = ["pe", "act", "dve", "pool", "sp"]
    NDMA = 24
    NSW = int(os.environ.get("KNSW", "40"))
    ROLL = 30000

    def __init__(self, nc):
        self.nc = nc
        self.ops = {e: [] for e in self.ENG}
        self.dma_last = [None] * self.NDMA
        self.dma_rr = 0
        self.dmas = []
        self.sw_last = [None] * self.NSW
        self.sw_rr = 0
        self.sw_fresh = SIM

    def _deps(self, o, r, w):
        for b in r:
            if b.excl:
                if b.last is not None and b.last is not o:
                    o.deps.setdefault(b.last, "raw" if b.last_w else "war")
                b.last, b.last_w = o, False
                continue
            if b.lw is not None and b.lw is not o:
                o.deps.setdefault(b.lw, "raw")
            b.rd.append(o)
        for b in w:
            if b.excl:
                if b.last is not None and b.last is not o:
                    o.deps[b.last] = "raw"
                b.last, b.last_w = o, True
                continue
            if b.lw is not None and b.lw is not o:
                o.deps[b.lw] = "raw"
            for x in b.rd:
                if x is not o:
                    o.deps.setdefault(x, "war")
            b.rd = []
            b.lw = o

    def op(self, eng, fn, r=(), w=()):
        o = Op(eng, fn, False)
        self._deps(o, r, w)
        self.ops[eng].append(o)
        return o

    def barrier(self):
        lasts = []
        for e in self.ENG:
            comp = [o for o in self.ops[e] if not o.isdma]
            if comp:
                lasts.append(comp[-1])
        lasts += [d for d in self.dma_last if d is not None] + [d for d in self.sw_last if d is not None]
        if self.sw_fresh:
            lasts += [d for d in self.dmas if d.semidx < 0]
        for e in self.ENG:
            o = Op(e, lambda eng: eng.nop(), False)
            for d in lasts:
                o.deps[d] = "raw"
            self.ops[e].append(o)

    def dma(self, q, fn, r=(), w=(), sw=False):
        o = Op(q, fn, True)
        if sw and self.sw_fresh:
            o.semidx = -1
        elif sw:
            i = self.sw_rr
            self.sw_rr = (i + 1) % self.NSW
            if self.sw_last[i] is not None:
                o.deps[self.sw_last[i]] = "raw"
            self.sw_last[i] = o
            o.semidx = self.NDMA + i
        else:
            i = self.dma_rr
            self.dma_rr = (i + 1) % self.NDMA
            if self.dma_last[i] is not None:
                o.deps[self.dma_last[i]] = "raw"
            self.dma_last[i] = o
            o.semidx = i
        self._deps(o, r, w)
        self.ops[q].append(o)
        self.dmas.append(o)
        return o

    def emit(self):
        nc = self.nc
        for e in self.ENG:
            for o in self.ops[e]:
                keep = {}
                for d, kind in o.deps.items():
                    if d.isdma:
                        keep[d] = kind
                    elif d.eng == o.eng and not o.isdma:
                        if o.eng == "pe":
                            continue
                        keep[d] = kind
                    else:
                        keep[d] = kind
                o.deps = keep
                for d in keep:
                    d.need = True
        nsem = self.NDMA + self.NSW
        dma_sems = [nc.alloc_semaphore(name=f"dq{i}") for i in range(nsem)]
        dma_cnt = [0] * nsem
        for o in self.dmas:
            if o.semidx < 0:
                dma_sems.append(nc.alloc_semaphore(name=f"dsw{len(dma_sems)}"))
                dma_cnt.append(0)
                o.semidx = len(dma_sems) - 1
            dma_cnt[o.semidx] += 16
            o.sig = (dma_sems[o.semidx], dma_cnt[o.semidx])
        for e in self.ENG:
            sem = None
            cnt = 0
            k = 0
            for o in self.ops[e]:
                if o.isdma or not o.need:
                    continue
                if sem is None or cnt >= self.ROLL:
                    sem = nc.alloc_semaphore(name=f"e_{e}_{k}")
                    k += 1
                    cnt = 0
                cnt += 1
                o.sig = (sem, cnt)
        handles = {"pe": "tensor", "act": "scalar", "dve": "vector", "pool": "gpsimd", "sp": "sync"}
        final = [(dma_sems[i], dma_cnt[i]) for i in range(len(dma_sems)) if dma_cnt[i] > 0]
        sched = self

        with nc.Block() as block:
            def run(e, eng):
                known = {}
                for o in sched.ops[e]:
                    waits = {}
                    for d in o.deps:
                        s, v = d.sig
                        if waits.get(s, 0) < v:
                            waits[s] = v
                    for s, v in waits.items():
                        if known.get(s, 0) >= v:
                            continue
                        eng.wait_ge(s, v)
                        known[s] = v
                    ins = o.fn(eng)
                    if o.sig is not None:
                        ins.then_inc(o.sig[0], 16 if o.isdma else 1)
                if e == "sp":
                    for s, v in final:
                        eng.wait_ge(s, v)

            @block.tensor
            def _(eng):
                run("pe", eng)

            @block.scalar
            def _(eng):
                run("act", eng)

            @block.vector
            def _(eng):
                run("dve", eng)

            @block.gpsimd
            def _(eng):
                run("pool", eng)

            @block.sync
            def _(eng):
                run("sp", eng)


C_ID, C_NTRI, C_MSB, C_MMB = 0, 128, 256, 384
C_COS, C_SIN = 512, 512 + NT * 8
C_W = 512 + 2 * NT * 8


def make_consts():
    c = np.zeros((128, C_W), np.float32)
    p = np.arange(128)
    c[:, C_ID:C_ID + 128] = np.eye(128, dtype=np.float32)
    c[:, C_NTRI:C_NTRI + 128] = -(p[:, None] >= p[None, :]).astype(np.float32)
    c[:, C_MSB:C_MSB + 128] = NEG * (p[:, None] >= p[None, :])
    c[:, C_MMB:C_MMB + 128] = NEG * (p[:, None] > p[None, :])
    half = 8
    inv = np.float32(500000.0) ** (-np.arange(half, dtype=np.float32) / half)
    pos = np.zeros((NT, 128), np.float32)
    for i in range(16):
        pos[i] = i * 128 + p
    pos[16, :64] = 2048 + (p[:64] % 4)
    ang = pos[:, :, None].astype(np.float32) * inv[None, None, :]
    cs = np.cos(ang).astype(np.float32).transpose(1, 0, 2)
    sn = np.sin(ang).astype(np.float32).transpose(1, 0, 2)
    c[:, C_COS:C_COS + NT * 8] = cs.reshape(128, NT * 8)
    c[:, C_SIN:C_SIN + NT * 8] = sn.reshape(128, NT * 8)
    return c


def build():
    nc = bass.Bass("TRN2", target_bir_lowering=False)
    K = Sched(nc)

    def dram(name, shape, dt, kind):
        return nc.dram_tensor(name, list(shape), dt, kind=kind).ap()

    xin = dram("xin", [NTOK, D], F32, "ExternalInput")
    cst_d = dram("cst", [128, C_W], F32, "ExternalInput")
    g_mix = dram("g_mix", [1, D], F32, "ExternalInput")
    w_in = dram("w_in", [D, 5120], F32, "ExternalInput")
    NROWS = (256 if SMALL else 2560) * 128
    cache = {nm: dram("cache_" + nm, [NROWS, 512], F32, "ExternalInput") for nm in ("sb_k", "sb_v", "mb_k", "mb_v")}
    ptab = dram("ptab", [1, 256], I32, "ExternalInput")
    m01_d = dram("m01", [2, 128, 512], F32, "ExternalInput")
    w_ps = dram("w_proj_sb", [512, D], F32, "ExternalInput")
    w_pm = dram("w_proj_moba", [512, D], F32, "ExternalInput")
    w_o = dram("w_out", [D, D], F32, "ExternalInput")
    g_ffn = dram("g_ffn", [1, D], F32, "ExternalInput")
    g_fin = dram("g_final", [1, D], F32, "ExternalInput")
    w_r = dram("w_router", [D, 20], F32, "ExternalInput")
    b_r = dram("b_router", [1, 20], F32, "ExternalInput")
    w_eg = dram("w_expert_gate", [16, D, 256], F32, "ExternalInput")
    w_eu = dram("w_expert_up", [16, D, 256], F32, "ExternalInput")
    w_ed = dram("w_expert_down", [16, 256, D], F32, "ExternalInput")
    kv_out = dram("kv_out", [4, NTOK, 512], F32, "ExternalOutput")
    y_out = dram("y_out", [NTOK, D], F32, "ExternalOutput")

    def sb(name, shape, dt):
        return nc.alloc_sbuf_tensor("sb_" + name, list(shape), dt).ap()

    ps = [nc.alloc_psum_tensor(f"ps{i}", [128, 512], F32).ap() for i in range(8)]
    psb = [Buf(f"ps{i}", excl=True) for i in range(8)]

    cst = sb("cst", [128, C_W], F32)
    b_cst = Buf("cst")
    K.dma("sp", lambda e: e.dma_start(out=cst[:], in_=cst_d[:]), w=[b_cst])
    cbf = sb("cbf", [128, 512], BF16)
    b_cbf = Buf("cbf")
    K.op("dve", lambda e: e.tensor_copy(out=cbf[:], in_=cst[:, 0:512]), r=[b_cst], w=[b_cbf])
    ident = cbf[:, C_ID:C_ID + 128]
    ntri = cbf[:, C_NTRI:C_NTRI + 128]
    msb = cbf[:, C_MSB:C_MSB + 128]
    mmb = cbf[:, C_MMB:C_MMB + 128]
    cosT = cst[:, C_COS:C_COS + NT * 8].rearrange("p (t f) -> p t f", f=8)
    sinT = cst[:, C_SIN:C_SIN + NT * 8].rearrange("p (t f) -> p t f", f=8)
    gbc = sb("gbc", [128, D], F32)
    b_gbc = Buf("gbc")
    K.dma("sp", lambda e: e.dma_start(out=gbc[:], in_=g_mix.partition_broadcast(128)), w=[b_gbc])

    xnT = sb("xnT", [128, 8, NTOK], BF16)
    b_xnT = [Buf(f"xnT{i}") for i in range(NT)]
    QsT = sb("QsT", [128, 4, NTOK], BF16)
    KsT = sb("KsT", [128, 4, NTOK], BF16)
    Vs = sb("Vs", [128, NT, 512], BF16)
    b_QsT = [Buf(f"QsT{i}") for i in range(NT)]
    b_KsT = [Buf(f"KsT{i}") for i in range(NT)]
    b_Vs = [Buf(f"Vs{i}") for i in range(NT)]

    xt = [sb(f"xt{j}", [128, D], F32) for j in range(2)]
    b_xt = [Buf(f"xt{j}") for j in range(2)]
    junk = sb("junk", [128, D], BF16)
    b_junk = Buf("junk")
    ss = sb("ss", [128, NT], F32)
    rs = sb("rs", [128, NT], F32)
    rstd = sb("rstd", [128, NT], F32)
    b_ss = [Buf(f"ss{i}") for i in range(NT)]
    b_rs = [Buf(f"rs{i}") for i in range(NT)]
    b_rstd = [Buf(f"rstd{i}") for i in range(NT)]
    xn = [sb(f"xn{j}", [128, D], BF16) for j in range(2)]
    b_xn = [Buf(f"xn{j}") for j in range(2)]

    for i in range(int(os.environ.get("KNT", NT))):
        j = i % 2
        K.dma("sp", lambda e, i=i, j=j: e.dma_start(out=xt[j][:], in_=xin[i * 128:(i + 1) * 128, :]), w=[b_xt[j]])
        K.op("act", lambda e, i=i, j=j: e.activation(out=junk[:], in_=xt[j][:], func=AF.Square,
                                                       accum_out=ss[:, i:i + 1]),
             r=[b_xt[j]], w=[b_junk, b_ss[i]])
        K.op("act", lambda e, i=i: e.activation(out=rs[:, i:i + 1], in_=ss[:, i:i + 1], func=AF.Sqrt,
                                                 bias=1e-6, scale=1.0 / D), r=[b_ss[i]], w=[b_rs[i]])
        K.op("dve", lambda e, i=i: e.reciprocal(out=rstd[:, i:i + 1], in_=rs[:, i:i + 1]), r=[b_rs[i]], w=[b_rstd[i]])
        K.op("dve", lambda e, i=i, j=j: e.scalar_tensor_tensor(out=xn[j][:], in0=xt[j][:], scalar=rstd[:, i:i + 1],
                                                                in1=gbc[:], op0=ALU.mult, op1=ALU.mult),
             r=[b_xt[j], b_rstd[i], b_gbc], w=[b_xn[j]])
        pb = i % 2
        pst = ps[pb].bitcast(BF16)
        for kc in range(8):
            K.op("pe", lambda e, kc=kc, j=j, pst=pst: e.transpose(out=pst[:, kc * 128:(kc + 1) * 128],
                                                                     in_=xn[j][:, kc * 128:(kc + 1) * 128],
                                                                     identity=ident),
                 r=[b_xn[j], b_cbf], w=[psb[pb]])
        K.op("act", lambda e, i=i, pst=pst: e.activation(out=xnT[:, :, i * 128:(i + 1) * 128],
                                                          in_=pst.rearrange("p (k t) -> p k t", k=8), func=AF.Copy),
             r=[psb[pb]], w=[b_xnT[i]])

    wb = [sb(f"wb{j}", [128, 8, 512], BF16) for j in range(2)]
    b_wb = [Buf(f"wb{j}") for j in range(2)]
    w_in_v = w_in.rearrange("(k p) n -> p k n", p=128)
    wcount = [0]

    wst = [sb(f"wst{j}", [128, 4, 512], F32) for j in range(2)]
    b_wst = [Buf(f"wst{j}") for j in range(2)]

    def load_w(c):
        j = wcount[0] % 2
        wcount[0] += 1
        for h in range(2):
            K.dma("sp", lambda e, c=c, h=h: e.dma_start(out=wst[h][:], in_=w_in_v[:, 4 * h:4 * h + 4, c * 512:(c + 1) * 512]),
                  w=[b_wst[h]])
            K.op("pool", lambda e, j=j, h=h: e.tensor_copy(out=wb[j][:, 4 * h:4 * h + 4, :], in_=wst[h][:]),
                 r=[b_wst[h]], w=[b_wb[j]])
        return j

    hf = [sb(f"hf{j}", [128, 512], F32) for j in range(2)]
    b_hf = [Buf(f"hf{j}") for j in range(2)]
    hb = [sb(f"hb{j}", [128, 512], BF16) for j in range(2)]
    b_hb = [Buf(f"hb{j}") for j in range(2)]
    cnt = {"proj": 0, "hf": 0, "hb": 0}

    def project(c, i, j):
        pb = 2 + cnt["proj"] % 2
        cnt["proj"] += 1
        for kc in range(8):
            K.op("pe", lambda e, kc=kc, i=i, j=j, pb=pb: e.matmul(ps[pb][:], lhsT=xnT[:, kc, i * 128:(i + 1) * 128],
                                                                    rhs=wb[j][:, kc, :], start=(kc == 0), stop=(kc == 7)),
                 r=[b_xnT[i], b_wb[j]], w=[psb[pb]])
        return pb

    def to_T(src_bf, b_src, dstT, b_dst, i, pbank, dst_ap=None):
        pst = ps[pbank].bitcast(BF16)
        if dst_ap is None:
            dst_ap = dstT[:, :, i * 128:(i + 1) * 128]
        for cc in range(4):
            K.op("pe", lambda e, cc=cc: e.transpose(out=pst[:, cc * 128:(cc + 1) * 128],
                                                     in_=src_bf[:, cc * 128:(cc + 1) * 128], identity=ident),
                 r=[b_src, b_cbf], w=[psb[pbank]])
        K.op("dve", lambda e: e.tensor_copy(out=dst_ap,
                                            in_=pst[:, 0:512].rearrange("p (k t) -> p k t", k=4)),
             r=[psb[pbank]], w=[b_dst])

    def kv_store(which, i, src, b_src):
        K.dma("sp", lambda e: e.dma_start(out=kv_out[which, i * 128:(i + 1) * 128, :], in_=src[:]), r=[b_src])

    samp = {}
    for br in ("sb", "mb"):
        samp[br] = dict(QT=sb(f"sQT_{br}", [128, 4, 128], BF16), KT=sb(f"sKT_{br}", [128, 4, 128], BF16),
                        V=sb(f"sV_{br}", [128, 512], BF16), b=Buf(f"samp_{br}"))
    sQTf = sb("sQTf", [128, 4, 128], F32)
    b_sQTf = Buf("sQTf")

    for (c, kind) in (((1, "k"), (2, "v"), (0, "q"))[:int(os.environ.get("KA1", 3))] if STAGE >= 1 else ()):
        j = load_w(c)
        for i in range(int(os.environ.get("KA1T", NT))):
            pb = project(c, i, j)
            if kind in ("k", "v"):
                a = cnt["hf"] % 2
                cnt["hf"] += 1
                K.op("act", lambda e, a=a, pb=pb: e.activation(out=hf[a][:], in_=ps[pb][:], func=AF.Copy),
                     r=[psb[pb]], w=[b_hf[a]])
                kv_store(0 if kind == "k" else 1, i, hf[a], b_hf[a])
            if kind == "k":
                b = cnt["hb"] % 2
                cnt["hb"] += 1
                K.op("dve", lambda e, b=b, pb=pb: e.tensor_copy(out=hb[b][:], in_=ps[pb][:]), r=[psb[pb]], w=[b_hb[b]])
                if i == 16:
                    to_T(hb[b], b_hb[b], None, samp["sb"]["b"], i, 4 + i % 2, dst_ap=samp["sb"]["KT"][:])
                else:
                    to_T(hb[b], b_hb[b], KsT, b_KsT[i], i, 4 + i % 2)
            elif kind == "v":
                if i == 16:
                    K.op("dve", lambda e, pb=pb: e.tensor_copy(out=samp["sb"]["V"][:], in_=ps[pb][:]), r=[psb[pb]], w=[samp["sb"]["b"]])
                else:
                    K.op("dve", lambda e, i=i, pb=pb: e.tensor_copy(out=Vs[:, i, :], in_=ps[pb][:]), r=[psb[pb]], w=[b_Vs[i]])
            else:
                b = cnt["hb"] % 2
                cnt["hb"] += 1
                K.op("act", lambda e, b=b, pb=pb: e.activation(out=hb[b][:], in_=ps[pb][:], func=AF.Copy, scale=0.125),
                     r=[psb[pb]], w=[b_hb[b]])
                if i == 16:
                    to_T(hb[b], b_hb[b], None, samp["sb"]["b"], i, 4 + i % 2, dst_ap=samp["sb"]["QT"][:])
                else:
                    to_T(hb[b], b_hb[b], QsT, b_QsT[i], i, 4 + i % 2)

    o_sT = sb("o_sT", [128, 4, NTOK], BF16)
    b_osT = Buf("o_sT")
    negones = sb("negones", [128, 128], BF16)
    ones_bf = sb("ones_bf", [128, 128], BF16)
    b_ones = Buf("ones")
    K.op("pool", lambda e: e.memset(negones[:], -1.0), w=[b_ones])
    K.op("pool", lambda e: e.memset(ones_bf[:], 1.0), w=[b_ones])
    e_sb = [xt[j][:, 0:512] for j in range(2)]
    sp_sb = [sb(f"sp_sb{j}", [128, 512], BF16) for j in range(2)]
    A_sb = [junk[:, 512 * j:512 * (j + 1)] for j in range(2)]
    srun = [sb(f"srun{j}", [128, 512], BF16) for j in range(3)]
    b_e = b_xt
    b_sp = [Buf(f"sp{j}") for j in range(2)]
    b_A = [Buf(f"A{j}") for j in range(2)]
    b_srun = [Buf(f"srun{j}") for j in range(3)]
    dbg_o = dram("dbg_o", [2, 128, 4, NTOK], BF16, "ExternalOutput")

    steps = []
    for h in range(8):
        for g in range(4):
            for j in range(4 * g + 3, -1, -1):
                steps.append((h, g, j))

    def zmm(P, pbank, h, g, j, q0, last):
        c, p0 = h // 2, (h % 2) * 64
        K.op("pe", lambda e: e.matmul(P[:, q0:512], lhsT=KsT[p0:p0 + 64, c, j * 128:(j + 1) * 128],
                                      rhs=QsT[p0:p0 + 64, c, g * 512 + q0:(g + 1) * 512], start=True,
                                      stop=(last and j < 4 * g)),
             r=[b_KsT[j]] + [b_QsT[4 * g + t] for t in range(4)], w=[psb[pbank]])
        if j >= 4 * g:
            K.op("pe", lambda e: e.matmul(P[:, q0:q0 + 128], lhsT=ident, rhs=msb, start=False, stop=last),
                 r=[b_cbf], w=[psb[pbank]])

    def sb_X(t):
        h, g, j = steps[t]
        a = t % 2
        q0 = max(j - 4 * g, 0) * 128
        first = (j == 4 * g + 3)
        zmm(ps[a], a, h, g, j, q0, True)
        K.op("act", lambda e: e.activation(out=e_sb[a][:, q0:], in_=ps[a][:, q0:], func=AF.Exp), r=[psb[a]], w=[b_e[a]])
        K.op("act", lambda e: e.activation(out=sp_sb[a][:, q0:], in_=e_sb[a][:, q0:], func=AF.Ln, bias=1.0),
             r=[b_e[a]], w=[b_sp[a]])
        cu, nx = t % 3, (t + 1) % 3
        if first:
            K.op("pool", lambda e: e.memset(srun[nx][:, 0:q0], 0.0), w=[b_srun[nx]])
            K.op("pool", lambda e: e.tensor_copy(out=srun[nx][:, q0:], in_=sp_sb[a][:, q0:]), r=[b_sp[a]], w=[b_srun[nx]])
        elif j > 0:
            if q0 > 0:
                K.op("pool", lambda e: e.tensor_copy(out=srun[nx][:, 0:q0], in_=srun[cu][:, 0:q0]), r=[b_srun[cu]], w=[b_srun[nx]])
            K.op("pool", lambda e: e.tensor_tensor(out=srun[nx][:, q0:], in0=srun[cu][:, q0:], in1=sp_sb[a][:, q0:], op=ALU.add),
                 r=[b_srun[cu], b_sp[a]], w=[b_srun[nx]])

    def sb_Y(t):
        h, g, j = steps[t]
        a = t % 2
        c, p0 = h // 2, (h % 2) * 64
        q0 = max(j - 4 * g, 0) * 128
        first = (j == 4 * g + 3)
        P2 = ps[2 + a]
        zmm(P2, 2 + a, h, g, j, q0, False)
        K.op("pe", lambda e: e.matmul(P2[:, q0:], lhsT=ntri, rhs=sp_sb[a][:, q0:], start=False, stop=first),
             r=[b_sp[a], b_cbf], w=[psb[2 + a]])
        if not first:
            K.op("pe", lambda e: e.matmul(P2[:, q0:], lhsT=negones[:], rhs=srun[t % 3][:, q0:], start=False, stop=True),
                 r=[b_srun[t % 3], b_ones], w=[psb[2 + a]])
        K.op("act", lambda e: e.activation(out=A_sb[a][:, q0:], in_=P2[:, q0:], func=AF.Exp), r=[psb[2 + a]], w=[b_A[a]])
        ob = 4 + (h * 4 + g) % 2
        K.op("pe", lambda e: e.matmul(ps[ob][:, q0:], lhsT=Vs[:, j, c * 128:(c + 1) * 128], rhs=A_sb[a][:, q0:],
                                      start=first, stop=(j == 0), skip_group_check=True),
             r=[b_A[a], b_Vs[j]], w=[psb[ob]])
        if j == 0:
            K.op("dve", lambda e: e.tensor_copy(out=o_sT[p0:p0 + 64, c, g * 512:(g + 1) * 512], in_=ps[ob][p0:p0 + 64, :]),
                 r=[psb[ob]], w=[b_osT])

    if STAGE >= 2:
        sb_X(0)
        for t in range(len(steps)):
            if t + 1 < len(steps):
                sb_X(t + 1)
            sb_Y(t)
        K.dma("sp", lambda e: e.dma_start(out=dbg_o[0, :, :, 0:S], in_=o_sT[:, :, 0:S]), r=[b_osT])

    def view(t, dt, off, shape):
        nd = len(t.shape)
        f = t[:]
        if nd > 2:
            names = " ".join(f"a{i}" for i in range(nd - 1))
            f = f.rearrange(f"p {names} -> p ({names})")
        isz = {F32: 4, I32: 4, BF16: 2}[dt]
        osz = {F32: 4, I32: 4, BF16: 2}[t.dtype]
        if isz != osz:
            f = f.bitcast(dt)
        n = 1
        for d_ in shape[1:]:
            n *= d_
        v = f[0:shape[0], off // isz:off // isz + n]
        if len(shape) > 2:
            names = " ".join(f"b{i}" for i in range(len(shape) - 1))
            kw = {f"b{i}": shape[i + 1] for i in range(len(shape) - 1)}
            v = v.rearrange(f"p ({names}) -> p {names}", **kw)
        return v

    offs = sb("offs", [128, 256], I32)
    ptb = xn[0][:, 0:512].bitcast(I32)
    iot = sb("iot", [128, 1], I32)
    b_offs = Buf("offs")
    K.dma("sp", lambda e: e.dma_start(out=ptb[:], in_=ptab.partition_broadcast(128)), r=[b_xn[0]], w=[b_offs, b_xn[0]])
    K.op("pool", lambda e: e.iota(iot[:], pattern=[[0, 1]], base=0, channel_multiplier=1), w=[b_offs])
    K.op("pool", lambda e: e.tensor_scalar(out=offs[:], in0=ptb[:], scalar1=128.0, scalar2=None, op0=ALU.mult), r=[b_offs], w=[b_offs])
    K.op("pool", lambda e: e.tensor_tensor(out=offs[:], in0=offs[:], in1=iot[:].broadcast_to([128, 256]), op=ALU.add), r=[b_offs], w=[b_offs])

    offs2 = sb("offs2", [128, 128], I32)
    ptb2 = ptb.rearrange("p (j two) -> p j two", two=2)
    K.op("pool", lambda e: e.tensor_copy(out=offs2[0:64, :], in_=ptb2[0:64, :, 0]), r=[b_offs], w=[b_offs])
    K.op("pool", lambda e: e.tensor_copy(out=offs2[64:128, :], in_=ptb2[64:128, :, 1]), r=[b_offs], w=[b_offs])
    K.op("pool", lambda e: e.tensor_scalar(out=offs2[:], in0=offs2[:], scalar1=64.0, scalar2=None, op0=ALU.mult), r=[b_offs], w=[b_offs])
    K.op("pool", lambda e: e.tensor_tensor(out=offs2[:], in0=offs2[:], in1=iot[:].broadcast_to([128, 128]), op=ALU.add), r=[b_offs], w=[b_offs])
    K.op("pool", lambda e: e.tensor_scalar(out=offs2[64:128, :], in0=offs2[64:128, :], scalar1=-64.0, scalar2=None, op0=ALU.add), r=[b_offs], w=[b_offs])

    def sample_attn(br):
        K.barrier()
        is_sb = (br == "sb")
        sm = samp[br]
        poolK, poolV = cache[br + "_k"], cache[br + "_v"]
        o_T, b_oT = (o_sT, b_osT) if is_sb else (o_mT, b_omT)
        Kg = view(QsT, BF16, 0, [128, 16, 512])
        Vg = view(KsT, BF16, 0, [128, 16, 512])
        alt = [view(xt[0], BF16, 0, [128, 4, 512]), view(xt[1], BF16, 0, [128, 4, 512]), view(hf[0], BF16, 0, [128, 2, 512]),
               view(hf[1], BF16, 0, [128, 2, 512])]
        altp = ([alt[0][:, q, :] for q in range(4)] + [alt[1][:, q, :] for q in range(4)] + [alt[k][:, q, :] for k in range(2, 4) for q in range(2)]
                + [sp_sb[0][:], sp_sb[1][:], srun[0][:], srun[1][:]])
        Vpg = [[Vg[:, pg, :] for pg in range(16)], altp]
        KTs = view(Vs, BF16, 0, [128, 4, 2048])
        Qbdf = view(wst[0], F32, 0, [128, 4, 16, 32])
        Qbd = view(wst[1], BF16, 0, [128, 4, 16, 32])
        IndN = view(wst[1], BF16, 4096, [128, 16, 128])
        e_s = view(wb[0], F32, 0, [128, 512])
        sp_s = view(wb[0], BF16, 2048, [128, 512])
        A_s = view(wb[0], BF16, 3072, [128, 512])
        en = view(wb[0], F32, 4096, [128, 512])
        spn = view(wb[0], BF16, 6144, [128, 512])
        An = view(wb[0], BF16, 7168, [128, 512])
        m01 = view(wb[1], F32, 0, [128, 512])
        kmTs = view(wb[1], F32, 2048, [128, 4, 8])
        sblk_s = view(wb[1], F32, 2304, [128, 8])
        mx_s = view(wb[1], F32, 2432, [128, 8])
        b01_s = view(wb[1], BF16, 2560, [128, 8])
        biasTs = view(wb[1], BF16, 2624, [128, 32])
        rl_s = view(wb[1], F32, 4096, [128, 512])
        tmpn = view(wb[1], F32, 6144, [128, 512])
        bq, bm, bn, bkg, bkt = Buf("s_q"), Buf("s_m"), Buf("s_n"), Buf("s_kg"), Buf("s_kt")
        bvg2 = [Buf("s_vg0"), Buf("s_vg1")]
        be, bsp, bA, bkm, bsb_, bbt, brl = Buf("s_e"), Buf("s_sp"), Buf("s_A"), Buf("s_km"), Buf("s_sblk"), Buf("s_bt"), Buf("s_rl")
        K.dma("sp", lambda e: e.dma_start(out=m01, in_=m01_d[0 if is_sb else 1]), w=[bm])
        K.op("pool", lambda e: e.memset(Qbd, 0.0), w=[bq])
        if not is_sb:
            K.op("pool", lambda e: e.memset(Qbdf, 0.0), w=[bq])
            K.op("pool", lambda e: e.memset(IndN[0:8], 0.0), w=[bq])
            K.op("dve", lambda e: e.tensor_copy(out=IndN[0:8].rearrange("p (n u) m -> p n (u m)", u=2),
                                                in_=id30k[0:8, 0:8].unsqueeze(2).broadcast_to([8, 8, 256])), r=[b_selm], w=[bq])
        for h in range(8):
            c, p0 = h // 2, (h % 2) * 64
            K.op("dve", lambda e, c=c, p0=p0, h=h: e.tensor_copy(out=Qbd[p0:p0 + 64, c, :, h * 4:(h + 1) * 4],
                                                               in_=sm["QT"][p0:p0 + 64, c, 0:64].rearrange("p (s t) -> p s t", t=4)),
                 r=[sm["b"]], w=[bq])
            if not is_sb:
                K.op("dve", lambda e, c=c, p0=p0, h=h: e.tensor_copy(out=Qbdf[p0:p0 + 64, c, :, h * 4:(h + 1) * 4],
                                                                   in_=sQTf[p0:p0 + 64, c, 0:64].rearrange("p (s t) -> p s t", t=4)),
                     r=[b_sQTf], w=[bq])
        def znew(bank):
            for s_ in range(16):
                for c in range(4):
                    K.op("pe", lambda e, s_=s_, c=c: e.matmul(ps[bank][:, s_ * 32:(s_ + 1) * 32], lhsT=sm["KT"][:, c, :], rhs=Qbd[:, c, s_, :],
                                                            start=(s_ == 0 and c == 0), stop=False, skip_group_check=True),
                         r=[sm["b"], bq], w=[psb[bank]])
        znew(4)
        K.op("act", lambda e: e.activation(out=en, in_=ps[4][:], func=AF.Exp), r=[psb[4]], w=[bn])
        K.op("dve", lambda e: e.tensor_tensor(out=en, in0=en, in1=m01, op=ALU.mult), r=[bn, bm], w=[bn])
        if is_sb:
            K.op("act", lambda e: e.activation(out=spn, in_=en, func=AF.Ln, bias=1.0), r=[bn], w=[bn])
            znew(5)
            K.op("pe", lambda e: e.matmul(ps[5][:], lhsT=ntri, rhs=spn, start=False, stop=True, skip_group_check=True), r=[bn, b_cbf], w=[psb[5]])
            K.op("act", lambda e: e.activation(out=tmpn, in_=ps[5][:], func=AF.Exp), r=[psb[5]], w=[bn])
            K.op("dve", lambda e: e.tensor_tensor(out=An, in0=tmpn, in1=m01, op=ALU.mult), r=[bn, bm], w=[bn])
        else:
            K.op("dve", lambda e: e.tensor_copy(out=An, in_=en), r=[bn], w=[bn])
        first_o = [True]
        for s_ in range(NSEQ):
            par = s_ % 2
            bvg = bvg2[par]
            order = []
            for pg in range(16):
                order += [(Kg[:, pg, :], poolK, bkg, pg), (Vpg[par][pg], poolV, bvg, pg)]
            if not is_sb and os.environ.get("KTWO", "1") == "1":
                pK2 = poolK.rearrange("(r two) w -> r (two w)", two=2)
                pV2 = poolV.rearrange("(r two) w -> r (two w)", two=2)
                for dm in range(8):
                    col = s_ * 8 + dm
                    K.dma("pool", lambda e, dm=dm, col=col: e.indirect_dma_start(
                        out=Kg[:, 2 * dm:2 * dm + 2, :].rearrange("p a b -> p (a b)"), out_offset=None, in_=pK2[:, :],
                        in_offset=bass.IndirectOffsetOnAxis(ap=offs2[:, col:col + 1], axis=0)), r=[b_offs], w=[bkg], sw=True)
                    K.dma("pool", lambda e, dm=dm, col=col: e.indirect_dma_start(
                        out=Vg[:, 2 * dm:2 * dm + 2, :].rearrange("p a b -> p (a b)"), out_offset=None, in_=pV2[:, :],
                        in_offset=bass.IndirectOffsetOnAxis(ap=offs2[:, col:col + 1], axis=0)), r=[b_offs], w=[bvg2[0]], sw=True)
                order = []
                par = 0
                bvg = bvg2[0]
            for (dstg, poolg, bg_, pg) in order:
                col = s_ * 16 + pg
                K.dma("pool", lambda e, pg=pg, col=col, dstg=dstg, poolg=poolg: e.indirect_dma_start(
                    out=dstg, out_offset=None, in_=poolg[:, :],
                    in_offset=bass.IndirectOffsetOnAxis(ap=offs[:, col:col + 1], axis=0)), r=[b_offs], w=[bg_], sw=True)
            for pg in range(16):
                bk = 6 + pg % 2
                pst = ps[bk].bitcast(BF16)
                for c in range(4):
                    K.op("pe", lambda e, pg=pg, c=c, pst=pst: e.transpose(out=pst[:, c * 128:(c + 1) * 128], in_=Kg[:, pg, c * 128:(c + 1) * 128], identity=ident),
                         r=[bkg, b_cbf], w=[psb[bk]])
                if pg % 2 == 0:
                    K.op("act", lambda e, pg=pg, pst=pst: e.activation(out=KTs[:, :, pg * 128:(pg + 1) * 128], in_=pst[:, 0:512].rearrange("p (c t) -> p c t", c=4), func=AF.Copy),
                         r=[psb[bk]], w=[bkt])
                else:
                    K.op("dve", lambda e, pg=pg, pst=pst: e.tensor_copy(out=KTs[:, :, pg * 128:(pg + 1) * 128], in_=pst[:, 0:512].rearrange("p (c t) -> p c t", c=4)),
                         r=[psb[bk]], w=[bkt])
            if not is_sb:
                for c in range(4):
                    for n in range(8):
                        for u in range(2):
                            K.op("pe", lambda e, c=c, n=n, u=u: e.matmul(ps[5][:, c * 8 + n:c * 8 + n + 1], lhsT=Kg[:, 2 * n + u, c * 128:(c + 1) * 128],
                                                                       rhs=ones256b[:, 0:1], start=(u == 0), stop=(u == 1)),
                                 r=[bkg, b_ones], w=[psb[5]])
                K.op("dve", lambda e: e.tensor_copy(out=kmTs, in_=ps[5][:, 0:32].rearrange("p (c n) -> p c n", c=4)), r=[psb[5]], w=[bkm])
                for c in range(4):
                    K.op("pe", lambda e, c=c, s_=s_: e.matmul(ps[5][0:32, 32:40], lhsT=Qbdf[:, c, s_, :], rhs=kmTs[:, c, :], start=(c == 0), stop=(c == 3)),
                         r=[bq, bkm], w=[psb[5]])
                K.op("dve", lambda e: e.tensor_copy(out=sblk_s[0:32], in_=ps[5][0:32, 32:40]), r=[psb[5]], w=[bsb_])
                K.op("dve", lambda e: e.max(out=mx_s[0:32], in_=sblk_s[0:32]), r=[bsb_], w=[bsb_])
                K.op("dve", lambda e: e.tensor_scalar(out=b01_s[0:32], in0=sblk_s[0:32], scalar1=mx_s[0:32, 2:3], scalar2=1.0,
                                                      op0=ALU.is_ge, op1=ALU.subtract), r=[bsb_], w=[bsb_])
                pst5 = ps[5].bitcast(BF16)
                K.op("pe", lambda e: e.transpose(out=pst5[0:8, 128:160], in_=b01_s[0:32], identity=ident[0:32, 0:32]), r=[bsb_, b_cbf], w=[psb[5]])
                K.op("dve", lambda e: e.tensor_copy(out=biasTs[0:8], in_=pst5[0:8, 128:160]), r=[psb[5]], w=[bbt])

            def zs(bank):
                for pg in range(16):
                    for c in range(4):
                        K.op("pe", lambda e, pg=pg, c=c, s_=s_: e.matmul(ps[bank][:, pg * 32:(pg + 1) * 32], lhsT=KTs[:, c, pg * 128:(pg + 1) * 128], rhs=Qbd[:, c, s_, :],
                                                                start=(pg == 0 and c == 0), stop=False, skip_group_check=True),
                             r=[bkt, bq], w=[psb[bank]])
            zs(0)
            if is_sb:
                K.op("act", lambda e: e.activation(out=e_s, in_=ps[0][:], func=AF.Exp), r=[psb[0]], w=[be])
                K.op("act", lambda e: e.activation(out=sp_s, in_=e_s, func=AF.Ln, bias=1.0), r=[be], w=[bsp])
                zs(1)
                K.op("pe", lambda e: e.matmul(ps[1][:], lhsT=ntri, rhs=sp_s, start=False, stop=False, skip_group_check=True), r=[bsp, b_cbf], w=[psb[1]])
                for pg in range(1, 16):
                    K.op("pe", lambda e, pg=pg: e.matmul(ps[1][:, 0:pg * 32].rearrange("p (a b) -> p a b", b=32), lhsT=negones[:],
                                                       rhs=sp_s[:, pg * 32:(pg + 1) * 32].unsqueeze(1).broadcast_to([128, pg, 32]),
                                                       start=False, stop=False, skip_group_check=True), r=[bsp, b_ones], w=[psb[1]])
                K.op("pe", lambda e, s_=s_: e.matmul(ps[1][:].rearrange("p (a b) -> p a b", b=32), lhsT=negones[:],
                                              rhs=spn[:, s_ * 32:(s_ + 1) * 32].unsqueeze(1).broadcast_to([128, 16, 32]),
                                              start=False, stop=True, skip_group_check=True), r=[bn, b_ones], w=[psb[1]])
                K.op("act", lambda e: e.activation(out=A_s, in_=ps[1][:], func=AF.Exp), r=[psb[1]], w=[bA])
            else:
                for pg in range(16):
                    K.op("pe", lambda e, pg=pg: e.matmul(ps[0][:, pg * 32:(pg + 1) * 32], lhsT=IndN[0:8, pg, :], rhs=biasTs[0:8, :],
                                                       start=False, stop=(pg == 15), skip_group_check=True), r=[bq, bbt], w=[psb[0]])
                K.op("act", lambda e: e.activation(out=A_s, in_=ps[0][:], func=AF.Exp), r=[psb[0]], w=[bA])
            for pg in range(17):
                for h in range(8):
                    c = h // 2
                    if pg < 16:
                        lh, rh, rr = Vpg[par][pg][:, c * 128:(c + 1) * 128], A_s[:, pg * 32 + h * 4:pg * 32 + h * 4 + 4], [bvg, bA]
                    else:
                        lh, rh, rr = sm["V"][:, c * 128:(c + 1) * 128], An[:, s_ * 32 + h * 4:s_ * 32 + h * 4 + 4], [sm["b"], bn]
                    K.op("pe", lambda e, lh=lh, rh=rh, h=h, st=first_o[0], s_=s_: e.matmul(ps[2][:, s_ * 32 + h * 4:s_ * 32 + h * 4 + 4], lhsT=lh, rhs=rh,
                                                                                  start=st, stop=False, skip_group_check=True), r=rr, w=[psb[2]])
                    first_o[0] = False
                if not is_sb:
                    rh = A_s[:, pg * 32:(pg + 1) * 32] if pg < 16 else An[:, s_ * 32:(s_ + 1) * 32]
                    K.op("pe", lambda e, rh=rh, st=(s_ == 0 and pg == 0), s_=s_: e.matmul(ps[3][:, s_ * 32:(s_ + 1) * 32], lhsT=ones_bf[:], rhs=rh,
                                                                                 start=st, stop=False, skip_group_check=True),
                         r=[bA, bn, b_ones], w=[psb[3]])
        ncol = NSEQ * 32
        if not is_sb:
            K.op("dve", lambda e: e.reciprocal(out=rl_s[:, 0:ncol], in_=ps[3][:, 0:ncol]), r=[psb[3]], w=[brl])
        for h in range(8):
            c, p0 = h // 2, (h % 2) * 64
            dst = o_T[p0:p0 + 64, c, S:S + 4 * NSEQ].rearrange("p (s t) -> p s t", t=4)
            src = ps[2][p0:p0 + 64, 0:ncol].rearrange("p (s x) -> p s x", x=32)[:, :, h * 4:(h + 1) * 4]
            if is_sb:
                K.op("dve", lambda e, dst=dst, src=src: e.tensor_copy(out=dst, in_=src), r=[psb[2]], w=[b_oT])
            else:
                rsrc = rl_s[p0:p0 + 64, 0:ncol].rearrange("p (s x) -> p s x", x=32)[:, :, h * 4:(h + 1) * 4]
                K.op("dve", lambda e, dst=dst, src=src, rsrc=rsrc: e.tensor_tensor(out=dst, in0=src, in1=rsrc, op=ALU.mult), r=[psb[2], brl], w=[b_oT])
        K.op("pool", lambda e: e.memset(o_T[:, :, S + 4 * NSEQ:NTOK], 0.0), w=[b_oT])
        K.barrier()

    ones256b = sb("ones256b", [128, 2], BF16)
    K.op("pool", lambda e: e.memset(ones256b[:], 1.0 / 256.0), w=[b_ones])
    if STAGE >= 4 and os.environ.get("KBR", "sb") in ("sb", "both") or (STAGE >= 4 and not SIM):
        sample_attn("sb")

    QmT, KmT, Vm = QsT, KsT, Vs
    b_QmT, b_KmT, b_Vm = b_QsT, b_KsT, b_Vs
    kmf = [sb(f"kmf{j}", [128, 512], F32) for j in range(2)]
    b_kmf = [Buf(f"kmf{j}") for j in range(2)]
    rot = sb("rot", [128, 4, 64], F32)
    b_rot = Buf("rot")
    ones256 = sb("ones256", [128, 2], F32)
    identf = cst[:, C_ID:C_ID + 128]
    kmT = sb("kmT", [128, 4, 8], F32)
    b_kmT = Buf("kmT")
    biasT = sb("biasT", [128, S], BF16)
    b_biasT = [Buf(f"biasT{i}") for i in range(16)]
    b_selm = Buf("selm")
    qTf = sb("qTf", [128, 4, 128], F32)
    b_qTf = Buf("qTf")
    sblk = sb("sblk", [128, 64], F32)
    mx8 = sb("mx8", [128, 64], F32)
    bias01 = sb("bias01", [128, 64], BF16)
    b_sblk, b_mx8, b_bias01 = Buf("sblk"), Buf("mx8"), Buf("bias01")
    id30k = sb("id30k", [64, 64], BF16)

    def rotary(t, b_t, i):
        tv = t[:].rearrange("p (h d) -> p h d", h=8)
        x1, x2 = tv[:, :, 0:8], tv[:, :, 8:16]
        cs = cosT[:, i, :].unsqueeze(1).broadcast_to([128, 8, 8])
        sn = sinT[:, i, :].unsqueeze(1).broadcast_to([128, 8, 8])
        K.op("dve", lambda e: e.tensor_tensor(out=rot[:, 0, :].rearrange("p (h f) -> p h f", h=8), in0=x1, in1=cs, op=ALU.mult), r=[b_t, b_cst], w=[b_rot])
        K.op("dve", lambda e: e.tensor_tensor(out=rot[:, 1, :].rearrange("p (h f) -> p h f", h=8), in0=x2, in1=sn, op=ALU.mult), r=[b_t, b_cst], w=[b_rot])
        K.op("dve", lambda e: e.tensor_tensor(out=rot[:, 2, :].rearrange("p (h f) -> p h f", h=8), in0=x1, in1=sn, op=ALU.mult), r=[b_t, b_cst], w=[b_rot])
        K.op("dve", lambda e: e.tensor_tensor(out=rot[:, 3, :].rearrange("p (h f) -> p h f", h=8), in0=x2, in1=cs, op=ALU.mult), r=[b_t, b_cst], w=[b_rot])
        K.op("dve", lambda e: e.tensor_tensor(out=x1, in0=rot[:, 0, :].rearrange("p (h f) -> p h f", h=8),
                                              in1=rot[:, 1, :].rearrange("p (h f) -> p h f", h=8), op=ALU.subtract), r=[b_rot], w=[b_t])
        K.op("dve", lambda e: e.tensor_tensor(out=x2, in0=rot[:, 2, :].rearrange("p (h f) -> p h f", h=8),
                                              in1=rot[:, 3, :].rearrange("p (h f) -> p h f", h=8), op=ALU.add), r=[b_rot], w=[b_t])

    if STAGE >= 3:
        K.op("pool", lambda e: e.memset(ones256[:], 1.0 / 256.0), w=[b_ones])
        K.op("dve", lambda e: e.tensor_scalar(out=id30k[:], in0=cst[0:64, C_ID:C_ID + 64], scalar1=30000.0, scalar2=None, op0=ALU.mult),
             r=[b_cst], w=[b_selm])
        K.op("pool", lambda e: e.memset(biasT[0:64, 0:256], 0.0), w=[b_biasT[0], b_biasT[1]])
        K.op("pool", lambda e: e.memset(biasT[64:128, :], 0.0), w=[b_selm])
        for (c, kind) in ((4, "k"), (5, "v"), (3, "q")):
            j = load_w(c)
            for i in range(NT):
                pb = project(c, i, j)
                if kind == "v":
                    a = cnt["hf"] % 2
                    cnt["hf"] += 1
                    K.op("act", lambda e, a=a, pb=pb: e.activation(out=hf[a][:], in_=ps[pb][:], func=AF.Copy), r=[psb[pb]], w=[b_hf[a]])
                    kv_store(3, i, hf[a], b_hf[a])
                    if i == 16:
                        K.op("dve", lambda e, pb=pb: e.tensor_copy(out=samp["mb"]["V"][:], in_=ps[pb][:]), r=[psb[pb]], w=[samp["mb"]["b"]])
                    else:
                        K.op("dve", lambda e, i=i, pb=pb: e.tensor_copy(out=Vm[:, i, :], in_=ps[pb][:]), r=[psb[pb]], w=[b_Vm[i]])
                    continue
                a = i % 2
                K.op("act", lambda e, a=a, pb=pb, kind=kind: e.activation(out=kmf[a][:], in_=ps[pb][:], func=AF.Copy,
                                                                         scale=(1.0 if kind == "k" else 0.125)),
                     r=[psb[pb]], w=[b_kmf[a]])
                rotary(kmf[a], b_kmf[a], i)
                b = cnt["hb"] % 2
                cnt["hb"] += 1
                K.op("pool", lambda e, a=a, b=b: e.tensor_copy(out=hb[b][:], in_=kmf[a][:]), r=[b_kmf[a]], w=[b_hb[b]])
                if kind == "k":
                    kv_store(2, i, kmf[a], b_kmf[a])
                    if i == 16:
                        to_T(hb[b], b_hb[b], None, samp["mb"]["b"], i, 4 + i % 2, dst_ap=samp["mb"]["KT"][:])
                    else:
                        to_T(hb[b], b_hb[b], KmT, b_KmT[i], i, 4 + i % 2)
                    if i < 16 and i % 2 == 1:
                        n = i // 2
                        for ch in range(4):
                            for u in range(2):
                                src = kmf[1 - a] if u == 0 else kmf[a]
                                K.op("pe", lambda e, ch=ch, u=u, src=src, n=n: e.matmul(ps[6][:, ch * 8 + n:ch * 8 + n + 1],
                                                                                    lhsT=src[:, ch * 128:(ch + 1) * 128],
                                                                                    rhs=ones256[:, 0:1], start=(u == 0), stop=(u == 1)),
                                     r=[b_kmf[0], b_kmf[1], b_ones], w=[psb[6]])
                        if i == 15:
                            K.op("dve", lambda e: e.tensor_copy(out=kmT[:], in_=ps[6][:, 0:32].rearrange("p (c n) -> p c n", c=4)),
                                 r=[psb[6]], w=[b_kmT])
                else:
                    if i == 16:
                        to_T(hb[b], b_hb[b], None, samp["mb"]["b"], i, 4 + i % 2, dst_ap=samp["mb"]["QT"][:])
                        for cc in range(4):
                            K.op("pe", lambda e, cc=cc, a=a: e.transpose(out=ps[7][:, cc * 128:(cc + 1) * 128], in_=kmf[a][:, cc * 128:(cc + 1) * 128],
                                                                      identity=identf), r=[b_kmf[a], b_cst], w=[psb[7]])
                        K.op("act", lambda e: e.activation(out=sQTf[:], in_=ps[7][:].rearrange("p (c t) -> p c t", c=4), func=AF.Copy),
                             r=[psb[7]], w=[b_sQTf])
                    else:
                        to_T(hb[b], b_hb[b], QmT, b_QmT[i], i, 4 + i % 2)
                    own = i // 2
                    if i < 16 and own >= 1:
                        for cc in range(4):
                            K.op("pe", lambda e, cc=cc, a=a: e.transpose(out=ps[7][:, cc * 128:(cc + 1) * 128], in_=kmf[a][:, cc * 128:(cc + 1) * 128],
                                                                      identity=identf), r=[b_kmf[a], b_cst], w=[psb[7]])
                        K.op("act", lambda e: e.activation(out=qTf[:], in_=ps[7][:].rearrange("p (c t) -> p c t", c=4), func=AF.Copy),
                             r=[psb[7]], w=[b_qTf])
                        for h in range(8):
                            c2, p0 = h // 2, (h % 2) * 64
                            K.op("pe", lambda e, h=h, c2=c2, p0=p0: e.matmul(ps[6][:, 64 + h * 8:64 + h * 8 + 8], lhsT=qTf[p0:p0 + 64, c2, :],
                                                                         rhs=kmT[p0:p0 + 64, c2, :], start=True, stop=True),
                                 r=[b_qTf, b_kmT], w=[psb[6]])
                        K.op("dve", lambda e: e.tensor_copy(out=sblk[:], in_=ps[6][:, 64:128]), r=[psb[6]], w=[b_sblk])
                        if own < 8:
                            K.op("dve", lambda e, own=own: e.memset(sblk[:].rearrange("p (h n) -> p h n", h=8)[:, :, own:8], -1e30), w=[b_sblk])
                        for h in range(8):
                            K.op("dve", lambda e, h=h: e.max(out=mx8[:, h * 8:(h + 1) * 8], in_=sblk[:, h * 8:(h + 1) * 8]), r=[b_sblk], w=[b_mx8])
                        ti = min(own, 3) - 1
                        for h in range(8):
                            K.op("dve", lambda e, h=h, ti=ti: e.tensor_scalar(out=bias01[:, h * 8:(h + 1) * 8], in0=sblk[:, h * 8:(h + 1) * 8],
                                                                          scalar1=mx8[:, h * 8 + ti:h * 8 + ti + 1], scalar2=1.0,
                                                                          op0=ALU.is_ge, op1=ALU.subtract),
                                 r=[b_sblk, b_mx8], w=[b_bias01])
                        if own < 8:
                            K.op("dve", lambda e, own=own: e.memset(bias01[:].rearrange("p (h n) -> p h n", h=8)[:, :, own:own + 1], 0.0), w=[b_bias01])
                        pstb = ps[7].bitcast(BF16)
                        K.op("pe", lambda e: e.transpose(out=pstb[0:64, 0:128], in_=bias01[:], identity=ident), r=[b_bias01, b_cbf], w=[psb[7]])
                        K.op("dve", lambda e, i=i: e.tensor_copy(out=biasT[0:64, i * 128:(i + 1) * 128], in_=pstb[0:64, 0:128]), r=[psb[7]], w=[b_biasT[i]])

    o_mT = sb("o_mT", [128, 4, NTOK], BF16)
    b_omT = Buf("o_mT")
    rl = xt[1][:, 0:512]
    b_rl = b_xt[1]

    def mb_P(t):
        h, g, j = steps_m[t]
        a = t % 2
        c, p0 = h // 2, (h % 2) * 64
        q0 = max(j - 4 * g, 0) * 128
        r_ = h * 8 + j // 2
        K.op("pe", lambda e: e.matmul(ps[a][:, q0:512], lhsT=KmT[p0:p0 + 64, c, j * 128:(j + 1) * 128],
                                      rhs=QmT[p0:p0 + 64, c, g * 512 + q0:(g + 1) * 512], start=True, stop=False),
             r=[b_KmT[j]] + [b_QmT[4 * g + u] for u in range(4)], w=[psb[a]])
        K.op("pe", lambda e: e.matmul(ps[a][:, q0:512], lhsT=selm[r_ // 32][:, r_ % 32, :], rhs=biasT[:, g * 512 + q0:(g + 1) * 512],
                                      start=False, stop=(j < 4 * g)),
             r=[b_wst[r_ // 32], b_selm] + [b_biasT[4 * g + u] for u in range(4)], w=[psb[a]])
        if j >= 4 * g:
            K.op("pe", lambda e: e.matmul(ps[a][:, q0:q0 + 128], lhsT=ident, rhs=mmb, start=False, stop=True), r=[b_cbf], w=[psb[a]])
        K.op("act", lambda e: e.activation(out=A_sb[a][:, q0:], in_=ps[a][:, q0:], func=AF.Exp), r=[psb[a]], w=[b_A[a]])

    def mb_AV(t):
        h, g, j = steps_m[t]
        a = t % 2
        c, p0 = h // 2, (h % 2) * 64
        q0 = max(j - 4 * g, 0) * 128
        ob = 4 + (h * 4 + g) % 2
        lb = 2 + (h * 4 + g) % 2
        first, last = (j == 0), (j == 4 * g + 3)
        K.op("pe", lambda e: e.matmul(ps[ob][:, q0:], lhsT=Vm[:, j, c * 128:(c + 1) * 128], rhs=A_sb[a][:, q0:], start=first, stop=last),
             r=[b_A[a], b_Vm[j]], w=[psb[ob]])
        K.op("pe", lambda e: e.matmul(ps[lb][:, q0:], lhsT=ones_bf[:], rhs=A_sb[a][:, q0:], start=first, stop=last),
             r=[b_A[a], b_ones], w=[psb[lb]])
        if last:
            K.op("dve", lambda e: e.reciprocal(out=rl[p0:p0 + 64, :], in_=ps[lb][p0:p0 + 64, :]), r=[psb[lb]], w=[b_rl])
            K.op("dve", lambda e: e.tensor_tensor(out=o_mT[p0:p0 + 64, c, g * 512:(g + 1) * 512], in0=ps[ob][p0:p0 + 64, :],
                                                  in1=rl[p0:p0 + 64, :], op=ALU.mult), r=[psb[ob], b_rl], w=[b_omT])

    selm = [wst[u][:].rearrange("p a b -> p (a b)").bitcast(BF16).rearrange("p (r m) -> p r m", r=32) for u in range(2)]
    if STAGE >= 3:
        for u in range(2):
            K.op("dve", lambda e, u=u: e.tensor_copy(out=selm[u][0:64], in_=id30k[:, 32 * u:32 * u + 32].unsqueeze(2).broadcast_to([64, 32, 128])),
                 r=[b_selm], w=[b_wst[u]])
            K.op("dve", lambda e, u=u: e.memset(selm[u][64:128], 0.0), w=[b_wst[u]])
    steps_m = []
    for h in range(8):
        for g in range(4):
            for j in range(0, 4 * g + 4):
                steps_m.append((h, g, j))
    if STAGE >= 3 and os.environ.get('KB2', '1') == '1':
        mb_P(0)
        for t in range(len(steps_m)):
            if t + 1 < len(steps_m):
                mb_P(t + 1)
            mb_AV(t)
        K.dma("sp", lambda e: e.dma_start(out=dbg_o[1, :, :, 0:S], in_=o_mT[:, :, 0:S]), r=[b_omT])

    if STAGE >= 4 and (not SIM or os.environ.get("KBR", "sb") in ("mb", "both")):
        sample_attn("mb")
    if STAGE >= 4:
        if SIM and os.environ.get("KBR", "sb") == "sb":
            K.op("pool", lambda e: e.memset(o_mT[:, :, S:NTOK], 0.0), w=[b_omT])
        if SIM and os.environ.get("KBR", "sb") == "mb":
            K.op("pool", lambda e: e.memset(o_sT[:, :, S:NTOK], 0.0), w=[b_osT])
        K.dma("sp", lambda e: e.dma_start(out=dbg_o[0, :, :, S:NTOK], in_=o_sT[:, :, S:NTOK]), r=[b_osT])
        K.dma("sp", lambda e: e.dma_start(out=dbg_o[1, :, :, S:NTOK], in_=o_mT[:, :, S:NTOK]), r=[b_omT])

    if STAGE >= 5:
        K.barrier()
        mT = [QsT, KsT]
        bm_T = [Buf(f"mT{i}") for i in range(NT)]
        Wps_h = view(Vs, BF16, 0, [128, 4, 512])
        Wpm_h = view(Vs, BF16, 4096, [128, 4, 512])
        sg = [view(Vs, F32, 8192, [128, 512]), view(Vs, F32, 10240, [128, 512])]
        mm_ = [view(Vs, F32, 12288, [128, 512]), view(Vs, F32, 14336, [128, 512])]
        bWp, bsg, bmm = Buf("Wp"), [Buf("sg0"), Buf("sg1")], [Buf("mm0"), Buf("mm1")]
        bwst2, bwb2 = [Buf("wst0c"), Buf("wst1c")], [Buf("wb0c"), Buf("wb1c")]

        def load_any(src_halves, dsts, bdst, engs=("pool", "pool")):
            for hh, (src, dst) in enumerate(zip(src_halves, dsts)):
                u = hh % 2
                shp = list(src.shape)
                stv = view(wst[u], F32, 0, shp)
                K.dma("sp", lambda e, src=src, stv=stv: e.dma_start(out=stv, in_=src), w=[bwst2[u]])
                if engs[hh] == "pool":
                    K.op("pool", lambda e, dst=dst, stv=stv: e.tensor_copy(out=dst, in_=stv), r=[bwst2[u]], w=[bdst])
                else:
                    K.op("act", lambda e, dst=dst, stv=stv: e.activation(out=dst, in_=stv, func=AF.Copy), r=[bwst2[u]], w=[bdst])

        w_ps_v = w_ps.rearrange("(k p) n -> p k n", p=128)
        w_pm_v = w_pm.rearrange("(k p) n -> p k n", p=128)
        w_o_v = w_o.rearrange("(k p) n -> p k n", p=128)
        mb16 = hb
        for cb in range(2):
            load_any([w_in_v[:, 0:4, (6 + cb) * 512:(7 + cb) * 512], w_in_v[:, 4:8, (6 + cb) * 512:(7 + cb) * 512]],
                     [wb[0][:, 0:4, :], wb[0][:, 4:8, :]], bwb2[0])
            load_any([w_in_v[:, 0:4, (8 + cb) * 512:(9 + cb) * 512], w_in_v[:, 4:8, (8 + cb) * 512:(9 + cb) * 512]],
                     [wb[1][:, 0:4, :], wb[1][:, 4:8, :]], bwb2[1])
            load_any([w_ps_v[:, :, cb * 512:(cb + 1) * 512], w_pm_v[:, :, cb * 512:(cb + 1) * 512]], [Wps_h, Wpm_h], bWp)
            for i in range(NT):
                tsl = slice(i * 128, (i + 1) * 128)
                for kc in range(8):
                    K.op("pe", lambda e, kc=kc, tsl=tsl: e.matmul(ps[0][:], lhsT=xnT[:, kc, tsl], rhs=wb[0][:, kc, :], start=(kc == 0), stop=(kc == 7)),
                         r=[b_xnT[i], bwb2[0]], w=[psb[0]])
                for kc in range(8):
                    K.op("pe", lambda e, kc=kc, tsl=tsl: e.matmul(ps[1][:], lhsT=xnT[:, kc, tsl], rhs=wb[1][:, kc, :], start=(kc == 0), stop=(kc == 7)),
                         r=[b_xnT[i], bwb2[1]], w=[psb[1]])
                for c in range(4):
                    K.op("pe", lambda e, c=c, tsl=tsl: e.matmul(ps[2][:], lhsT=o_sT[:, c, tsl], rhs=Wps_h[:, c, :], start=(c == 0), stop=(c == 3)),
                         r=[b_osT, bWp], w=[psb[2]])
                for c in range(4):
                    K.op("pe", lambda e, c=c, tsl=tsl: e.matmul(ps[3][:], lhsT=o_mT[:, c, tsl], rhs=Wpm_h[:, c, :], start=(c == 0), stop=(c == 3)),
                         r=[b_omT, bWp], w=[psb[3]])
                K.op("act", lambda e: e.activation(out=sg[0], in_=ps[0][:], func=AF.Sigmoid), r=[psb[0]], w=[bsg[0]])
                K.op("act", lambda e: e.activation(out=sg[1], in_=ps[1][:], func=AF.Sigmoid), r=[psb[1]], w=[bsg[1]])
                K.op("dve", lambda e: e.tensor_tensor(out=mm_[0], in0=sg[0], in1=ps[2][:], op=ALU.mult), r=[bsg[0], psb[2]], w=[bmm[0]])
                K.op("dve", lambda e: e.tensor_tensor(out=mm_[1], in0=sg[1], in1=ps[3][:], op=ALU.mult), r=[bsg[1], psb[3]], w=[bmm[1]])
                b = i % 2
                K.op("pool", lambda e, b=b: e.tensor_tensor(out=mb16[b][:], in0=mm_[0], in1=mm_[1], op=ALU.add), r=bmm, w=[b_hb[b]])
                to_T(mb16[b], b_hb[b], mT[cb], bm_T[i], i, 4 + i % 2)

        K.barrier()
        load_any([w_o_v[:, 0:4, 0:512], w_o_v[:, 4:8, 0:512]], [wb[0][:, 0:4, :], wb[0][:, 4:8, :]], bwb2[0])
        load_any([w_o_v[:, 0:4, 512:1024], w_o_v[:, 4:8, 512:1024]], [wb[1][:, 0:4, :], wb[1][:, 4:8, :]], bwb2[1])
        bg2 = Buf("gbc2")
        K.dma("sp", lambda e: e.dma_start(out=gbc[:], in_=g_ffn.partition_broadcast(128)), w=[bg2])
        xn2f = view(Vs, F32, 0, [128, D])
        xTf = view(Vs, F32, 4096, [128, 8, 128])
        combT = view(Vs, BF16, 8192, [128, NTOK])
        SelE = view(Vs, BF16, 8192 + 2 * NTOK, [128, 16, 128])
        bx2, bxT, bcT, bSel = Buf("xn2f"), Buf("xTf"), Buf("combT"), Buf("SelE")
        wr = sb("wr", [128, 8, 20], F32)
        brb = sb("brb", [128, 20], F32)
        bwr = Buf("wr")
        K.dma("sp", lambda e: e.dma_start(out=wr[:], in_=w_r.rearrange("(k p) n -> p k n", p=128)), w=[bwr])
        K.dma("sp", lambda e: e.dma_start(out=brb[:], in_=b_r.partition_broadcast(128)), w=[bwr])
        rt = sb("rt", [128, 64], F32)
        comb = sb("comb", [128, 16], F32)
        combb = sb("combb", [128, 16], BF16)
        brt = Buf("rt")
        b_x1d = [Buf(f"x1d{i}") for i in range(NT)]
        K.op("dve", lambda e: e.tensor_copy(out=SelE[0:16], in_=ident[0:16, 0:16].unsqueeze(2).broadcast_to([16, 16, 128])), r=[b_cbf], w=[bSel])
        lgs, ohg, esel, msk1, e2, msk2, c4 = rt[:, 0:20], rt[:, 20:24], rt[:, 24:28], rt[:, 28:32], rt[:, 32:36], rt[:, 36:40], rt[:, 40:44]
        sc = lambda k: rt[:, 48 + k:49 + k]
        eg = rt[:, 44:48]

        def R(fn, eng="dve"):
            K.op(eng, fn, r=[brt], w=[brt])

        for i in range(NT):
            j = i % 2
            tsl = slice(i * 128, (i + 1) * 128)
            K.dma("sp", lambda e, j=j, tsl=tsl: e.dma_start(out=xt[j][:], in_=xin[tsl, :]), w=[b_xt[j]])
            for half in range(2):
                for kc in range(8):
                    K.op("pe", lambda e, kc=kc, half=half, tsl=tsl: e.matmul(ps[half][:], lhsT=mT[kc // 4][:, kc % 4, tsl], rhs=wb[half][:, kc, :],
                                                                           start=(kc == 0), stop=(kc == 7)), r=[bm_T[i], bwb2[half]], w=[psb[half]])
                K.op("dve", lambda e, j=j, half=half: e.tensor_tensor(out=xt[j][:, half * 512:(half + 1) * 512], in0=xt[j][:, half * 512:(half + 1) * 512],
                                                                    in1=ps[half][:], op=ALU.add), r=[b_xt[j], psb[half]], w=[b_xt[j]])
            K.dma("sp", lambda e, j=j, tsl=tsl: e.dma_start(out=y_out[tsl, :], in_=xt[j][:]), r=[b_xt[j]], w=[b_x1d[i]])
            K.op("act", lambda e, i=i, j=j: e.activation(out=junk[:], in_=xt[j][:], func=AF.Square, accum_out=ss[:, i:i + 1]),
                 r=[b_xt[j]], w=[b_junk, b_ss[i]])
            K.op("act", lambda e, i=i: e.activation(out=rs[:, i:i + 1], in_=ss[:, i:i + 1], func=AF.Sqrt, bias=1e-6, scale=1.0 / D), r=[b_ss[i]], w=[b_rs[i]])
            K.op("dve", lambda e, i=i: e.reciprocal(out=rstd[:, i:i + 1], in_=rs[:, i:i + 1]), r=[b_rs[i]], w=[b_rstd[i]])
            K.op("dve", lambda e, i=i, j=j: e.scalar_tensor_tensor(out=xn2f, in0=xt[j][:], scalar=rstd[:, i:i + 1], in1=gbc[:], op0=ALU.mult, op1=ALU.mult),
                 r=[b_xt[j], b_rstd[i], bg2], w=[bx2])
            K.op("pool", lambda e, j=j: e.tensor_copy(out=xn[j][:], in_=xn2f), r=[bx2], w=[b_xn[j]])
            pst = ps[2].bitcast(BF16)
            for kc in range(8):
                K.op("pe", lambda e, kc=kc, j=j, pst=pst: e.transpose(out=pst[:, kc * 128:(kc + 1) * 128], in_=xn[j][:, kc * 128:(kc + 1) * 128], identity=ident),
                     r=[b_xn[j], b_cbf], w=[psb[2]])
            K.op("act", lambda e, tsl=tsl, pst=pst: e.activation(out=xnT[:, :, tsl], in_=pst.rearrange("p (k t) -> p k t", k=8), func=AF.Copy),
                 r=[psb[2]], w=[b_xnT[i]])
            for half in range(2):
                for kk in range(4):
                    kc = half * 4 + kk
                    K.op("pe", lambda e, kc=kc, kk=kk, half=half: e.transpose(out=ps[3 + half][:, kk * 128:(kk + 1) * 128], in_=xn2f[:, kc * 128:(kc + 1) * 128], identity=identf),
                         r=[bx2, b_cst], w=[psb[3 + half]])
                K.op("act" if half == 0 else "dve",
                     (lambda e, half=half: e.activation(out=xTf[:, half * 4:half * 4 + 4, :], in_=ps[3 + half][:].rearrange("p (k t) -> p k t", k=4), func=AF.Copy)) if half == 0 else
                     (lambda e, half=half: e.tensor_copy(out=xTf[:, half * 4:half * 4 + 4, :], in_=ps[3 + half][:].rearrange("p (k t) -> p k t", k=4))),
                     r=[psb[3 + half]], w=[bxT])
            for kc in range(8):
                K.op("pe", lambda e, kc=kc: e.matmul(ps[5][:, 0:20], lhsT=xTf[:, kc, :], rhs=wr[:, kc, :], start=(kc == 0), stop=(kc == 7)),
                     r=[bxT, bwr], w=[psb[5]])
            K.op("dve", lambda e: e.tensor_tensor(out=lgs, in0=ps[5][:, 0:20], in1=brb[:], op=ALU.add), r=[psb[5], bwr, brt], w=[brt])
            R(lambda e: e.tensor_reduce(out=sc(0), in_=lgs[:, 0:4], axis=AX.X, op=ALU.max))
            R(lambda e: e.tensor_scalar(out=ohg, in0=lgs[:, 0:4], scalar1=sc(0), scalar2=None, op0=ALU.is_ge))
            R(lambda e: e.tensor_scalar(out=sc(1), in0=sc(0), scalar1=-1.0, scalar2=None, op0=ALU.mult))
            R(lambda e: e.activation(out=eg, in_=lgs[:, 0:4], func=AF.Exp, bias=sc(1), accum_out=sc(2)), "act")
            R(lambda e: e.reciprocal(out=sc(3), in_=sc(2)))
            R(lambda e: e.tensor_scalar(out=esel, in0=lgs[:, 4:8], scalar1=ohg[:, 0:1], scalar2=None, op0=ALU.mult))
            for g in range(1, 4):
                R(lambda e, g=g: e.scalar_tensor_tensor(out=esel, in0=lgs[:, 4 + 4 * g:8 + 4 * g], scalar=ohg[:, g:g + 1], in1=esel, op0=ALU.mult, op1=ALU.add))
            R(lambda e: e.tensor_reduce(out=sc(4), in_=esel, axis=AX.X, op=ALU.max))
            R(lambda e: e.tensor_scalar(out=msk1, in0=esel, scalar1=sc(4), scalar2=None, op0=ALU.is_ge))
            R(lambda e: e.scalar_tensor_tensor(out=e2, in0=msk1, scalar=-1e30, in1=esel, op0=ALU.mult, op1=ALU.add))
            R(lambda e: e.tensor_reduce(out=sc(5), in_=e2, axis=AX.X, op=ALU.max))
            R(lambda e: e.tensor_scalar(out=msk2, in0=e2, scalar1=sc(5), scalar2=None, op0=ALU.is_ge))
            R(lambda e: e.tensor_tensor(out=sc(6), in0=sc(5), in1=sc(4), op=ALU.subtract))
            R(lambda e: e.activation(out=sc(7), in_=sc(6), func=AF.Exp), "act")
            R(lambda e: e.tensor_scalar(out=sc(8), in0=sc(7), scalar1=1.0, scalar2=None, op0=ALU.add))
            R(lambda e: e.reciprocal(out=sc(9), in_=sc(8)))
            R(lambda e: e.tensor_tensor(out=sc(10), in0=sc(3), in1=sc(9), op=ALU.mult))
            R(lambda e: e.tensor_tensor(out=sc(11), in0=sc(10), in1=sc(7), op=ALU.mult))
            R(lambda e: e.tensor_scalar(out=c4, in0=msk1, scalar1=sc(10), scalar2=None, op0=ALU.mult))
            R(lambda e: e.scalar_tensor_tensor(out=c4, in0=msk2, scalar=sc(11), in1=c4, op0=ALU.mult, op1=ALU.add))
            for g in range(4):
                R(lambda e, g=g: e.tensor_scalar(out=comb[:, 4 * g:4 * g + 4], in0=c4, scalar1=ohg[:, g:g + 1], scalar2=None, op0=ALU.mult))
            R(lambda e: e.tensor_copy(out=combb[:], in_=comb[:]))
            pst5 = ps[5].bitcast(BF16)
            K.op("pe", lambda e: e.transpose(out=pst5[0:16, 128:256], in_=combb[:], identity=ident), r=[brt, b_cbf], w=[psb[5]])
            K.op("dve", lambda e, tsl=tsl: e.tensor_copy(out=combT[0:16, tsl], in_=pst5[0:16, 128:256]), r=[psb[5]], w=[bcT])

        K.barrier()
        bg3 = Buf("gbc3")
        K.dma("sp", lambda e: e.dma_start(out=gbc[:], in_=g_fin.partition_broadcast(128)), w=[bg3])
        hdn = [view(QsT, BF16, 0, [128, 16, 512]), view(KsT, BF16, 0, [128, 16, 512])]
        bhd = Buf("hdn")
        sa = [view(xn[0], F32, 0, [128, 512]), view(xn[1], F32, 0, [128, 512])]
        tt = [xn2f[:, 0:512], xn2f[:, 512:1024]]
        bsa, btt = [Buf("sa0"), Buf("sa1")], [Buf("tt0"), Buf("tt1")]
        wdb = [view(o_sT, BF16, 0, [128, 2, D]), view(o_mT, BF16, 0, [128, 2, D])]
        bwd = [Buf("wd0"), Buf("wd1")]
        w_eg_v = w_eg.rearrange("e (k p) n -> e p k n", p=128)
        w_eu_v = w_eu.rearrange("e (k p) n -> e p k n", p=128)
        w_ed_v = w_ed.rearrange("e (k p) n -> e p k n", p=128)
        groups = [(g * 512, 512) for g in range(4)] + [(S, 128)]
        it_ = 0
        for (t0, nt) in groups:
            for ex in range(16):
                u = ex % 2
                load_any([w_eg_v[ex], w_eu_v[ex]], [wb[u][:, :, 0:256], wb[u][:, :, 256:512]], bwb2[u],
                         engs=(("pool", "act") if os.environ.get("KUP", "act") == "act" else ("pool", "pool")))
                K.op("pe", lambda e, ex=ex, t0=t0, nt=nt: e.matmul(ps[4][:, 0:nt], lhsT=SelE[0:16, ex, :], rhs=combT[0:16, t0:t0 + nt], start=True, stop=True),
                     r=[bSel, bcT], w=[psb[4]])
                for fc in range(2):
                    a = it_ % 2
                    it_ += 1
                    for kc in range(8):
                        K.op("pe", lambda e, kc=kc, u=u, fc=fc, a=a, t0=t0, nt=nt: e.matmul(ps[a][:, 0:nt], lhsT=wb[u][:, kc, fc * 128:(fc + 1) * 128],
                                                                                          rhs=xnT[:, kc, t0:t0 + nt], start=(kc == 0), stop=(kc == 7)),
                             r=[bwb2[u]] + [b_xnT[t0 // 128 + q] for q in range(nt // 128)], w=[psb[a]])
                    for kc in range(8):
                        K.op("pe", lambda e, kc=kc, u=u, fc=fc, a=a, t0=t0, nt=nt: e.matmul(ps[2 + a][:, 0:nt], lhsT=wb[u][:, kc, 256 + fc * 128:256 + (fc + 1) * 128],
                                                                                          rhs=xnT[:, kc, t0:t0 + nt], start=(kc == 0), stop=(kc == 7)),
                             r=[bwb2[u]] + [b_xnT[t0 // 128 + q] for q in range(nt // 128)], w=[psb[2 + a]])
                    K.op("act", lambda e, a=a, nt=nt: e.activation(out=sa[a][:, 0:nt], in_=ps[a][:, 0:nt], func=AF.Silu), r=[psb[a]], w=[bsa[a]])
                    K.op("dve", lambda e, a=a, nt=nt: e.tensor_tensor(out=tt[a][:, 0:nt], in0=sa[a][:, 0:nt], in1=ps[2 + a][:, 0:nt], op=ALU.mult),
                         r=[bsa[a], psb[2 + a]], w=[btt[a]])
                    ch = ex * 2 + fc
                    K.op("dve", lambda e, a=a, nt=nt, ch=ch: e.tensor_tensor(out=hdn[ch // 16][:, ch % 16, 0:nt], in0=tt[a][:, 0:nt], in1=ps[4][:, 0:nt], op=ALU.mult),
                         r=[btt[a], psb[4]], w=[bhd])
            ntile = nt // 128
            for ex in range(16):
                u = ex % 2
                stv = view(wst[u], F32, 0, [128, 2, D])
                K.dma("sp", lambda e, ex=ex, stv=stv: e.dma_start(out=stv, in_=w_ed_v[ex]), w=[bwst2[u]])
                K.op("act", lambda e, u=u, stv=stv: e.activation(out=wdb[u], in_=stv, func=AF.Copy), r=[bwst2[u]], w=[bwd[u]])
                for fc in range(2):
                    ch = ex * 2 + fc
                    for q in range(ntile):
                        for half in range(2):
                            K.op("pe", lambda e, ch=ch, q=q, half=half, u=u, fc=fc: e.matmul(ps[q * 2 + half][:], lhsT=hdn[ch // 16][:, ch % 16, q * 128:(q + 1) * 128],
                                                                                           rhs=wdb[u][:, fc, half * 512:(half + 1) * 512],
                                                                                           start=(ch == 0), stop=(ch == 31)),
                                 r=[bhd, bwd[u]], w=[psb[q * 2 + half]])
            for q in range(ntile):
                i = t0 // 128 + q
                j = i % 2
                tsl = slice(i * 128, (i + 1) * 128)
                K.dma("sp", lambda e, j=j, tsl=tsl: e.dma_start(out=xt[j][:], in_=y_out[tsl, :]), r=[b_x1d[i]], w=[b_xt[j]])
                for half in range(2):
                    K.op("dve", lambda e, j=j, half=half, q=q: e.tensor_tensor(out=xt[j][:, half * 512:(half + 1) * 512], in0=xt[j][:, half * 512:(half + 1) * 512],
                                                                             in1=ps[q * 2 + half][:], op=ALU.add), r=[b_xt[j], psb[q * 2 + half]], w=[b_xt[j]])
                K.op("act", lambda e, i=i, j=j: e.activation(out=junk[:], in_=xt[j][:], func=AF.Square, accum_out=ss[:, i:i + 1]),
                     r=[b_xt[j]], w=[b_junk, b_ss[i]])
                K.op("act", lambda e, i=i: e.activation(out=rs[:, i:i + 1], in_=ss[:, i:i + 1], func=AF.Sqrt, bias=1e-6, scale=1.0 / D), r=[b_ss[i]], w=[b_rs[i]])
                K.op("dve", lambda e, i=i: e.reciprocal(out=rstd[:, i:i + 1], in_=rs[:, i:i + 1]), r=[b_rs[i]], w=[b_rstd[i]])
                K.op("dve", lambda e, i=i, j=j: e.scalar_tensor_tensor(out=xt[j][:], in0=xt[j][:], scalar=rstd[:, i:i + 1], in1=gbc[:], op0=ALU.mult, op1=ALU.mult),
                     r=[b_xt[j], b_rstd[i], bg3], w=[b_xt[j]])
                K.dma("sp", lambda e, j=j, tsl=tsl: e.dma_start(out=y_out[tsl, :], in_=xt[j][:]), r=[b_xt[j], b_x1d[i]], w=[b_x1d[i]])

    K.emit()
    return nc


_NC = None


def kernel(**inputs):
    global _NC
    if _NC is None:
        _NC = build()
    nc = _NC
    xp = np.asarray(inputs["x_prompt"], np.float32)
    xs = np.asarray(inputs["x_sample"], np.float32)
    cst = make_consts()
    tok = np.arange(128)
    col = np.arange(512)
    s_of, t_of = col // 32, col % 4
    same = (tok[:, None] // 4 == s_of[None, :]) & (tok[:, None] < 64)
    m01 = np.stack([same & ((tok[:, None] % 4) < t_of[None, :]), same & ((tok[:, None] % 4) <= t_of[None, :])]).astype(np.float32)
    pt = np.asarray(inputs["page_table"], np.int32)
    in_maps = []
    for c in range(NCORES):
        xin = np.zeros((NTOK, D), np.float32)
        xin[:S] = xp[c]
        xin[S:S + 64] = xs[16 * c:16 * c + 16].reshape(64, D)
        in_maps.append({
            "xin": xin, "cst": cst,
            "g_mix": np.ascontiguousarray(inputs["g_mix"][0][None, :], np.float32),
            "w_in": np.ascontiguousarray(inputs["w_in"][0], np.float32),
            "m01": m01,
            "w_proj_sb": np.ascontiguousarray(inputs["w_proj_sb"][0], np.float32),
            "w_proj_moba": np.ascontiguousarray(inputs["w_proj_moba"][0], np.float32),
            "w_out": np.ascontiguousarray(inputs["w_out"][0], np.float32),
            "g_ffn": np.ascontiguousarray(inputs["g_ffn"][0][None, :], np.float32),
            "g_final": np.ascontiguousarray(np.asarray(inputs["g_final"])[None, :], np.float32),
            "w_router": np.ascontiguousarray(np.concatenate([inputs["w_router_group"][0], inputs["w_router_expert"][0]], axis=1), np.float32),
            "b_router": np.ascontiguousarray(np.concatenate([inputs["b_router_group"][0], inputs["b_router_expert"][0]])[None, :], np.float32),
            "w_expert_gate": np.ascontiguousarray(inputs["w_expert_gate"][0], np.float32),
            "w_expert_up": np.ascontiguousarray(inputs["w_expert_up"][0], np.float32),
            "w_expert_down": np.ascontiguousarray(inputs["w_expert_down"][0], np.float32),
        })
        m = in_maps[-1]
        if SMALL:
            rows = pt[16 * c:16 * c + 16].reshape(-1)
            for nm, key in (("sb_k", "cache_sb_k"), ("sb_v", "cache_sb_v"), ("mb_k", "cache_moba_k"), ("mb_v", "cache_moba_v")):
                m["cache_" + nm] = np.ascontiguousarray(inputs[key][0][rows].reshape(256 * 128, 512)) if (c == 0 or not SIM) else np.zeros((256 * 128, 512), np.float32)
            m["ptab"] = np.arange(256, dtype=np.int32)[None, :]
        else:
            for nm, key in (("sb_k", "cache_sb_k"), ("sb_v", "cache_sb_v"), ("mb_k", "cache_moba_k"), ("mb_v", "cache_moba_v")):
                m["cache_" + nm] = inputs[key][0].reshape(2560 * 128, 512)
            m["ptab"] = np.ascontiguousarray(pt[16 * c:16 * c + 16].reshape(1, 256))
    res = run_bass_kernel_spmd(nc, in_maps, core_ids=list(range(NCORES)), **({"trace": True} if os.environ.get("KTRACE") else {}))
    if os.environ.get("KTRACE"):
        print("EXEC_TIME_NS", res.exec_time_ns)
    outs = res.results
    global _LAST
    _LAST = outs
    kv = np.stack([o["kv_out"] for o in outs])
    y = np.stack([o["y_out"] for o in outs])
    y_prompt = y[:, :S, :]
    y_sample = y[:, S:S + 64, :].reshape(128, 4, D)
    p = [kv[:, k, :S, :].reshape(1, 8, S, 8, 64) for k in range(4)]
    s = [kv[:, k, S:S + 64, :].reshape(1, 128, 4, 8, 64) for k in range(4)]
    return (y_prompt, y_sample, p[0], p[1], p[2], p[3], s[0], s[1], s[2], s[3])
```
